# Optimizing a Trainium2 kernel written in Bass

```python
import math
import jax
import jax.numpy as jnp
from jax import lax
import numpy as np

D_MODEL = 1024
BATCH = 2
SEQ = 8192
DEPTH = 1

CHUNK = 64
LEFT_CHUNKS = 8
BAND = (LEFT_CHUNKS + 1) * CHUNK

HEAD_DIM = 64
N_HEADS_A = 8
N_HEADS_B = 8
WIDTH_A = N_HEADS_A * HEAD_DIM
WIDTH_B = N_HEADS_B * HEAD_DIM
MIX_WIDTH = WIDTH_A + WIDTH_B
REL_CLIP = 128
N_REL = 2 * REL_CLIP + 1
SB_BLOCK = 128

MEM_LEN = 256
N_HEADS_MEM = 4
HEAD_DIM_MEM = D_MODEL // N_HEADS_MEM

N_EXPERTS = 32
TOP_K = 4
D_FF = D_MODEL
SWIGLU_LIMIT = 7.0
SWIGLU_ALPHA = 1.702
MOE_BLOCK = 128

LN_EPS = 1e-5
RMS_EPS = 1e-6
DEEPNORM_ALPHA = (2.0 * DEPTH) ** 0.25
DEEPNORM_BETA = (8.0 * DEPTH) ** -0.25
NEG_INF = -1e30

kernel_name = "hybrid_chunked_stickbreaking_memxattn_moe"


def layer_norm(x, g, b):
    xf = x.astype(jnp.float32)
    mu = jnp.mean(xf, axis=-1, keepdims=True)
    var = jnp.mean(jnp.square(xf - mu), axis=-1, keepdims=True)
    return ((xf - mu) * lax.rsqrt(var + LN_EPS) * g + b).astype(x.dtype)


def rms_norm(x, g):
    xf = x.astype(jnp.float32)
    return (xf * lax.rsqrt(jnp.mean(jnp.square(xf), axis=-1, keepdims=True) + RMS_EPS) * g).astype(x.dtype)


def chunked_relpos_attention(q, k, v, rel_bias):
    b, h, s, dh = q.shape
    nc = s // CHUNK
    qc = q.reshape(b, h, nc, CHUNK, dh)
    pad = ((0, 0), (0, 0), (LEFT_CHUNKS * CHUNK, 0), (0, 0))
    kc = jnp.pad(k, pad).reshape(b, h, nc + LEFT_CHUNKS, CHUNK, dh)
    vc = jnp.pad(v, pad).reshape(b, h, nc + LEFT_CHUNKS, CHUNK, dh)
    k_band = jnp.concatenate([kc[:, :, o:o + nc] for o in range(LEFT_CHUNKS + 1)], axis=3)
    v_band = jnp.concatenate([vc[:, :, o:o + nc] for o in range(LEFT_CHUNKS + 1)], axis=3)
    band_pos = jnp.arange(BAND)
    key_chunk = jnp.arange(nc)[:, None] - LEFT_CHUNKS + band_pos[None, :] // CHUNK
    valid = key_chunk >= 0
    rel = LEFT_CHUNKS * CHUNK + jnp.arange(CHUNK)[:, None] - band_pos[None, :]
    rel_idx = jnp.clip(rel, -REL_CLIP, REL_CLIP) + REL_CLIP
    bias = rel_bias[:, rel_idx].astype(jnp.float32)
    scores = jnp.einsum('bhcqd,bhckd->bhcqk', qc, k_band).astype(jnp.float32) * (dh ** -0.5)
    scores = scores + bias[None, :, None]
    scores = jnp.where(valid[None, None, :, None, :], scores, NEG_INF)
    p = jax.nn.softmax(scores, axis=-1).astype(v.dtype)
    out = jnp.einsum('bhcqk,bhckd->bhcqd', p, v_band)
    return out.reshape(b, h, s, dh)


def stick_breaking_attention(q, k, v):
    b, h, s, dh = q.shape
    nb = s // SB_BLOCK
    scale = dh ** -0.5
    kf = k.astype(jnp.float32)
    vf = v.astype(jnp.float32)
    key_pos = jnp.arange(s)
    qb = q.reshape(b, h, nb, SB_BLOCK, dh).transpose(2, 0, 1, 3, 4)

    def block(args):
        q_blk, blk_idx = args
        q_pos = blk_idx * SB_BLOCK + jnp.arange(SB_BLOCK)
        causal = key_pos[None, :] < q_pos[:, None]
        z = jnp.einsum('bhqd,bhkd->bhqk', q_blk.astype(jnp.float32), kf) * scale
        log_beta = jax.nn.log_sigmoid(z)
        log_one_minus = jnp.where(causal, jax.nn.log_sigmoid(-z), 0.0)
        between = lax.cumsum(log_one_minus, axis=3, reverse=True) - log_one_minus
        weights = jnp.where(causal, jnp.exp(log_beta + between), 0.0)
        return jnp.einsum('bhqk,bhkd->bhqd', weights, vf)

    out = lax.map(block, (qb, jnp.arange(nb)))
    return out.transpose(1, 2, 0, 3, 4).reshape(b, h, s, dh).astype(q.dtype)


def hybrid_mixer(h, w_in, rel_bias, g_group_a, g_group_b, w_out):
    b, s, _ = h.shape
    proj = h @ w_in
    o1 = WIDTH_A
    o2 = 2 * WIDTH_A
    o3 = 3 * WIDTH_A
    o4 = o3 + WIDTH_B
    o5 = o3 + 2 * WIDTH_B
    qa, ka, va, qb, kb, vb = jnp.split(proj, [o1, o2, o3, o4, o5], axis=-1)

    def heads(t, n):
        return t.reshape(b, s, n, HEAD_DIM).transpose(0, 2, 1, 3)

    def merge(t):
        return t.transpose(0, 2, 1, 3).reshape(b, s, -1)

    out_a = merge(chunked_relpos_attention(heads(qa, N_HEADS_A), heads(ka, N_HEADS_A), heads(va, N_HEADS_A), rel_bias))
    out_b = merge(stick_breaking_attention(heads(qb, N_HEADS_B), heads(kb, N_HEADS_B), heads(vb, N_HEADS_B)))
    out = jnp.concatenate([rms_norm(out_a, g_group_a), rms_norm(out_b, g_group_b)], axis=-1)
    return out @ w_out


def memory_cross_attention(h, mem, w_q_mem, w_kv_mem, w_o_mem):
    b, s, d = h.shape
    m = mem.shape[1]
    q = (h @ w_q_mem).reshape(b, s, N_HEADS_MEM, HEAD_DIM_MEM)
    k, v = jnp.split(mem @ w_kv_mem, 2, axis=-1)
    k = k.reshape(b, m, N_HEADS_MEM, HEAD_DIM_MEM)
    v = v.reshape(b, m, N_HEADS_MEM, HEAD_DIM_MEM)
    scores = jnp.einsum('bqhd,bkhd->bhqk', q, k).astype(jnp.float32) * (HEAD_DIM_MEM ** -0.5)
    p = jax.nn.softmax(scores, axis=-1).astype(v.dtype)
    o = jnp.einsum('bhqk,bkhd->bqhd', p, v).reshape(b, s, d)
    return o @ w_o_mem


def moe_ffn(h, w_router, b_router, w_gate_up, b_gate_up, w_down, b_down):
    b, s, d = h.shape
    n = b * s
    x = h.reshape(n, d)
    logits = (x @ w_router + b_router).astype(jnp.float32)
    top_logits, top_idx = lax.top_k(logits, TOP_K)
    gates = jax.nn.softmax(top_logits, axis=-1)
    n_assign = n * TOP_K
    flat_expert = top_idx.reshape(-1)
    flat_token = jnp.repeat(jnp.arange(n, dtype=jnp.int32), TOP_K)
    flat_gate = gates.reshape(-1)
    order = jnp.argsort(flat_expert)
    sorted_expert = flat_expert[order]
    counts = jnp.bincount(flat_expert, length=N_EXPERTS)
    padded = (counts + MOE_BLOCK - 1) // MOE_BLOCK * MOE_BLOCK
    start = jnp.cumsum(counts) - counts
    pad_end = jnp.cumsum(padded)
    pad_start = pad_end - padded
    dest = pad_start[sorted_expert] + jnp.arange(n_assign) - start[sorted_expert]
    n_rows = n_assign + N_EXPERTS * MOE_BLOCK
    n_blocks = n_rows // MOE_BLOCK
    row_token = jnp.full((n_rows,), n, jnp.int32).at[dest].set(flat_token[order])
    row_gate = jnp.zeros((n_rows,), jnp.float32).at[dest].set(flat_gate[order])
    block_expert = jnp.minimum(
        jnp.searchsorted(pad_end, jnp.arange(n_blocks) * MOE_BLOCK, side='right'), N_EXPERTS - 1)
    x_pad = jnp.concatenate([x, jnp.zeros((1, d), x.dtype)], axis=0)
    x_rows = x_pad[row_token].reshape(n_blocks, MOE_BLOCK, d)

    def expert_block(args):
        xb, e = args
        gu = xb @ w_gate_up[e] + b_gate_up[e]
        gate, up = jnp.split(gu, 2, axis=-1)
        gate = jnp.minimum(gate, SWIGLU_LIMIT)
        up = jnp.clip(up, -SWIGLU_LIMIT, SWIGLU_LIMIT)
        glu = gate * jax.nn.sigmoid(gate * SWIGLU_ALPHA)
        return ((up + 1.0) * glu) @ w_down[e] + b_down[e]

    y_rows = lax.map(expert_block, (x_rows, block_expert)).reshape(n_rows, d)
    y = jax.ops.segment_sum(y_rows * row_gate[:, None].astype(y_rows.dtype), row_token, num_segments=n + 1)[:n]
    return y.reshape(b, s, d)


def setup_inputs(seed: int = 0) -> dict:
    key = jax.random.key(seed)
    ks = jax.random.split(key, 20)

    def normal(k, shape, scale):
        return jax.random.normal(k, shape, jnp.float32) * scale

    beta = DEEPNORM_BETA
    x = normal(ks[0], (BATCH, SEQ, D_MODEL), 1.0)
    mem = normal(ks[1], (BATCH, MEM_LEN, D_MODEL), 1.0)
    col_scale = jnp.concatenate([
        jnp.ones((2 * WIDTH_A,), jnp.float32), jnp.full((WIDTH_A,), beta, jnp.float32),
        jnp.ones((2 * WIDTH_B,), jnp.float32), jnp.full((WIDTH_B,), beta, jnp.float32)])
    w_in = normal(ks[2], (DEPTH, D_MODEL, 3 * MIX_WIDTH), D_MODEL ** -0.5) * col_scale
    rel_bias = normal(ks[3], (DEPTH, N_HEADS_A, N_REL), 0.1)
    g_group_a = 1.0 + normal(ks[4], (DEPTH, WIDTH_A), 0.01)
    g_group_b = 1.0 + normal(ks[5], (DEPTH, WIDTH_B), 0.01)
    w_out = normal(ks[6], (DEPTH, MIX_WIDTH, D_MODEL), MIX_WIDTH ** -0.5 * beta)
    w_q_mem = normal(ks[7], (DEPTH, D_MODEL, D_MODEL), D_MODEL ** -0.5)
    kv_scale = jnp.concatenate([jnp.ones((D_MODEL,), jnp.float32), jnp.full((D_MODEL,), beta, jnp.float32)])
    w_kv_mem = normal(ks[8], (DEPTH, D_MODEL, 2 * D_MODEL), D_MODEL ** -0.5) * kv_scale
    w_o_mem = normal(ks[9], (DEPTH, D_MODEL, D_MODEL), D_MODEL ** -0.5 * beta)
    w_router = normal(ks[10], (DEPTH, D_MODEL, N_EXPERTS), D_MODEL ** -0.5)
    b_router = normal(ks[11], (DEPTH, N_EXPERTS), 0.01)
    w_gate_up = normal(ks[12], (DEPTH, N_EXPERTS, D_MODEL, 2 * D_FF), D_MODEL ** -0.5)
    b_gate_up = normal(ks[13], (DEPTH, N_EXPERTS, 2 * D_FF), 0.01)
    w_down = normal(ks[14], (DEPTH, N_EXPERTS, D_FF, D_MODEL), D_FF ** -0.5 * beta)
    b_down = normal(ks[15], (DEPTH, N_EXPERTS, D_MODEL), 0.01)
    ln_g = 1.0 + normal(ks[16], (DEPTH, 3, D_MODEL), 0.01)
    ln_b = normal(ks[17], (DEPTH, 3, D_MODEL), 0.01)
    return {"x": x, "mem": mem, "w_in": w_in, "rel_bias": rel_bias, "g_group_a": g_group_a,
            "g_group_b": g_group_b, "w_out": w_out, "w_q_mem": w_q_mem, "w_kv_mem": w_kv_mem,
            "w_o_mem": w_o_mem, "w_router": w_router, "b_router": b_router, "w_gate_up": w_gate_up,
            "b_gate_up": b_gate_up, "w_down": w_down, "b_down": b_down, "ln_g": ln_g, "ln_b": ln_b}


def reference(x, mem, w_in, rel_bias, g_group_a, g_group_b, w_out, w_q_mem, w_kv_mem, w_o_mem,
              w_router, b_router, w_gate_up, b_gate_up, w_down, b_down, ln_g, ln_b):
    for l in range(DEPTH):
        mix = hybrid_mixer(x, w_in[l], rel_bias[l], g_group_a[l], g_group_b[l], w_out[l])
        x = layer_norm(DEEPNORM_ALPHA * x + mix, ln_g[l, 0], ln_b[l, 0])
        xa = memory_cross_attention(x, mem, w_q_mem[l], w_kv_mem[l], w_o_mem[l])
        x = layer_norm(DEEPNORM_ALPHA * x + xa, ln_g[l, 1], ln_b[l, 1])
        ff = moe_ffn(x, w_router[l], b_router[l], w_gate_up[l], b_gate_up[l], w_down[l], b_down[l])
        x = layer_norm(DEEPNORM_ALPHA * x + ff, ln_g[l, 2], ln_b[l, 2])
    return x
```

```python
import numpy as np
from contextlib import ExitStack
import concourse.bass as bass
import concourse.mybir as mybir
from concourse.bass_utils import run_bass_kernel_spmd

F32 = mybir.dt.float32
BF16 = mybir.dt.bfloat16
I32 = mybir.dt.int32
AF = mybir.ActivationFunctionType
ALU = mybir.AluOpType

D = 1024
SEQ = 8192
NBLK = 64
NOWN = 16
NEXP = 32
CAP = 512
NRB = CAP // 128
NROWS = NEXP * CAP
ALPHA = 2.0 ** 0.25
LN_EPS = 1e-5
RMS_EPS = 1e-6
BIG = float(NROWS + 4096)


class Buf:
    __slots__ = ("name", "writer", "readers")

    def __init__(self, name):
        self.name = name
        self.writer = None
        self.readers = []


class Sched:
    ENGS = ("pe", "act", "dve", "pool", "sp")

    def __init__(self):
        self.epoch = 0
        self._reset()

    def _reset(self):
        self.prog = {e: [] for e in self.ENGS}
        self.cnt = {e: 0 for e in self.ENGS}
        self.dcnt = {}
        self.seen = {e: {} for e in self.ENGS}
        self.pending = {e: False for e in self.ENGS}

    def _need(self, e, tok, waits):
        if tok is None:
            return
        ep, kind, src, val = tok
        if ep != self.epoch:
            return
        if kind == "E" and src == e and e == "pe":
            return
        k = (kind, src)
        if self.seen[e].get(k, -1) >= val:
            return
        if waits.get(k, -1) < val:
            waits[k] = val

    def _emit_waits(self, e, waits):
        for (kind, src), val in waits.items():
            self.seen[e][(kind, src)] = val
            self.prog[e].append(("wait", kind, src, val))

    def _deps(self, e, reads, writes):
        waits = {}
        for b in reads:
            self._need(e, b.writer, waits)
        for b in writes:
            self._need(e, b.writer, waits)
            for t in b.readers:
                self._need(e, t, waits)
        self._emit_waits(e, waits)

    def _mark(self, tok, reads, writes):
        for b in reads:
            if b.readers and b.readers[0][0] != self.epoch:
                b.readers = []
            b.readers.append(tok)
        for b in writes:
            b.writer = tok
            b.readers = []

    def op(self, e, fn, reads=(), writes=(), sig=True):
        self._deps(e, reads, writes)
        idx = self.cnt[e]
        if sig:
            self.cnt[e] += 1
            self.prog[e].append(("op", fn, idx))
            self.pending[e] = False
        else:
            self.prog[e].append(("opn", fn, idx))
            self.pending[e] = True
        self._mark((self.epoch, "E", e, idx), reads, writes)

    def dma(self, e, key, fn, reads=(), writes=()):
        self._deps(e, reads, writes)
        v = self.dcnt.get(key, 0) + 16
        self.dcnt[key] = v
        self.prog[e].append(("dma", fn, key))
        self._mark((self.epoch, "D", key, v), reads, writes)

    def wait_all(self, e):
        waits = {}
        for e2 in self.ENGS:
            if self.cnt[e2] > 0:
                self._need(e, (self.epoch, "E", e2, self.cnt[e2] - 1), waits)
        for key, v in self.dcnt.items():
            self._need(e, (self.epoch, "D", key, v), waits)
        self._emit_waits(e, waits)

    def flush(self, nc, stack=None):
        assert not any(self.pending.values()), self.pending
        for e in self.ENGS:
            self.wait_all(e)
        ep = self.epoch
        CH = 2000
        with ExitStack() as sst:
            esem = {e: [sst.enter_context(nc.semaphore(f"s{ep}_{e}{i}")) for i in range(self.cnt[e] // CH + 1)]
                    for e in self.ENGS}
            dsem = {k: sst.enter_context(nc.semaphore(f"d{ep}_{k}")) for k in self.dcnt}
            prog = self.prog

            def run(e, eng):
                for it in prog[e]:
                    if it[0] == "wait":
                        _, kind, src, val = it
                        if kind == "E":
                            eng.wait_ge(esem[src][val // CH], val % CH + 1)
                        else:
                            eng.wait_ge(dsem[src], val)
                    elif it[0] == "op":
                        it[1](eng).then_inc(esem[e][it[2] // CH], 1)
                    elif it[0] == "opn":
                        it[1](eng)
                    else:
                        it[1](eng).then_inc(dsem[it[2]], 16)

            with nc.Block() as block:
                @block.tensor
                def _(eng):
                    run("pe", eng)

                @block.scalar
                def _(eng):
                    run("act", eng)

                @block.vector
                def _(eng):
                    run("dve", eng)

                @block.gpsimd
                def _(eng):
                    run("pool", eng)

                @block.sync
                def _(eng):
                    run("sp", eng)
            allsems = [x for l in esem.values() for x in l] + list(dsem.values())
            with nc.Block() as block:
                @block.sync
                def _(eng):
                    for x in allsems:
                        eng.sem_clear(x)
        self.epoch += 1
        self._reset()


def build_nc(dbg=False, stop_after=99):
    nc = bass.Bass("TRN2", target_bir_lowering=False)
    S = Sched()

    def din(name, shape, dt=F32):
        return nc.dram_tensor(name, list(shape), dt, kind="ExternalInput").ap()

    def dscr(name, shape, dt):
        return nc.dram_tensor(name, list(shape), dt).ap()

    xb = din("xb", [SEQ, D])
    xo = din("xo", [NOWN * 128, D])
    memb = din("memb", [256, D])
    w_in = din("w_in", [D, 3072])
    w_out = din("w_out", [D, D])
    w_q = din("w_q", [D, D])
    w_kv = din("w_kv", [D, 2 * D])
    w_o = din("w_o", [D, D])
    w_r = din("w_r", [D, NEXP])
    w_gu = din("w_gu", [NEXP, D, 2 * D]) if (stop_after >= 4 and stop_after != 15) else None
    w_d = din("w_d", [NEXP, D, D]) if (stop_after >= 4 and stop_after != 15) else None
    b_r = din("b_r", [1, NEXP])
    b_gu = din("b_gu", [NEXP * 16, 128])
    b_d = din("b_d", [NEXP, D])
    ln_g = din("ln_g", [3, D])
    ln_b = din("ln_b", [3, D])
    gga = din("gga", [1, 512])
    ggb = din("ggb", [1, 512])
    bmT = din("bmT", [128, 5 * 8 * 128])
    cmat = din("cmat", [128, 5 * 128])
    padm = din("padm", [128, 4])
    eoff = din("eoff", [128, NEXP])
    out = nc.dram_tensor("out", [NOWN * 128, D], F32, kind="ExternalOutput").ap()
    dbg_o = {}
    if dbg:
        for nm, shp in (("d_oa", [NOWN * 128, 512]), ("d_ob", [NOWN * 128, 512]), ("d_x1", [NOWN * 128, D]),
                        ("d_x2", [NOWN * 128, D]), ("d_lg", [NOWN * 128, NEXP]), ("d_idx", [NOWN * 128, 4]),
                        ("d_gate", [NOWN * 128, 4])):
            dbg_o[nm] = nc.dram_tensor(nm, shp, F32, kind="ExternalOutput").ap()

    KT_s = dscr("KT_s", [8, 128, SEQ], BF16)
    VA_s = dscr("VA_s", [NBLK, 128, 8 * 65], BF16)
    VB_s = dscr("VB_s", [NBLK, 128, 512], BF16)
    X2S = dscr("X2S", [NOWN * 128, D], F32)
    XS = dscr("XS", [NROWS + 128, D], BF16)
    YS = dscr("YS", [NROWS + 128, D], F32)
    b_KT = [Buf(f"KT{c}") for c in range(8)]
    b_VA, b_VB, b_X2S, b_XS, b_YS = Buf("VA"), Buf("VB"), Buf("X2S"), Buf("XS"), Buf("YS")

    with ExitStack() as top:
        def sb(name, shape, dt, stack=top):
            return stack.enter_context(nc.sbuf_tensor(name, list(shape), dt))

        def pst(name, shape, dt, stack):
            return stack.enter_context(nc.psum_tensor(name, list(shape), dt))

        def OP(e, name, reads, writes, *a, sig=True, **kw):
            S.op(e, lambda eng: getattr(eng, name)(*a, **kw), reads, writes, sig=sig)

        def DMA(e, key, reads, writes, o, i, **kw):
            S.dma(e, key, lambda eng: eng.dma_start(out=o, in_=i, **kw), reads, writes)

        cm = sb("cm", [128, 640], F32)
        cmb = sb("cmb", [128, 640], BF16)
        padt = sb("padt", [128, 4], F32)
        eofft = sb("eofft", [128, NEXP], F32)
        tril8 = sb("tril8", [128, 1024], F32)
        b_cm, b_cmb, b_padt, b_eoff, b_tril8 = Buf("cm"), Buf("cmb"), Buf("padt"), Buf("eoff"), Buf("tril8")
        DMA("sp", "c_cm", [], [b_cm], cm[:], cmat)
        DMA("sp", "c_padt", [], [b_padt], padt[:], padm)
        DMA("sp", "c_eoff", [], [b_eoff], eofft[:], eoff)
        OP("dve", "tensor_copy", [b_cm], [b_cmb], cmb[:], cm[:])
        for h in range(8):
            OP("pool", "tensor_copy", [b_cm], [b_tril8], tril8[:, h * 128:(h + 1) * 128], cm[:, 384:512])
        identF = cm[:, 0:128]
        identB = cmb[:, 0:128]
        triU = cmb[:, 128:256]
        comp = cmb[:, 256:384]
        onesB = cmb[:, 512:640]

        GT = sb("GT", [128, NOWN, 4], F32)
        IDX = sb("IDX", [128, NOWN, 4], I32)
        oas = ExitStack()
        OA = sb("OA", [128, NOWN, 512], BF16, oas)
        OB = sb("OB", [128, NOWN, 512], BF16, oas)
        qs = ExitStack()
        QT = sb("QT", [128, 4, NOWN * 128], BF16, qs)
        QTB = sb("QTB", [128, 4, NOWN, 256], BF16, qs)
        b_QT = [Buf(f"QT{c}") for c in range(8)]
        b_OA = [Buf(f"OA{j}") for j in range(NOWN)]
        b_OB = [Buf(f"OB{j}") for j in range(NOWN)]

        with ExitStack() as ph:
            win = sb("win", [128, 8, 3072], BF16, ph)
            b_win = Buf("win")
            for k in range(8):
                DMA("pool", "win", [], [b_win], win[:, k, :], w_in[k * 128:(k + 1) * 128, :])
            xs = [sb(f"xs{i}", [128, 4, D], F32, ph) for i in range(2)]
            b_xs = [Buf(f"xs{i}") for i in range(2)]
            xT = [sb(f"xT{i}", [128, 8, 512], BF16, ph) for i in range(2)]
            b_xT = [Buf(f"xT{i}") for i in range(2)]
            kst = [sb(f"kst{i}", [128, 8, 512], BF16, ph) for i in range(1)] * 2
            b_kst = [Buf("kst0")] * 2
            vsa = [sb(f"vsa{i}", [128, 4, 8, 65], BF16, ph) for i in range(1)] * 2
            vsb = [sb(f"vsb{i}", [128, 4, 512], BF16, ph) for i in range(1)] * 2
            b_vsa = [Buf("vsa0")] * 2
            b_vsb = [Buf("vsb0")] * 2
            OP("pool", "memset", [], [b_vsa[0]], vsa[0][:], 1.0)
            OP("pool", "memset", [], b_QT[4:8], QTB[:], 0.0)
            tp = [pst(f"tp{i}", [128, 1024], F32, ph) for i in range(2)]
            b_tp = [Buf(f"tp{i}") for i in range(2)]
            mm = [pst(f"mm{i}", [128, 512], F32, ph) for i in range(4)]
            b_mm = [Buf(f"mm{i}") for i in range(4)]
            cnt = {"tp": 0, "mm": 0, "ev": 0}

            def transpose_block(src_ap_fn, dstT, b_src, b_dst, col0):
                i = cnt["tp"] % 2
                cnt["tp"] += 1
                for k in range(8):
                    OP("pe", "transpose", [b_src, b_cm], [b_tp[i]], out=tp[i][:, k * 128:(k + 1) * 128],
                       in_=src_ap_fn(k), identity=identF, sig=(k == 7))
                eng = "act" if cnt["tp"] % 2 else "dve"
                if eng == "act":
                    OP("act", "copy", [b_tp[i]], [b_dst], out=dstT[:, :, col0:col0 + 128],
                       in_=tp[i][:].rearrange("p (k n) -> p k n", k=8))
                else:
                    OP("dve", "tensor_copy", [b_tp[i]], [b_dst], out=dstT[:, :, col0:col0 + 128],
                       in_=tp[i][:].rearrange("p (k n) -> p k n", k=8))

            def evac(dst_ap, src_ap, b_src, b_dst, scale=None):
                cnt["ev"] += 1
                if scale is not None:
                    OP("act", "mul", [b_src], [b_dst], out=dst_ap, in_=src_ap, mul=scale)
                elif cnt["ev"] % 2:
                    OP("act", "copy", [b_src], [b_dst], out=dst_ap, in_=src_ap)
                else:
                    OP("dve", "tensor_copy", [b_src], [b_dst], out=dst_ap, in_=src_ap)

            KCOLS = [512 + c * 128 for c in range(4)] + [2048 + c * 128 for c in range(4)]
            QCOLS = [c * 128 for c in range(4)] + [1536 + c * 128 for c in range(4)]
            for g in range(NBLK // 4):
                i2 = g % 2
                DMA("sp", f"xs{i2}", [], [b_xs[i2]], xs[i2][:],
                    xb[g * 512:(g + 1) * 512, :].rearrange("(a p) n -> p a n", p=128))
                for a in range(4):
                    transpose_block(lambda k, a=a, i2=i2: xs[i2][:, a, k * 128:(k + 1) * 128], xT[i2], b_xs[i2], b_xT[i2], a * 128)
                for c in range(8):
                    m = cnt["mm"] % 4
                    cnt["mm"] += 1
                    for k in range(8):
                        OP("pe", "matmul", [b_win, b_xT[i2]], [b_mm[m]], mm[m][:], lhsT=win[:, k, KCOLS[c]:KCOLS[c] + 128],
                           rhs=xT[i2][:, k, :], start=(k == 0), stop=(k == 7), sig=(k == 7))
                    evac(kst[i2][:, c, :], mm[m][:], b_mm[m], b_kst[i2])
                DMA("sp", "kst0", [b_kst[i2]], b_KT, KT_s[:, :, g * 512:(g + 1) * 512].rearrange("c p n -> p c n"), kst[i2][:])
                for a in range(4):
                    for vg in range(2):
                        m = cnt["mm"] % 4
                        cnt["mm"] += 1
                        c0 = 1024 if vg == 0 else 2560
                        for k in range(8):
                            OP("pe", "matmul", [b_win, b_xT[i2]], [b_mm[m]], mm[m][:], lhsT=xT[i2][:, k, a * 128:(a + 1) * 128],
                               rhs=win[:, k, c0:c0 + 512], start=(k == 0), stop=(k == 7), sig=(k == 7))
                        if vg == 0:
                            evac(vsa[i2][:, a, :, 0:64], mm[m][:].rearrange("p (h d) -> p h d", h=8), b_mm[m], b_vsa[i2])
                        else:
                            evac(vsb[i2][:, a, :], mm[m][:], b_mm[m], b_vsb[i2])
                DMA("sp", "vsa0", [b_vsa[i2]], [b_VA], VA_s[g * 4:(g + 1) * 4, :, :].rearrange("b p n -> p b n"),
                    vsa[i2][:].rearrange("p a h d -> p a (h d)"))
                DMA("sp", "vsb0", [b_vsb[i2]], [b_VB], VB_s[g * 4:(g + 1) * 4, :, :].rearrange("b p n -> p b n"), vsb[i2][:])
            for g in range(NOWN // 4):
                i2 = g % 2
                DMA("sp", f"xs{i2}", [], [b_xs[i2]], xs[i2][:],
                    xo[g * 512:(g + 1) * 512, :].rearrange("(a p) n -> p a n", p=128))
                for a in range(4):
                    transpose_block(lambda k, a=a, i2=i2: xs[i2][:, a, k * 128:(k + 1) * 128], xT[i2], b_xs[i2], b_xT[i2], a * 128)
                for c in range(8):
                    m = cnt["mm"] % 4
                    cnt["mm"] += 1
                    for k in range(8):
                        OP("pe", "matmul", [b_win, b_xT[i2]], [b_mm[m]], mm[m][:], lhsT=win[:, k, QCOLS[c]:QCOLS[c] + 128],
                           rhs=xT[i2][:, k, :], start=(k == 0), stop=(k == 7), sig=(k == 7))
                    if c < 4:
                        evac(QT[:, c, g * 512:(g + 1) * 512], mm[m][:], b_mm[m], b_QT[c], scale=0.125)
                    else:
                        OP("act", "mul", [b_mm[m]], [b_QT[c]], out=QTB[0:64, c - 4, 4 * g:4 * g + 4, 0:128],
                           in_=mm[m][0:64, :].rearrange("p (a q) -> p a q", a=4), mul=0.125)
                        OP("act", "mul", [b_mm[m]], [b_QT[c]], out=QTB[64:128, c - 4, 4 * g:4 * g + 4, 128:256],
                           in_=mm[m][64:128, :].rearrange("p (a q) -> p a q", a=4), mul=0.125)
            S.flush(nc, top)

        if stop_after <= 1:
            with ExitStack() as ph:
                t32 = sb("t32q", [128, 4, NOWN * 128], F32, ph)
                b_t32 = Buf("t32q")
                OP("dve", "tensor_copy", b_QT, [b_t32], out=t32[:], in_=QT[:])
                DMA("sp", "dbgq", [b_t32], [], dbg_o["d_ob"].rearrange("(c p) n -> p c n", p=128)[:, 0:4, 0:512], t32[:, :, 0:512])
                S.flush(nc, top)
            qs.close()
            oas.close()
            return nc
        with ExitStack() as ph:
            bm = sb("bm", [128, 5, 8, 128], BF16, ph)
            b_bm = Buf("bm")
            zt = sb("zt", [128, NRB * D], BF16, ph)
            b_zt = Buf("zt")
            OP("pool", "memset", [], [b_zt], zt[:], 0.0)
            for e in range(NEXP):
                DMA("sp", "zx", [b_zt], [b_XS], XS[e * CAP:(e + 1) * CAP, :].rearrange("(a p) n -> p a n", p=128),
                    zt[:].rearrange("p (a n) -> p a n", a=NRB))
            DMA("pool", "bm", [], [b_bm], bm[:].rearrange("p o h q -> p (o h q)"), bmT)
            kta = [sb(f"kta{i}", [128, SEQ], BF16, ph) for i in range(2)]
            va = [sb(f"va{i}", [128, NBLK, 130], BF16, ph) for i in range(2)]
            b_kta = [Buf(f"kta{i}") for i in range(2)]
            b_va = [Buf(f"va{i}") for i in range(2)]
            et = [sb(f"et{i}", [128, 10, 128], BF16, ph) for i in range(2)]
            b_et = [Buf(f"et{i}") for i in range(2)]
            rd = [sb(f"rd{i}", [128, 2], F32, ph) for i in range(2)]
            b_rd = [Buf(f"rd{i}") for i in range(2)]
            sps = [[pst(f"sp{i}_{t}", [128, 512], F32, ph) for t in range(3)] for i in range(2)]
            b_sps = [[Buf(f"sp{i}_{t}") for t in range(3)] for i in range(2)]
            ops_f = [pst(f"oa{i}", [128, 512], F32, ph) for i in range(2)]
            ops_ = [t[:, 0:130].rearrange("p (h d) -> p h d", h=2) for t in ops_f]
            b_ops = [Buf(f"oa{i}") for i in range(2)]
            it = 0
            for p in range(4):
                pi = p % 2
                DMA("sp", f"kta{pi}", [b_KT[p]], [b_kta[pi]], kta[pi][:], KT_s[p, :, :])
                for q4 in range(4):
                    DMA("sp", f"va{pi}", [b_VA], [b_va[pi]], va[pi][:, q4 * 16:(q4 + 1) * 16, :],
                        VA_s[q4 * 16:(q4 + 1) * 16, :, p * 130:(p + 1) * 130].rearrange("b p n -> p b n"))
                for j in range(NOWN):
                    i2 = it % 2
                    it += 1
                    L = 4 * j + 3
                    offs = [o for o in range(5) if L - 4 + o >= 0]
                    def slot(h, o):
                        return (h, o) if o < 4 else (2, h)
                    for h in range(2):
                        for o in offs:
                            Lk = L - 4 + o
                            bnk, col = slot(h, o)
                            dst = sps[i2][bnk][:, col * 128:(col + 1) * 128]
                            OP("pe", "matmul", [b_kta[pi], b_QT[p]], [b_sps[i2][bnk]], dst,
                               lhsT=kta[pi][64 * h:64 * h + 64, Lk * 128:(Lk + 1) * 128],
                               rhs=QT[64 * h:64 * h + 64, p, j * 128:(j + 1) * 128], start=True, stop=False, sig=False)
                            OP("pe", "matmul", [b_bm, b_cmb], [b_sps[i2][bnk]], dst,
                               lhsT=identB, rhs=bm[:, o, 2 * p + h, :], start=False, stop=True)
                    for h in range(2):
                        o_lo = offs[0]
                        n4 = len([o for o in offs if o < 4])
                        OP("act", "activation", [b_sps[i2][h]], [b_et[i2]],
                           out=et[i2][:, h * 5 + o_lo:h * 5 + o_lo + n4, :],
                           in_=sps[i2][h][:, o_lo * 128:(o_lo + n4) * 128].rearrange("p (o q) -> p o q", q=128), func=AF.Exp)
                    for h in range(2):
                        OP("act", "activation", [b_sps[i2][2]], [b_et[i2]], out=et[i2][:, h * 5 + 4, :],
                           in_=sps[i2][2][:, h * 128:(h + 1) * 128], func=AF.Exp)
                    if j == 0:
                        for h in range(2):
                            for o in offs:
                                Lk = L - 4 + o
                                if Lk < 3:
                                    OP("dve", "tensor_scalar", [b_et[i2], b_padt], [b_et[i2]], out=et[i2][:, h * 5 + o, :],
                                       in0=et[i2][:, h * 5 + o, :], scalar1=padt[:, Lk:Lk + 1], scalar2=None, op0=ALU.mult)
                    for h in range(2):
                        for n, o in enumerate(offs):
                            Lk = L - 4 + o
                            OP("pe", "matmul", [b_et[i2], b_va[pi]], [b_ops[i2]], ops_[i2][:, h, :],
                               lhsT=et[i2][:, h * 5 + o, :], rhs=va[pi][:, Lk, h * 65:(h + 1) * 65],
                               start=(n == 0), stop=(n == len(offs) - 1), sig=(n == len(offs) - 1))
                    OP("dve", "reciprocal", [b_ops[i2]], [b_rd[i2]], out=rd[i2][:], in_=ops_[i2][:, :, 64])
                    for h in range(2):
                        hh = 2 * p + h
                        OP("dve", "tensor_scalar", [b_ops[i2], b_rd[i2]], [b_OA[j]], out=OA[:, j, hh * 64:(hh + 1) * 64],
                           in0=ops_[i2][:, h, 0:64], scalar1=rd[i2][:, h:h + 1], scalar2=None, op0=ALU.mult)
            S.flush(nc, top)

        if stop_after == 15:
            with ExitStack() as ph:
                t32 = sb("t32", [128, NOWN, 512], F32, ph)
                b_t32 = Buf("t32")
                OP("dve", "tensor_copy", b_OA, [b_t32], out=t32[:], in_=OA[:])
                DMA("sp", "dbg", [b_t32], [], dbg_o["d_oa"].rearrange("(j p) n -> p j n", p=128), t32[:])
                S.flush(nc, top)
            qs.close()
            oas.close()
            return nc
        with ExitStack() as ph:
            ktb = sb("ktb", [128, 2, SEQ], BF16, ph)
            vb = sb("vb", [128, NBLK, 256], BF16, ph)
            b_ktb, b_vb = Buf("ktb"), Buf("vb")
            ee = [sb(f"ee{i}", [128, 512], F32, ph) for i in range(4)]
            ll = [sb(f"ll{i}", [128, 512], BF16, ph) for i in range(4)]
            ex = [sb(f"ex{i}", [128, 512], F32, ph) for i in range(3)]
            at = [sb(f"at{i}", [128, 512], BF16, ph) for i in range(3)]
            b_ee = [Buf(f"ee{i}") for i in range(4)]
            b_ll = [Buf(f"ll{i}") for i in range(4)]
            b_ex = [Buf(f"ex{i}") for i in range(3)]
            b_at = [Buf(f"at{i}") for i in range(3)]
            zp = [pst(f"zp{i}", [128, 512], F32, ph) for i in range(3)]
            b_zp = [Buf(f"zp{i}") for i in range(3)]
            cp = [pst(f"cp{i}", [128, 512], F32, ph) for i in range(2)]
            b_cp = [Buf(f"cp{i}") for i in range(2)]
            op_ = [pst(f"ob{i}", [128, 512], F32, ph) for i in range(2)]
            b_op = [Buf(f"ob{i}") for i in range(2)]
            import os
            SB_G = int(os.environ.get("SB_G", "2"))
            SB_J = int(os.environ.get("SB_J", str(NOWN)))
            SB_PIPE = int(os.environ.get("SB_PIPE", "1"))
            for G in range(SB_G):
                DMA("sp", "ktb", [b_KT[4 + 2 * G], b_KT[5 + 2 * G]], [b_ktb], ktb[:],
                    KT_s[4 + 2 * G:6 + 2 * G, :, :].rearrange("c p n -> p c n"))
                for q4 in range(4):
                    DMA("sp", "vb", [b_VB], [b_vb], vb[:, q4 * 16:(q4 + 1) * 16, :],
                        VB_s[q4 * 16:(q4 + 1) * 16, :, G * 256:(G + 1) * 256].rearrange("b p n -> p b n"))
                steps = []
                for j in range(SB_J):
                    L = 4 * j + 3
                    for n, Lk in enumerate(range(L, -1, -1)):
                        steps.append((j, Lk, n == 0, Lk == 0))

                def stA(n, G=G):
                    j, Lk, first, last = steps[n]
                    s3, s4 = n % 3, n % 4
                    for pl in range(2):
                        OP("pe", "matmul", [b_ktb, b_QT[4 + 2 * G + pl]], [b_zp[s3]], zp[s3][:, pl * 256:(pl + 1) * 256],
                           lhsT=ktb[:, pl, Lk * 128:(Lk + 1) * 128], rhs=QTB[:, 2 * G + pl, j, :],
                           start=True, stop=True, sig=(pl == 1))
                    OP("act", "activation", [b_zp[s3]], [b_ee[s4]], out=ee[s4][:], in_=zp[s3][:], func=AF.Exp)
                    if first:
                        OP("dve", "tensor_tensor", [b_ee[s4], b_tril8], [b_ee[s4]], out=ee[s4][:], in0=ee[s4][:],
                           in1=tril8[:, 0:512], op=ALU.mult)
                    elif Lk < 3:
                        OP("dve", "tensor_scalar", [b_ee[s4], b_padt], [b_ee[s4]], out=ee[s4][:], in0=ee[s4][:],
                           scalar1=padt[:, Lk:Lk + 1], scalar2=None, op0=ALU.mult)
                    OP("act", "activation", [b_ee[s4]], [b_ll[s4]], out=ll[s4][:], in_=ee[s4][:], func=AF.Ln, bias=1.0)

                def stU(n, G=G):
                    j, Lk, first, last = steps[n]
                    s4, s2, ci = n % 4, n % 2, j % 2
                    OP("pe", "matmul", [b_ll[s4], b_cmb], [b_cp[ci]], cp[ci][:], lhsT=triU, rhs=ll[s4][:],
                       start=first, stop=True, skip_group_check=True)
                    OP("act", "activation", [b_cp[ci]], [b_ex[s2]], out=ex[s2][:], in_=cp[ci][:], func=AF.Exp, scale=-1.0)

                def stC(n, G=G):
                    j, Lk, first, last = steps[n]
                    s4, ci = n % 4, j % 2
                    if not last:
                        OP("pe", "matmul", [b_ll[s4], b_cmb], [b_cp[ci]], cp[ci][:], lhsT=comp, rhs=ll[s4][:],
                           start=False, stop=True, skip_group_check=True)

                def stV(n, G=G):
                    j, Lk, first, last = steps[n]
                    s4, s2, ci = n % 4, n % 2, j % 2
                    OP("dve", "tensor_tensor", [b_ee[s4], b_ex[s2]], [b_at[s2]], out=at[s2][:], in0=ee[s4][:],
                       in1=ex[s2][:], op=ALU.mult)
                    for hl in range(4):
                        OP("pe", "matmul", [b_at[s2], b_vb], [b_op[ci]], op_[ci][:, hl * 64:(hl + 1) * 64],
                           lhsT=at[s2][:, hl * 128:(hl + 1) * 128], rhs=vb[:, Lk, hl * 64:(hl + 1) * 64],
                           start=(first and hl == 0), stop=last, skip_group_check=True, sig=(hl == 3))
                    if last:
                        OP("dve", "tensor_copy", [b_op[ci]], [b_OB[j]], out=OB[:, j, G * 256:(G + 1) * 256], in_=op_[ci][:, 0:256])

                NS = len(steps)
                for n in range(min(3, NS)):
                    stA(n)
                stU(0)
                for n in range(NS):
                    stC(n)
                    if n + 1 < NS:
                        stU(n + 1)
                    stV(n)
                    if n + 3 < NS:
                        stA(n + 3)
            S.flush(nc, top)

        qs.close()
        if dbg:
            with ExitStack() as ph:
                t32 = sb("t32", [128, NOWN, 512], F32, ph)
                b_t32 = Buf("t32")
                for nm, src, bsrc in (("d_oa", OA, b_OA), ("d_ob", OB, b_OB)):
                    OP("dve", "tensor_copy", bsrc, [b_t32], out=t32[:], in_=src[:])
                    DMA("sp", "dbg", [b_t32], [], dbg_o[nm].rearrange("(j p) n -> p j n", p=128), t32[:])
                S.flush(nc, top)

        if stop_after <= 2:
            oas.close()
            return nc
        b_GT = [Buf(f"GT{j}") for j in range(NOWN)]
        b_IDX = [Buf(f"IDX{j}") for j in range(NOWN)]
        with ExitStack() as ph:
            wout = sb("wout", [128, 8, D], BF16, ph)
            wq = sb("wq", [128, 8, D], BF16, ph)
            wo = sb("wo", [128, 8, D], BF16, ph)
            wr = sb("wr", [128, 8, NEXP], F32, ph)
            b_wout, b_wq, b_wo, b_wkv, b_wr = Buf("wout"), Buf("wq"), Buf("wo"), Buf("wkv"), Buf("wr")
            for k in range(8):
                DMA("pool", "w_wout", [], [b_wout], wout[:, k, :], w_out[k * 128:(k + 1) * 128, :])
                DMA("pool", "w_wq", [], [b_wq], wq[:, k, :], w_q[k * 128:(k + 1) * 128, :])
                DMA("pool", "w_wo", [], [b_wo], wo[:, k, :], w_o[k * 128:(k + 1) * 128, :])
            DMA("sp", "w_wr", [], [b_wr], wr[:], w_r.rearrange("(k p) n -> p k n", p=128))
            lng = sb("lng", [128, 2, D], F32, ph)
            lnb = sb("lnb", [128, 2, D], F32, ph)
            ggt = sb("ggt", [128, 2, 512], F32, ph)
            brt = sb("brt", [128, NEXP], F32, ph)
            b_vec = Buf("vec")
            for i in range(2):
                DMA("sp", "w_vec", [], [b_vec], lng[:, i, :], ln_g[i:i + 1, :].partition_broadcast(128))
                DMA("sp", "w_vec", [], [b_vec], lnb[:, i, :], ln_b[i:i + 1, :].partition_broadcast(128))
            DMA("sp", "w_vec", [], [b_vec], ggt[:, 0, :], gga.partition_broadcast(128))
            DMA("sp", "w_vec", [], [b_vec], ggt[:, 1, :], ggb.partition_broadcast(128))
            DMA("sp", "w_vec", [], [b_vec], brt[:], b_r.partition_broadcast(128))

            p3 = [pst(f"p3_{i}", [128, 512], F32, ph) for i in range(6)]
            b_p3 = [Buf(f"p3_{i}") for i in range(6)]
            pbt = pst("pbt", [128, 1024], BF16, ph)
            b_pbt = Buf("pbt")
            pcnt = {"i": 0}

            def bank():
                i = pcnt["i"] % 6
                pcnt["i"] += 1
                return p3[i], b_p3[i]

            KmT = sb("KmT", [128, 8, 256], BF16, ph)
            Vm = sb("Vm", [128, 2, D], BF16, ph)
            cntt = sb("cntt", [128, NEXP], F32, ph)
            kvs = ExitStack()
            wkv = sb("wkv", [128, 8, 2 * D], BF16, kvs)
            mems = sb("mems", [128, 2, D], F32, kvs)
            memT = sb("memT", [128, 8, 256], BF16, kvs)
            for k in range(8):
                DMA("pool", "w_wkv", [], [b_wkv], wkv[:, k, :], w_kv[k * 128:(k + 1) * 128, :])
            b_mems, b_memT, b_KmT, b_Vm = Buf("mems"), Buf("memT"), Buf("KmT"), Buf("Vm")
            DMA("sp", "w_mems", [], [b_mems], mems[:], memb.rearrange("(a p) n -> p a n", p=128))
            for a in range(2):
                for hf in range(2):
                    pb, bb = bank()
                    for k4 in range(4):
                        k = hf * 4 + k4
                        OP("pe", "transpose", [b_mems, b_cm], [bb], out=pb[:, k4 * 128:(k4 + 1) * 128],
                           in_=mems[:, a, k * 128:(k + 1) * 128], identity=identF, sig=(k4 == 3))
                    OP("dve", "tensor_copy", [bb], [b_memT], out=memT[:, hf * 4:hf * 4 + 4, a * 128:(a + 1) * 128],
                       in_=pb[:].rearrange("p (k n) -> p k n", k=4))
            for c in range(8):
                pb, bb = bank()
                for k in range(8):
                    OP("pe", "matmul", [b_wkv, b_memT], [bb], pb[:, 0:256], lhsT=wkv[:, k, c * 128:(c + 1) * 128],
                       rhs=memT[:, k, :], start=(k == 0), stop=(k == 7), sig=(k == 7))
                OP("act", "copy", [bb], [b_KmT], out=KmT[:, c, :], in_=pb[:, 0:256])
            for a in range(2):
                for hf in range(2):
                    pb, bb = bank()
                    for k in range(8):
                        OP("pe", "matmul", [b_wkv, b_memT], [bb], pb[:], lhsT=memT[:, k, a * 128:(a + 1) * 128],
                           rhs=wkv[:, k, D + hf * 512:D + (hf + 1) * 512], start=(k == 0), stop=(k == 7), sig=(k == 7))
                    OP("dve", "tensor_copy", [bb], [b_Vm], out=Vm[:, a, hf * 512:(hf + 1) * 512], in_=pb[:])

            S.flush(nc, top)
            kvs.close()
            def mk(name, shape, dt, n=2):
                return [sb(f"{name}{i}", shape, dt, ph) for i in range(n)], [Buf(f"{name}{i}") for i in range(n)]
            xoj, b_xoj = mk("xoj", [128, D], F32)
            cat, b_cat = mk("cat", [128, D], BF16)
            catT, b_catT = mk("catT", [128, 8, 128], BF16)
            yy, b_yy = mk("yy", [128, D], F32)
            x1, b_x1 = mk("x1", [128, D], F32)
            x1T, b_x1T = mk("x1T", [128, 8, 128], BF16)
            qT, b_qT = mk("qT", [128, 8, 128], BF16)
            eT, b_eT = mk("eT", [128, 8, 128], BF16)
            rdn, b_rdn = mk("rdn", [128, 512], F32)
            oT, b_oT = mk("oT", [128, 8, 128], BF16)
            x2, b_x2 = mk("x2", [128, D], F32)
            x2b, b_x2b = mk("x2b", [128, D], BF16)
            x2T, b_x2T = mk("x2T", [128, 8, 128], F32)
            junk, b_junk = mk("junk", [128, D], F32)
            sm, b_sm = mk("sm", [128, 64], F32)
            lg, b_lg = mk("lg", [128, 8, NEXP], F32)
            mselb, b_mselb = mk("mselb", [128, NEXP], BF16)
            idxf, b_idxf = mk("idxf", [128, 4], F32)
            b_cntt = Buf("cntt")
            OP("pool", "memset", [], [b_cntt], cntt[:], 0.0)

            def layer_norm(i2, src, b_src, dst, b_dst, li):
                s_ = sm[i2]
                bs = b_sm[i2]
                OP("dve", "memset", [], [bs], s_[:, 0:8], 0.0)
                OP("act", "activation", [b_src, bs], [b_junk[i2], bs], out=junk[i2][:], in_=src[:], func=AF.Copy,
                   accum_out=s_[:, 0:1])
                OP("act", "activation", [b_src, bs], [b_junk[i2], bs], out=junk[i2][:], in_=src[:], func=AF.Square,
                   accum_out=s_[:, 1:2])
                OP("dve", "tensor_scalar", [bs], [bs], out=s_[:, 2:4], in0=s_[:, 0:2], scalar1=1.0 / D, scalar2=None, op0=ALU.mult)
                OP("dve", "tensor_tensor", [bs], [bs], out=s_[:, 4:5], in0=s_[:, 2:3], in1=s_[:, 2:3], op=ALU.mult)
                OP("dve", "tensor_tensor", [bs], [bs], out=s_[:, 5:6], in0=s_[:, 3:4], in1=s_[:, 4:5], op=ALU.subtract)
                OP("act", "activation", [bs], [bs], out=s_[:, 6:7], in_=s_[:, 5:6], func=AF.Ln, bias=LN_EPS)
                OP("act", "activation", [bs], [bs], out=s_[:, 7:8], in_=s_[:, 6:7], func=AF.Exp, scale=-0.5)
                OP("dve", "tensor_scalar", [b_src, bs], [b_dst], out=dst[:], in0=src[:], scalar1=s_[:, 2:3], scalar2=s_[:, 7:8],
                   op0=ALU.subtract, op1=ALU.mult)
                OP("pool", "tensor_tensor", [b_dst, b_vec], [b_dst], out=dst[:], in0=dst[:], in1=lng[:, li, :], op=ALU.mult)
                OP("pool", "tensor_tensor", [b_dst, b_vec], [b_dst], out=dst[:], in0=dst[:], in1=lnb[:, li, :], op=ALU.add)

            for j in range(NOWN):
                i2 = j % 2
                s_ = sm[i2]
                bs = b_sm[i2]
                DMA("sp", f"xoj{i2}", [], [b_xoj[i2]], xoj[i2][:], xo[j * 128:(j + 1) * 128, :])
                OP("dve", "memset", [], [bs], s_[:, 16:24], 0.0)
                for gi, (O_, bO) in enumerate(((OA, b_OA[j]), (OB, b_OB[j]))):
                    OP("act", "activation", [bO, bs], [b_junk[i2], bs], out=junk[i2][:, 0:512], in_=O_[:, j, :], func=AF.Square,
                       accum_out=s_[:, 16 + gi:17 + gi])
                OP("act", "activation", [bs], [bs], out=s_[:, 18:20], in_=s_[:, 16:18], func=AF.Ln, scale=1.0 / 512, bias=RMS_EPS)
                OP("act", "activation", [bs], [bs], out=s_[:, 20:22], in_=s_[:, 18:20], func=AF.Exp, scale=-0.5)
                for gi, (O_, bO) in enumerate(((OA, b_OA[j]), (OB, b_OB[j]))):
                    OP("dve", "scalar_tensor_tensor", [bO, bs, b_vec], [b_cat[i2]], out=cat[i2][:, gi * 512:(gi + 1) * 512],
                       in0=O_[:, j, :], scalar=s_[:, 20 + gi:21 + gi], in1=ggt[:, gi, :], op0=ALU.mult, op1=ALU.mult)
                for k in range(8):
                    OP("pe", "transpose", [b_cat[i2], b_cmb], [b_pbt], out=pbt[:, k * 128:(k + 1) * 128],
                       in_=cat[i2][:, k * 128:(k + 1) * 128], identity=identB, sig=(k == 7))
                OP("dve", "tensor_copy", [b_pbt], [b_catT[i2]], out=catT[i2][:], in_=pbt[:].rearrange("p (k n) -> p k n", k=8))
                for hf in range(2):
                    pb, bb = bank()
                    for k in range(8):
                        OP("pe", "matmul", [b_catT[i2], b_wout], [bb], pb[:], lhsT=catT[i2][:, k, :],
                           rhs=wout[:, k, hf * 512:(hf + 1) * 512], start=(k == 0), stop=(k == 7), sig=(k == 7))
                    OP("dve", "scalar_tensor_tensor", [b_xoj[i2], bb], [b_yy[i2]], out=yy[i2][:, hf * 512:(hf + 1) * 512],
                       in0=xoj[i2][:, hf * 512:(hf + 1) * 512], scalar=ALPHA, in1=pb[:], op0=ALU.mult, op1=ALU.add)
                layer_norm(i2, yy[i2], b_yy[i2], x1[i2], b_x1[i2], 0)
                if dbg:
                    DMA("sp", "dbg_x1", [b_x1[i2]], [], dbg_o["d_x1"][j * 128:(j + 1) * 128, :], x1[i2][:])
                for hf in range(2):
                    pb, bb = bank()
                    for k4 in range(4):
                        k = hf * 4 + k4
                        OP("pe", "transpose", [b_x1[i2], b_cm], [bb], out=pb[:, k4 * 128:(k4 + 1) * 128],
                           in_=x1[i2][:, k * 128:(k + 1) * 128], identity=identF)
                    OP("act", "copy", [bb], [b_x1T[i2]], out=x1T[i2][:, hf * 4:hf * 4 + 4, :],
                       in_=pb[:].rearrange("p (k n) -> p k n", k=4))
                for hf in range(2):
                    pb, bb = bank()
                    for c4 in range(4):
                        c = hf * 4 + c4
                        for k in range(8):
                            OP("pe", "matmul", [b_wq, b_x1T[i2]], [bb], pb[:, c4 * 128:(c4 + 1) * 128],
                               lhsT=wq[:, k, c * 128:(c + 1) * 128], rhs=x1T[i2][:, k, :], start=(k == 0), stop=(k == 7), sig=(k == 7))
                    OP("act", "mul", [bb], [b_qT[i2]], out=qT[i2][:, hf * 4:hf * 4 + 4, :],
                       in_=pb[:].rearrange("p (k n) -> p k n", k=4), mul=1.0 / 16)
                for hf in range(2):
                    pb, bb = bank()
                    for t4 in range(4):
                        t = hf * 4 + t4
                        hd, mc = t // 2, t % 2
                        for dc in range(2):
                            OP("pe", "matmul", [b_KmT, b_qT[i2]], [bb], pb[:, t4 * 128:(t4 + 1) * 128],
                               lhsT=KmT[:, 2 * hd + dc, mc * 128:(mc + 1) * 128], rhs=qT[i2][:, 2 * hd + dc, :],
                               start=(dc == 0), stop=(dc == 1), sig=(dc == 1))
                    OP("act", "activation", [bb], [b_eT[i2]], out=eT[i2][:, hf * 4:hf * 4 + 4, :],
                       in_=pb[:].rearrange("p (k n) -> p k n", k=4), func=AF.Exp)
                pbd, bbd = bank()
                for hd in range(4):
                    for mc in range(2):
                        OP("pe", "matmul", [b_eT[i2], b_cmb], [bbd], pbd[:, hd * 128:(hd + 1) * 128], lhsT=onesB,
                           rhs=eT[i2][:, hd * 2 + mc, :], start=(mc == 0), stop=(mc == 1), sig=(mc == 1))
                OP("dve", "reciprocal", [bbd], [b_rdn[i2]], out=rdn[i2][:], in_=pbd[:])
                for hf in range(2):
                    pb, bb = bank()
                    for t4 in range(4):
                        t = hf * 4 + t4
                        hd, dc = t // 2, t % 2
                        for mc in range(2):
                            OP("pe", "matmul", [b_Vm, b_eT[i2]], [bb], pb[:, t4 * 128:(t4 + 1) * 128],
                               lhsT=Vm[:, mc, t * 128:(t + 1) * 128], rhs=eT[i2][:, hd * 2 + mc, :],
                               start=(mc == 0), stop=(mc == 1), sig=(mc == 1))
                    for t4 in range(4):
                        t = hf * 4 + t4
                        hd = t // 2
                        OP("dve", "tensor_tensor", [bb, b_rdn[i2]], [b_oT[i2]], out=oT[i2][:, t, :],
                           in0=pb[:, t4 * 128:(t4 + 1) * 128], in1=rdn[i2][:, hd * 128:(hd + 1) * 128], op=ALU.mult)
                for hf in range(2):
                    pb, bb = bank()
                    for k in range(8):
                        OP("pe", "matmul", [b_oT[i2], b_wo], [bb], pb[:], lhsT=oT[i2][:, k, :],
                           rhs=wo[:, k, hf * 512:(hf + 1) * 512], start=(k == 0), stop=(k == 7), sig=(k == 7))
                    OP("dve", "scalar_tensor_tensor", [b_x1[i2], bb], [b_yy[i2]], out=yy[i2][:, hf * 512:(hf + 1) * 512],
                       in0=x1[i2][:, hf * 512:(hf + 1) * 512], scalar=ALPHA, in1=pb[:], op0=ALU.mult, op1=ALU.add)
                layer_norm(i2, yy[i2], b_yy[i2], x2[i2], b_x2[i2], 1)
                DMA("sp", f"x2s{i2}", [b_x2[i2]], [b_X2S], X2S[j * 128:(j + 1) * 128, :], x2[i2][:])
                if dbg:
                    DMA("sp", "dbg_x2", [b_x2[i2]], [], dbg_o["d_x2"][j * 128:(j + 1) * 128, :], x2[i2][:])
                OP("act", "copy", [b_x2[i2]], [b_x2b[i2]], out=x2b[i2][:], in_=x2[i2][:])
                for hf in range(2):
                    pb, bb = bank()
                    for k4 in range(4):
                        k = hf * 4 + k4
                        OP("pe", "transpose", [b_x2[i2], b_cm], [bb], out=pb[:, k4 * 128:(k4 + 1) * 128],
                           in_=x2[i2][:, k * 128:(k + 1) * 128], identity=identF)
                    OP("dve", "tensor_copy", [bb], [b_x2T[i2]], out=x2T[i2][:, hf * 4:hf * 4 + 4, :],
                       in_=pb[:].rearrange("p (k n) -> p k n", k=4))
                pb, bb = bank()
                for k in range(8):
                    OP("pe", "matmul", [b_x2T[i2], b_wr], [bb], pb[:, 0:NEXP], lhsT=x2T[i2][:, k, :], rhs=wr[:, k, :],
                       start=(k == 0), stop=(k == 7), sig=(k == 7))
                W = lg[i2]
                bW = b_lg[i2]
                OP("dve", "tensor_tensor", [bb, b_vec], [bW], out=W[:, 0, :], in0=pb[:, 0:NEXP], in1=brt[:], op=ALU.add)
                if dbg:
                    DMA("sp", "dbg_lg", [bW], [], dbg_o["d_lg"][j * 128:(j + 1) * 128, :], W[:, 0, :])
                OP("dve", "max", [bW], [bs], out=s_[:, 32:40], in_=W[:, 0, :])
                OP("dve", "tensor_scalar", [bW, bs], [bW], out=W[:, 1, :], in0=W[:, 0, :], scalar1=s_[:, 35:36], scalar2=None,
                   op0=ALU.is_ge)
                OP("dve", "tensor_scalar", [bs], [bs], out=s_[:, 40:41], in0=s_[:, 32:33], scalar1=-1.0, scalar2=None, op0=ALU.mult)
                OP("act", "activation", [bW, bs], [bW], out=W[:, 2, :], in_=W[:, 0, :], func=AF.Exp, bias=s_[:, 40:41])
                OP("dve", "memset", [], [bs], s_[:, 41:42], 0.0)
                OP("dve", "tensor_tensor", [bW], [bW], out=W[:, 3, :], in0=W[:, 2, :], in1=W[:, 1, :], op=ALU.mult)
                OP("dve", "reduce_sum", [bW], [bs], out=s_[:, 41:42], in_=W[:, 3, :], axis=mybir.AxisListType.X)
                OP("dve", "reciprocal", [bs], [bs], out=s_[:, 42:43], in_=s_[:, 41:42])
                OP("dve", "tensor_scalar", [bW, bs], [bW], out=W[:, 3, :], in0=W[:, 3, :], scalar1=s_[:, 42:43], scalar2=None,
                   op0=ALU.mult)
                OP("dve", "tensor_copy", [bW], [b_mselb[i2]], out=mselb[i2][:], in_=W[:, 1, :])
                pb2, bb2 = bank()
                OP("pe", "matmul", [b_mselb[i2], b_cmb], [bb2], pb2[:, 0:NEXP], lhsT=comp, rhs=mselb[i2][:], start=True, stop=True)
                OP("pe", "matmul", [b_mselb[i2], b_cmb], [bb2], pb2[:, 64:64 + NEXP], lhsT=onesB, rhs=mselb[i2][:], start=True, stop=True)
                OP("dve", "tensor_tensor", [bb2, b_cntt], [bW], out=W[:, 4, :], in0=pb2[:, 0:NEXP], in1=cntt[:], op=ALU.add)
                OP("dve", "tensor_tensor", [bb2, b_cntt], [b_cntt], out=cntt[:], in0=pb2[:, 64:64 + NEXP], in1=cntt[:], op=ALU.add)
                OP("dve", "tensor_scalar", [bW], [bW], out=W[:, 5, :], in0=W[:, 4, :], scalar1=float(CAP), scalar2=BIG,
                   op0=ALU.is_ge, op1=ALU.mult)
                OP("dve", "tensor_tensor", [bW, b_eoff], [bW], out=W[:, 4, :], in0=W[:, 4, :], in1=eofft[:], op=ALU.add)
                OP("dve", "tensor_tensor", [bW], [bW], out=W[:, 4, :], in0=W[:, 4, :], in1=W[:, 5, :], op=ALU.add)
                OP("dve", "memset", [], [b_idxf[i2]], idxf[i2][:], 0.0)
                OP("dve", "memset", [], [b_GT[j]], GT[:, j, :], 0.0)
                for kk in range(4):
                    OP("dve", "tensor_scalar", [bW, bs], [bW], out=W[:, 6, :], in0=W[:, 0, :], scalar1=s_[:, 32 + kk:33 + kk],
                       scalar2=None, op0=ALU.is_equal)
                    OP("dve", "tensor_tensor", [bW], [bW], out=W[:, 7, :], in0=W[:, 6, :], in1=W[:, 4, :], op=ALU.mult)
                    OP("dve", "reduce_sum", [bW], [b_idxf[i2]], out=idxf[i2][:, kk:kk + 1], in_=W[:, 7, :], axis=mybir.AxisListType.X)
                    OP("dve", "tensor_tensor", [bW], [bW], out=W[:, 7, :], in0=W[:, 6, :], in1=W[:, 3, :], op=ALU.mult)
                    OP("dve", "reduce_sum", [bW], [b_GT[j]], out=GT[:, j, kk:kk + 1], in_=W[:, 7, :], axis=mybir.AxisListType.X)
                OP("dve", "tensor_scalar", [b_idxf[i2]], [bs], out=s_[:, 44:48], in0=idxf[i2][:], scalar1=float(NROWS), scalar2=None,
                   op0=ALU.is_lt)
                OP("dve", "tensor_tensor", [b_GT[j], bs], [b_GT[j]], out=GT[:, j, :], in0=GT[:, j, :], in1=s_[:, 44:48], op=ALU.mult)
                OP("dve", "tensor_scalar", [b_idxf[i2]], [b_idxf[i2]], out=idxf[i2][:], in0=idxf[i2][:], scalar1=float(NROWS), scalar2=None,
                   op0=ALU.min)
                OP("dve", "tensor_copy", [b_idxf[i2]], [b_IDX[j]], out=IDX[:, j, :], in_=idxf[i2][:])
                if dbg:
                    DMA("sp", "dbg_idx", [b_idxf[i2]], [], dbg_o["d_idx"][j * 128:(j + 1) * 128, :], idxf[i2][:])
                    DMA("sp", "dbg_gt", [b_GT[j]], [], dbg_o["d_gate"][j * 128:(j + 1) * 128, :], GT[:, j, :])
                for kk in range(4):
                    S.dma("pool", "scat", lambda eng, j=j, kk=kk, i2=i2: eng.indirect_dma_start(
                        out=XS[:, :], out_offset=bass.IndirectOffsetOnAxis(ap=IDX[:, j, kk:kk + 1], axis=0),
                        in_=x2b[i2][:, :], in_offset=None),
                        [b_x2b[i2], b_IDX[j]], [b_XS])
            S.flush(nc, top)

        oas.close()
        if stop_after <= 3:
            return nc
        with ExitStack() as ph:
            wgu = [sb(f"wgu{i}", [128, 8, 2 * D], BF16, ph) for i in range(2)]
            wd = [sb(f"wd{i}", [128, 8, D], BF16, ph) for i in range(2)]
            b_wgu = [[Buf(f"wgu{i}_{k}") for k in range(8)] for i in range(2)]
            b_wd = [[Buf(f"wd{i}_{k}") for k in range(8)] for i in range(2)]
            stg = [sb(f"stg{i}", [128, 2 * D], F32, ph) for i in range(3)]
            std = [sb(f"std{i}", [128, D], F32, ph) for i in range(2)]
            b_stg = [Buf(f"stg{i}") for i in range(3)]
            b_std = [Buf(f"std{i}") for i in range(2)]
            bdt = [sb(f"bdt{i}", [128, D], F32, ph) for i in range(1)] * 2
            b_bdt = [Buf("bdt0")] * 2
            xg = [sb(f"xg{i}", [128, NRB, D], BF16, ph) for i in range(1)] * 2
            b_xg = [Buf("xg0")] * 2
            xgT = [sb(f"xgT{i}", [128, 8, CAP], BF16, ph) for i in range(2)]
            b_xgT = [Buf(f"xgT{i}") for i in range(2)]
            hT = [sb(f"hT{i}", [128, 8, CAP], BF16, ph) for i in range(2)]
            b_hT = [Buf(f"hT{i}") for i in range(2)]
            gg = [sb(f"gg{i}", [128, CAP], F32, ph) for i in range(2)]
            sg = [sb(f"sg{i}", [128, CAP], F32, ph) for i in range(2)]
            uu = [sb(f"uu{i}", [128, CAP], F32, ph) for i in range(2)]
            b_gg = [Buf(f"gg{i}") for i in range(2)]
            b_sg = [Buf(f"sg{i}") for i in range(2)]
            b_uu = [Buf(f"uu{i}") for i in range(2)]
            ys = [sb(f"ys{i}", [128, D], F32, ph) for i in range(1)] * 2
            b_ys = [Buf("ys0")] * 2
            bgs = sb("bgs", [128, 4, 128], F32, ph)
            bgT = sb("bgT", [128, 512], F32, ph)
            b_bgs, b_bgT = Buf("bgs"), Buf("bgT")
            pt = [pst(f"pt{i}", [128, 1024], BF16, ph) for i in range(2)]
            b_pt = [Buf(f"pt{i}") for i in range(2)]
            pg = [pst(f"pg{i}", [128, 512], F32, ph) for i in range(4)]
            b_pg = [Buf(f"pg{i}") for i in range(4)]
            py = [pst(f"py{i}", [128, 512], F32, ph) for i in range(2)]
            b_py = [Buf(f"py{i}") for i in range(2)]
            DMA("sp", "bgs", [], [b_bgs], bgs[:], b_gu.rearrange("(a r) p -> r a p", r=128))
            for a in range(4):
                OP("pe", "transpose", [b_bgs, b_cm], [b_pg[0]], out=pg[0][:, a * 128:(a + 1) * 128], in_=bgs[:, a, :], identity=identF)
            OP("dve", "tensor_copy", [b_pg[0]], [b_bgT], out=bgT[:], in_=pg[0][:])
            bgT3 = bgT[:].rearrange("p (e c) -> p e c", c=16)
            OP("dve", "tensor_scalar", [b_bgT], [b_bgT], out=bgT3[:, :, 8:16], in0=bgT3[:, :, 8:16], scalar1=1.0, scalar2=None,
               op0=ALU.add)
            tc_ = {"t": 0, "g": 0, "y": 0}
            OP("pool", "memset", [], [b_ys[0]], ys[0][:], 0.0)
            DMA("sp", "ys0", [b_ys[0]], [b_YS], YS[NROWS:NROWS + 128, :], ys[0][:])

            def issue_gu(e, k):
                sl = k % 3
                DMA("sp", f"stg{sl}", [], [b_stg[sl]], stg[sl][:], w_gu[e, k * 128:(k + 1) * 128, :])

            def issue_d(e, k):
                sl = k % 2
                DMA("sp", f"std{sl}", [], [b_std[sl]], std[sl][:], w_d[e, k * 128:(k + 1) * 128, :])

            def cast_gu(e, k):
                sl, i = k % 3, e % 2
                if k % 2 == 0:
                    OP("act", "copy", [b_stg[sl]], [b_wgu[i][k]], out=wgu[i][:, k, :], in_=stg[sl][:])
                else:
                    OP("dve", "tensor_copy", [b_stg[sl]], [b_wgu[i][k]], out=wgu[i][:, k, :], in_=stg[sl][:])

            def cast_d(e, k):
                sl, i = k % 2, e % 2
                if k % 2 == 1:
                    OP("act", "copy", [b_std[sl]], [b_wd[i][k]], out=wd[i][:, k, :], in_=std[sl][:])
                else:
                    OP("dve", "tensor_copy", [b_std[sl]], [b_wd[i][k]], out=wd[i][:, k, :], in_=std[sl][:])

            def load_bd(e):
                i = e % 2
                DMA("sp", "bdt0", [], [b_bdt[i]], bdt[i][:], b_d[e:e + 1, :].partition_broadcast(128))

            def load_xg(e):
                i = e % 2
                DMA("sp", "xg0", [b_XS], [b_xg[i]], xg[i][:], XS[e * CAP:(e + 1) * CAP, :].rearrange("(a p) n -> p a n", p=128))

            def transposes(e):
                i = e % 2
                for a in range(NRB):
                    t = tc_["t"] % 2
                    tc_["t"] += 1
                    for k in range(8):
                        OP("pe", "transpose", [b_xg[i], b_cmb], [b_pt[t]], out=pt[t][:, k * 128:(k + 1) * 128],
                           in_=xg[i][:, a, k * 128:(k + 1) * 128], identity=identB, sig=(k == 7))
                    OP("act" if a % 2 else "dve", "copy" if a % 2 else "tensor_copy", [b_pt[t]], [b_xgT[i]],
                       out=xgT[i][:, :, a * 128:(a + 1) * 128], in_=pt[t][:].rearrange("p (k n) -> p k n", k=8))

            load_bd(0)
            load_xg(0)
            for k in range(8):
                issue_gu(0, k)
                cast_gu(0, k)
            for k in range(8):
                issue_d(0, k)
                cast_d(0, k)
            for e in range(NEXP):
                i = e % 2
                nxt = e + 1 < NEXP
                if nxt:
                    for k in range(3):
                        issue_gu(e + 1, k)
                    for k in range(2):
                        issue_d(e + 1, k)
                if e == 0:
                    transposes(0)
                    load_xg(1)
                for c in range(8):
                    g0 = tc_["g"] % 2
                    tc_["g"] += 1
                    pgg, pgu = pg[2 * g0], pg[2 * g0 + 1]
                    bgg_, bgu_ = b_pg[2 * g0], b_pg[2 * g0 + 1]
                    for k in range(8):
                        OP("pe", "matmul", [b_wgu[i][k], b_xgT[i]], [bgg_], pgg[:, 0:CAP], lhsT=wgu[i][:, k, c * 128:(c + 1) * 128],
                           rhs=xgT[i][:, k, :], start=(k == 0), stop=(k == 7), sig=(k == 7))
                    for k in range(8):
                        OP("pe", "matmul", [b_wgu[i][k], b_xgT[i]], [bgu_], pgu[:, 0:CAP], lhsT=wgu[i][:, k, D + c * 128:D + (c + 1) * 128],
                           rhs=xgT[i][:, k, :], start=(k == 0), stop=(k == 7), sig=(k == 7))
                    w2 = g0
                    OP("dve", "tensor_scalar", [bgg_, b_bgT], [b_gg[w2]], out=gg[w2][:], in0=pgg[:, 0:CAP],
                       scalar1=bgT[:, e * 16 + c:e * 16 + c + 1], scalar2=7.0, op0=ALU.add, op1=ALU.min)
                    OP("act", "activation", [b_gg[w2]], [b_sg[w2]], out=sg[w2][:], in_=gg[w2][:], func=AF.Silu, scale=1.702)
                    OP("act", "activation", [bgu_, b_bgT], [b_uu[w2]], out=uu[w2][:], in_=pgu[:, 0:CAP], func=AF.Identity,
                       bias=bgT[:, e * 16 + 8 + c:e * 16 + 8 + c + 1])
                    OP("dve", "tensor_scalar", [b_uu[w2]], [b_uu[w2]], out=uu[w2][:], in0=uu[w2][:], scalar1=-6.0, scalar2=8.0,
                       op0=ALU.max, op1=ALU.min)
                    OP("dve", "scalar_tensor_tensor", [b_uu[w2], b_sg[w2]], [b_hT[i]], out=hT[i][:, c, :], in0=uu[w2][:],
                       scalar=1.0 / 1.702, in1=sg[w2][:], op0=ALU.mult, op1=ALU.mult)
                    if nxt:
                        cast_gu(e + 1, c)
                        if c + 3 < 8:
                            issue_gu(e + 1, c + 3)
                        cast_d(e + 1, c)
                        if c + 2 < 8:
                            issue_d(e + 1, c + 2)
                if nxt:
                    transposes(e + 1)
                    if e + 2 < NEXP:
                        load_xg(e + 2)
                for a in range(NRB):
                    yi = tc_["y"] % 2
                    tc_["y"] += 1
                    for hf in range(2):
                        for k in range(8):
                            OP("pe", "matmul", [b_hT[i], b_wd[i][k]], [b_py[hf]], py[hf][:], lhsT=hT[i][:, k, a * 128:(a + 1) * 128],
                               rhs=wd[i][:, k, hf * 512:(hf + 1) * 512], start=(k == 0), stop=(k == 7), sig=(k == 7))
                        OP("dve", "tensor_tensor", [b_py[hf], b_bdt[i]], [b_ys[yi]], out=ys[yi][:, hf * 512:(hf + 1) * 512],
                           in0=py[hf][:], in1=bdt[i][:, hf * 512:(hf + 1) * 512], op=ALU.add)
                    DMA("sp", "ys0", [b_ys[yi]], [b_YS], YS[e * CAP + a * 128:e * CAP + (a + 1) * 128, :], ys[yi][:])
                if nxt:
                    load_bd(e + 1)
            S.flush(nc, top)

        with ExitStack() as ph:
            lng3 = sb("lng3", [128, D], F32, ph)
            lnb3 = sb("lnb3", [128, D], F32, ph)
            b_v3 = Buf("v3")
            DMA("sp", "v3", [], [b_v3], lng3[:], ln_g[2:3, :].partition_broadcast(128))
            DMA("sp", "v3", [], [b_v3], lnb3[:], ln_b[2:3, :].partition_broadcast(128))
            x2r = [sb(f"x2r{i}", [128, D], F32, ph) for i in range(2)]
            yk = [[sb(f"yk{i}_{k}", [128, D], F32, ph) for k in range(4)] for i in range(2)]
            acc = [sb(f"acc{i}", [128, D], F32, ph) for i in range(2)]
            res = [sb(f"res{i}", [128, D], F32, ph) for i in range(2)]
            jk5 = [sb(f"jk5{i}", [128, D], F32, ph) for i in range(2)]
            s5 = [sb(f"s5{i}", [128, 8], F32, ph) for i in range(2)]
            b_x2r = [Buf(f"x2r{i}") for i in range(2)]
            b_yk = [[Buf(f"yk{i}_{k}") for k in range(4)] for i in range(2)]
            b_acc = [Buf(f"acc{i}") for i in range(2)]
            b_res = [Buf(f"res{i}") for i in range(2)]
            b_jk5 = [Buf(f"jk5{i}") for i in range(2)]
            b_s5 = [Buf(f"s5{i}") for i in range(2)]
            for i in range(2):
                for k in range(4):
                    OP("pool", "memset", [], [b_yk[i][k]], yk[i][k][:], 0.0)
            for j in range(NOWN):
                i2 = j % 2
                DMA("sp", f"x2r{i2}", [b_X2S], [b_x2r[i2]], x2r[i2][:], X2S[j * 128:(j + 1) * 128, :])
                for kk in range(4):
                    S.dma("pool", f"gath{i2}{kk}", lambda eng, j=j, kk=kk, i2=i2: eng.indirect_dma_start(
                        out=yk[i2][kk][:, :], out_offset=None, in_=YS[:, :],
                        in_offset=bass.IndirectOffsetOnAxis(ap=IDX[:, j, kk:kk + 1], axis=0)), [b_YS, b_IDX[j]], [b_yk[i2][kk]])
                OP("act", "mul", [b_x2r[i2]], [b_acc[i2]], out=acc[i2][:], in_=x2r[i2][:], mul=ALPHA)
                for kk in range(4):
                    OP("dve", "scalar_tensor_tensor", [b_yk[i2][kk], b_GT[j], b_acc[i2]], [b_acc[i2]], out=acc[i2][:],
                       in0=yk[i2][kk][:], scalar=GT[:, j, kk:kk + 1], in1=acc[i2][:], op0=ALU.mult, op1=ALU.add)
                s_ = s5[i2]
                bs = b_s5[i2]
                OP("dve", "memset", [], [bs], s_[:], 0.0)
                OP("act", "activation", [b_acc[i2], bs], [b_jk5[i2], bs], out=jk5[i2][:], in_=acc[i2][:], func=AF.Copy, accum_out=s_[:, 0:1])
                OP("act", "activation", [b_acc[i2], bs], [b_jk5[i2], bs], out=jk5[i2][:], in_=acc[i2][:], func=AF.Square, accum_out=s_[:, 1:2])
                OP("dve", "tensor_scalar", [bs], [bs], out=s_[:, 2:4], in0=s_[:, 0:2], scalar1=1.0 / D, scalar2=None, op0=ALU.mult)
                OP("dve", "tensor_tensor", [bs], [bs], out=s_[:, 4:5], in0=s_[:, 2:3], in1=s_[:, 2:3], op=ALU.mult)
                OP("dve", "tensor_tensor", [bs], [bs], out=s_[:, 5:6], in0=s_[:, 3:4], in1=s_[:, 4:5], op=ALU.subtract)
                OP("act", "activation", [bs], [bs], out=s_[:, 6:7], in_=s_[:, 5:6], func=AF.Ln, bias=LN_EPS)
                OP("act", "activation", [bs], [bs], out=s_[:, 7:8], in_=s_[:, 6:7], func=AF.Exp, scale=-0.5)
                OP("dve", "tensor_scalar", [b_acc[i2], bs], [b_res[i2]], out=res[i2][:], in0=acc[i2][:], scalar1=s_[:, 2:3],
                   scalar2=s_[:, 7:8], op0=ALU.subtract, op1=ALU.mult)
                OP("pool", "tensor_tensor", [b_res[i2], b_v3], [b_res[i2]], out=res[i2][:], in0=res[i2][:], in1=lng3[:], op=ALU.mult)
                OP("pool", "tensor_tensor", [b_res[i2], b_v3], [b_res[i2]], out=res[i2][:], in0=res[i2][:], in1=lnb3[:], op=ALU.add)
                DMA("sp", f"out{i2}", [b_res[i2]], [], out[j * 128:(j + 1) * 128, :], res[i2][:])
            S.flush(nc, top)

    return nc


def _const_mats():
    idx = np.arange(128)
    ident = np.eye(128, dtype=np.float32)
    triU = (idx[:, None] >= idx[None, :]).astype(np.float32)
    comp = (idx[:, None] < idx[None, :]).astype(np.float32)
    tril = (idx[:, None] < idx[None, :]).astype(np.float32)
    ones = np.ones((128, 128), np.float32)
    return np.concatenate([ident, triU, comp, tril, ones], axis=1)


def _bias_mask(rel_bias):
    kl = np.arange(640)
    q = np.arange(128)
    rel = 512 + q[None, :] - kl[:, None]
    ridx = np.clip(rel, -128, 128) + 128
    cq = 8 + q // 64
    ck = kl // 64
    vis = (ck[:, None] >= cq[None, :] - 8) & (ck[:, None] <= cq[None, :])
    bm = rel_bias[:, ridx]
    bm = np.where(vis[None], bm, np.float32(-30000.0)).astype(np.float32)
    bm = bm.reshape(8, 5, 128, 128).transpose(2, 1, 0, 3)
    return np.ascontiguousarray(bm.reshape(128, 5 * 8 * 128))


def make_in_maps(inputs, cores=range(8)):
    x = np.asarray(inputs["x"], np.float32)
    f = lambda k: np.ascontiguousarray(np.asarray(inputs[k], np.float32)[0])
    shared = {
        "w_in": f("w_in"), "w_out": f("w_out"), "w_q": f("w_q_mem"), "w_kv": f("w_kv_mem"), "w_o": f("w_o_mem"),
        "w_r": f("w_router"), "w_gu": f("w_gate_up"), "w_d": f("w_down"),
        "b_r": f("b_router").reshape(1, NEXP), "b_gu": f("b_gate_up").reshape(NEXP * 16, 128), "b_d": f("b_down"),
        "ln_g": f("ln_g"), "ln_b": f("ln_b"), "gga": f("g_group_a").reshape(1, 512), "ggb": f("g_group_b").reshape(1, 512),
        "bmT": _bias_mask(f("rel_bias")), "cmat": _const_mats(),
        "eoff": np.ascontiguousarray(np.broadcast_to((np.arange(NEXP) * CAP).astype(np.float32)[None, :], (128, NEXP))),
    }
    maps = []
    for c in cores:
        b, r = c // 4, c % 4
        sh = 3 - r
        xbs = np.zeros((SEQ, D), np.float32)
        xbs[sh * 128:] = x[b, :SEQ - sh * 128]
        xoo = np.ascontiguousarray(x[b].reshape(NBLK, 128, D)[r::4].reshape(NOWN * 128, D))
        padm = np.zeros((128, 4), np.float32)
        for Lk in range(4):
            padm[:, Lk] = 1.0 if Lk >= sh else 0.0
        m = dict(shared)
        m.update({"xb": xbs, "xo": xoo, "memb": np.ascontiguousarray(np.asarray(inputs["mem"], np.float32)[b]), "padm": padm})
        maps.append(m)
    return maps


_NC_CACHE = {}


def kernel(**inputs):
    if "nc" not in _NC_CACHE:
        _NC_CACHE["nc"] = build_nc()
    nc = _NC_CACHE["nc"]
    maps = make_in_maps(inputs)
    res = run_bass_kernel_spmd(nc, maps, core_ids=list(range(8)))
    outp = np.zeros((2, SEQ, D), np.float32)
    for c in range(8):
        b, r = c // 4, c % 4
        o = np.asarray(res.results[c]["out"]).reshape(NOWN, 128, D)
        outp[b].reshape(NBLK, 128, D)[r::4] = o
    return outp
```

```python
import numpy as np
from contextlib import ExitStack
import concourse.bass as bass
import concourse.mybir as mybir
from concourse.bass_utils import run_bass_kernel_spmd

F32 = mybir.dt.float32
BF16 = mybir.dt.bfloat16
I32 = mybir.dt.int32
AF = mybir.ActivationFunctionType
ALU = mybir.AluOpType

D = 1024
SEQ = 8192
NBLK = 64
NOWN = 16
NEXP = 32
CAP = 512
NRB = CAP // 128
NROWS = NEXP * CAP
ALPHA = 2.0 ** 0.25
LN_EPS = 1e-5
RMS_EPS = 1e-6
BIG = float(NROWS + 4096)


class Buf:
    __slots__ = ("name", "writer", "readers")

    def __init__(self, name):
        self.name = name
        self.writer = None
        self.readers = []


class Sched:
    ENGS = ("pe", "act", "dve", "pool", "sp")

    def __init__(self):
        self.epoch = 0
        self._reset()

    def _reset(self):
        self.prog = {e: [] for e in self.ENGS}
        self.cnt = {e: 0 for e in self.ENGS}
        self.dcnt = {}
        self.seen = {e: {} for e in self.ENGS}
        self.pending = {e: False for e in self.ENGS}

    def _need(self, e, tok, waits):
        if tok is None:
            return
        ep, kind, src, val = tok
        if ep != self.epoch:
            return
        if kind == "E" and src == e and e == "pe":
            return
        k = (kind, src)
        if self.seen[e].get(k, -1) >= val:
            return
        if waits.get(k, -1) < val:
            waits[k] = val

    def _emit_waits(self, e, waits):
        for (kind, src), val in waits.items():
            self.seen[e][(kind, src)] = val
            self.prog[e].append(("wait", kind, src, val))

    def _deps(self, e, reads, writes):
        waits = {}
        for b in reads:
            self._need(e, b.writer, waits)
        for b in writes:
            self._need(e, b.writer, waits)
            for t in b.readers:
                self._need(e, t, waits)
        self._emit_waits(e, waits)

    def _mark(self, tok, reads, writes):
        for b in reads:
            if b.readers and b.readers[0][0] != self.epoch:
                b.readers = []
            b.readers.append(tok)
        for b in writes:
            b.writer = tok
            b.readers = []

    def op(self, e, fn, reads=(), writes=(), sig=True):
        self._deps(e, reads, writes)
        idx = self.cnt[e]
        if sig:
            self.cnt[e] += 1
            self.prog[e].append(("op", fn, idx))
            self.pending[e] = False
        else:
            self.prog[e].append(("opn", fn, idx))
            self.pending[e] = True
        self._mark((self.epoch, "E", e, idx), reads, writes)

    def dma(self, e, key, fn, reads=(), writes=()):
        self._deps(e, reads, writes)
        v = self.dcnt.get(key, 0) + 16
        self.dcnt[key] = v
        self.prog[e].append(("dma", fn, key))
        self._mark((self.epoch, "D", key, v), reads, writes)

    def wait_all(self, e):
        waits = {}
        for e2 in self.ENGS:
            if self.cnt[e2] > 0:
                self._need(e, (self.epoch, "E", e2, self.cnt[e2] - 1), waits)
        for key, v in self.dcnt.items():
            self._need(e, (self.epoch, "D", key, v), waits)
        self._emit_waits(e, waits)

    def flush(self, nc, stack=None):
        assert not any(self.pending.values()), self.pending
        for e in self.ENGS:
            self.wait_all(e)
        ep = self.epoch
        CH = 2000
        with ExitStack() as sst:
            esem = {e: [sst.enter_context(nc.semaphore(f"s{ep}_{e}{i}")) for i in range(self.cnt[e] // CH + 1)]
                    for e in self.ENGS}
            dsem = {k: sst.enter_context(nc.semaphore(f"d{ep}_{k}")) for k in self.dcnt}
            prog = self.prog

            def run(e, eng):
                for it in prog[e]:
                    if it[0] == "wait":
                        _, kind, src, val = it
                        if kind == "E":
                            eng.wait_ge(esem[src][val // CH], val % CH + 1)
                        else:
                            eng.wait_ge(dsem[src], val)
                    elif it[0] == "op":
                        it[1](eng).then_inc(esem[e][it[2] // CH], 1)
                    elif it[0] == "opn":
                        it[1](eng)
                    else:
                        it[1](eng).then_inc(dsem[it[2]], 16)

            with nc.Block() as block:
                @block.tensor
                def _(eng):
                    run("pe", eng)

                @block.scalar
                def _(eng):
                    run("act", eng)

                @block.vector
                def _(eng):
                    run("dve", eng)

                @block.gpsimd
                def _(eng):
                    run("pool", eng)

                @block.sync
                def _(eng):
                    run("sp", eng)
            allsems = [x for l in esem.values() for x in l] + list(dsem.values())
            with nc.Block() as block:
                @block.sync
                def _(eng):
                    for x in allsems:
                        eng.sem_clear(x)
        self.epoch += 1
        self._reset()


def build_nc(dbg=False, stop_after=99):
    nc = bass.Bass("TRN2", target_bir_lowering=False)
    S = Sched()

    def din(name, shape, dt=F32):
        return nc.dram_tensor(name, list(shape), dt, kind="ExternalInput").ap()

    def dscr(name, shape, dt):
        return nc.dram_tensor(name, list(shape), dt).ap()

    xb = din("xb", [SEQ, D])
    xo = din("xo", [NOWN * 128, D])
    memb = din("memb", [256, D])
    w_in = din("w_in", [D, 3072])
    w_out = din("w_out", [D, D])
    w_q = din("w_q", [D, D])
    w_kv = din("w_kv", [D, 2 * D])
    w_o = din("w_o", [D, D])
    w_r = din("w_r", [D, NEXP])
    w_gu = din("w_gu", [NEXP, D, 2 * D]) if (stop_after >= 4 and stop_after != 15) else None
    w_d = din("w_d", [NEXP, D, D]) if (stop_after >= 4 and stop_after != 15) else None
    b_r = din("b_r", [1, NEXP])
    b_gu = din("b_gu", [NEXP * 16, 128])
    b_d = din("b_d", [NEXP, D])
    ln_g = din("ln_g", [3, D])
    ln_b = din("ln_b", [3, D])
    gga = din("gga", [1, 512])
    ggb = din("ggb", [1, 512])
    bmT = din("bmT", [128, 5 * 8 * 128])
    cmat = din("cmat", [128, 5 * 128])
    padm = din("padm", [128, 4])
    eoff = din("eoff", [128, NEXP])
    out = nc.dram_tensor("out", [NOWN * 128, D], F32, kind="ExternalOutput").ap()
    dbg_o = {}
    if dbg:
        for nm, shp in (("d_oa", [NOWN * 128, 512]), ("d_ob", [NOWN * 128, 512]), ("d_x1", [NOWN * 128, D]),
                        ("d_x2", [NOWN * 128, D]), ("d_lg", [NOWN * 128, NEXP]), ("d_idx", [NOWN * 128, 4]),
                        ("d_gate", [NOWN * 128, 4])):
            dbg_o[nm] = nc.dram_tensor(nm, shp, F32, kind="ExternalOutput").ap()

    KT_s = dscr("KT_s", [8, 128, SEQ], BF16)
    VA_s = dscr("VA_s", [NBLK, 128, 8 * 65], BF16)
    VB_s = dscr("VB_s", [NBLK, 128, 512], BF16)
    X2S = dscr("X2S", [NOWN * 128, D], F32)
    XS = dscr("XS", [NROWS + 128, D], BF16)
    YS = dscr("YS", [NROWS + 128, D], F32)
    b_KT = [Buf(f"KT{c}") for c in range(8)]
    b_VA, b_VB, b_X2S, b_XS, b_YS = Buf("VA"), Buf("VB"), Buf("X2S"), Buf("XS"), Buf("YS")

    with ExitStack() as top:
        def sb(name, shape, dt, stack=top):
            return stack.enter_context(nc.sbuf_tensor(name, list(shape), dt))

        def pst(name, shape, dt, stack):
            return stack.enter_context(nc.psum_tensor(name, list(shape), dt))

        def OP(e, name, reads, writes, *a, sig=True, **kw):
            S.op(e, lambda eng: getattr(eng, name)(*a, **kw), reads, writes, sig=sig)

        def DMA(e, key, reads, writes, o, i, **kw):
            S.dma(e, key, lambda eng: eng.dma_start(out=o, in_=i, **kw), reads, writes)

        cm = sb("cm", [128, 640], F32)
        cmb = sb("cmb", [128, 640], BF16)
        padt = sb("padt", [128, 4], F32)
        eofft = sb("eofft", [128, NEXP], F32)
        tril8 = sb("tril8", [128, 1024], F32)
        b_cm, b_cmb, b_padt, b_eoff, b_tril8 = Buf("cm"), Buf("cmb"), Buf("padt"), Buf("eoff"), Buf("tril8")
        DMA("sp", "c_cm", [], [b_cm], cm[:], cmat)
        DMA("sp", "c_padt", [], [b_padt], padt[:], padm)
        DMA("sp", "c_eoff", [], [b_eoff], eofft[:], eoff)
        OP("dve", "tensor_copy", [b_cm], [b_cmb], cmb[:], cm[:])
        for h in range(8):
            OP("pool", "tensor_copy", [b_cm], [b_tril8], tril8[:, h * 128:(h + 1) * 128], cm[:, 384:512])
        identF = cm[:, 0:128]
        identB = cmb[:, 0:128]
        triU = cmb[:, 128:256]
        comp = cmb[:, 256:384]
        onesB = cmb[:, 512:640]

        GT = sb("GT", [128, NOWN, 4], F32)
        IDX = sb("IDX", [128, NOWN, 4], I32)
        oas = ExitStack()
        OA = sb("OA", [128, NOWN, 512], BF16, oas)
        OB = sb("OB", [128, NOWN, 512], BF16, oas)
        qs = ExitStack()
        QT = sb("QT", [128, 4, NOWN * 128], BF16, qs)
        QTB = sb("QTB", [128, 4, NOWN, 256], BF16, qs)
        b_QT = [Buf(f"QT{c}") for c in range(8)]
        b_OA = [Buf(f"OA{j}") for j in range(NOWN)]
        b_OB = [Buf(f"OB{j}") for j in range(NOWN)]

        with ExitStack() as ph:
            win = sb("win", [128, 8, 3072], BF16, ph)
            b_win = Buf("win")
            for k in range(8):
                DMA("pool", "win", [], [b_win], win[:, k, :], w_in[k * 128:(k + 1) * 128, :])
            xs = [sb(f"xs{i}", [128, 4, D], F32, ph) for i in range(2)]
            b_xs = [Buf(f"xs{i}") for i in range(2)]
            xT = [sb(f"xT{i}", [128, 8, 512], BF16, ph) for i in range(2)]
            b_xT = [Buf(f"xT{i}") for i in range(2)]
            kst = [sb(f"kst{i}", [128, 8, 512], BF16, ph) for i in range(1)] * 2
            b_kst = [Buf("kst0")] * 2
            vsa = [sb(f"vsa{i}", [128, 4, 8, 65], BF16, ph) for i in range(1)] * 2
            vsb = [sb(f"vsb{i}", [128, 4, 512], BF16, ph) for i in range(1)] * 2
            b_vsa = [Buf("vsa0")] * 2
            b_vsb = [Buf("vsb0")] * 2
            OP("pool", "memset", [], [b_vsa[0]], vsa[0][:], 1.0)
            OP("pool", "memset", [], b_QT[4:8], QTB[:], 0.0)
            tp = [pst(f"tp{i}", [128, 1024], F32, ph) for i in range(2)]
            b_tp = [Buf(f"tp{i}") for i in range(2)]
            mm = [pst(f"mm{i}", [128, 512], F32, ph) for i in range(4)]
            b_mm = [Buf(f"mm{i}") for i in range(4)]
            cnt = {"tp": 0, "mm": 0, "ev": 0}

            def transpose_block(src_ap_fn, dstT, b_src, b_dst, col0):
                i = cnt["tp"] % 2
                cnt["tp"] += 1
                for k in range(8):
                    OP("pe", "transpose", [b_src, b_cm], [b_tp[i]], out=tp[i][:, k * 128:(k + 1) * 128],
                       in_=src_ap_fn(k), identity=identF, sig=(k == 7))
                eng = "act" if cnt["tp"] % 2 else "dve"
                if eng == "act":
                    OP("act", "copy", [b_tp[i]], [b_dst], out=dstT[:, :, col0:col0 + 128],
                       in_=tp[i][:].rearrange("p (k n) -> p k n", k=8))
                else:
                    OP("dve", "tensor_copy", [b_tp[i]], [b_dst], out=dstT[:, :, col0:col0 + 128],
                       in_=tp[i][:].rearrange("p (k n) -> p k n", k=8))

            def evac(dst_ap, src_ap, b_src, b_dst, scale=None):
                cnt["ev"] += 1
                if scale is not None:
                    OP("act", "mul", [b_src], [b_dst], out=dst_ap, in_=src_ap, mul=scale)
                elif cnt["ev"] % 2:
                    OP("act", "copy", [b_src], [b_dst], out=dst_ap, in_=src_ap)
                else:
                    OP("dve", "tensor_copy", [b_src], [b_dst], out=dst_ap, in_=src_ap)

            KCOLS = [512 + c * 128 for c in range(4)] + [2048 + c * 128 for c in range(4)]
            QCOLS = [c * 128 for c in range(4)] + [1536 + c * 128 for c in range(4)]
            for g in range(NBLK // 4):
                i2 = g % 2
                DMA("sp", f"xs{i2}", [], [b_xs[i2]], xs[i2][:],
                    xb[g * 512:(g + 1) * 512, :].rearrange("(a p) n -> p a n", p=128))
                for a in range(4):
                    transpose_block(lambda k, a=a, i2=i2: xs[i2][:, a, k * 128:(k + 1) * 128], xT[i2], b_xs[i2], b_xT[i2], a * 128)
                for c in range(8):
                    m = cnt["mm"] % 4
                    cnt["mm"] += 1
                    for k in range(8):
                        OP("pe", "matmul", [b_win, b_xT[i2]], [b_mm[m]], mm[m][:], lhsT=win[:, k, KCOLS[c]:KCOLS[c] + 128],
                           rhs=xT[i2][:, k, :], start=(k == 0), stop=(k == 7), sig=(k == 7))
                    evac(kst[i2][:, c, :], mm[m][:], b_mm[m], b_kst[i2])
                DMA("sp", "kst0", [b_kst[i2]], b_KT, KT_s[:, :, g * 512:(g + 1) * 512].rearrange("c p n -> p c n"), kst[i2][:])
                for a in range(4):
                    for vg in range(2):
                        m = cnt["mm"] % 4
                        cnt["mm"] += 1
                        c0 = 1024 if vg == 0 else 2560
                        for k in range(8):
                            OP("pe", "matmul", [b_win, b_xT[i2]], [b_mm[m]], mm[m][:], lhsT=xT[i2][:, k, a * 128:(a + 1) * 128],
                               rhs=win[:, k, c0:c0 + 512], start=(k == 0), stop=(k == 7), sig=(k == 7))
                        if vg == 0:
                            evac(vsa[i2][:, a, :, 0:64], mm[m][:].rearrange("p (h d) -> p h d", h=8), b_mm[m], b_vsa[i2])
                        else:
                            evac(vsb[i2][:, a, :], mm[m][:], b_mm[m], b_vsb[i2])
                DMA("sp", "vsa0", [b_vsa[i2]], [b_VA], VA_s[g * 4:(g + 1) * 4, :, :].rearrange("b p n -> p b n"),
                    vsa[i2][:].rearrange("p a h d -> p a (h d)"))
                DMA("sp", "vsb0", [b_vsb[i2]], [b_VB], VB_s[g * 4:(g + 1) * 4, :, :].rearrange("b p n -> p b n"), vsb[i2][:])
            for g in range(NOWN // 4):
                i2 = g % 2
                DMA("sp", f"xs{i2}", [], [b_xs[i2]], xs[i2][:],
                    xo[g * 512:(g + 1) * 512, :].rearrange("(a p) n -> p a n", p=128))
                for a in range(4):
                    transpose_block(lambda k, a=a, i2=i2: xs[i2][:, a, k * 128:(k + 1) * 128], xT[i2], b_xs[i2], b_xT[i2], a * 128)
                for c in range(8):
                    m = cnt["mm"] % 4
                    cnt["mm"] += 1
                    for k in range(8):
                        OP("pe", "matmul", [b_win, b_xT[i2]], [b_mm[m]], mm[m][:], lhsT=win[:, k, QCOLS[c]:QCOLS[c] + 128],
                           rhs=xT[i2][:, k, :], start=(k == 0), stop=(k == 7), sig=(k == 7))
                    if c < 4:
                        evac(QT[:, c, g * 512:(g + 1) * 512], mm[m][:], b_mm[m], b_QT[c], scale=0.125)
                    else:
                        OP("act", "mul", [b_mm[m]], [b_QT[c]], out=QTB[0:64, c - 4, 4 * g:4 * g + 4, 0:128],
                           in_=mm[m][0:64, :].rearrange("p (a q) -> p a q", a=4), mul=0.125)
                        OP("act", "mul", [b_mm[m]], [b_QT[c]], out=QTB[64:128, c - 4, 4 * g:4 * g + 4, 128:256],
                           in_=mm[m][64:128, :].rearrange("p (a q) -> p a q", a=4), mul=0.125)
            S.flush(nc, top)

        if stop_after <= 1:
            with ExitStack() as ph:
                t32 = sb("t32q", [128, 4, NOWN * 128], F32, ph)
                b_t32 = Buf("t32q")
                OP("dve", "tensor_copy", b_QT, [b_t32], out=t32[:], in_=QT[:])
                DMA("sp", "dbgq", [b_t32], [], dbg_o["d_ob"].rearrange("(c p) n -> p c n", p=128)[:, 0:4, 0:512], t32[:, :, 0:512])
                S.flush(nc, top)
            qs.close()
            oas.close()
            return nc
        with ExitStack() as ph:
            bm = sb("bm", [128, 5, 8, 128], BF16, ph)
            b_bm = Buf("bm")
            zt = sb("zt", [128, NRB * D], BF16, ph)
            b_zt = Buf("zt")
            OP("pool", "memset", [], [b_zt], zt[:], 0.0)
            for e in range(NEXP):
                DMA("sp", "zx", [b_zt], [b_XS], XS[e * CAP:(e + 1) * CAP, :].rearrange("(a p) n -> p a n", p=128),
                    zt[:].rearrange("p (a n) -> p a n", a=NRB))
            DMA("pool", "bm", [], [b_bm], bm[:].rearrange("p o h q -> p (o h q)"), bmT)
            kta = [sb(f"kta{i}", [128, SEQ], BF16, ph) for i in range(2)]
            va = [sb(f"va{i}", [128, NBLK, 130], BF16, ph) for i in range(2)]
            b_kta = [Buf(f"kta{i}") for i in range(2)]
            b_va = [Buf(f"va{i}") for i in range(2)]
            et = [sb(f"et{i}", [128, 10, 128], BF16, ph) for i in range(2)]
            b_et = [Buf(f"et{i}") for i in range(2)]
            rd = [sb(f"rd{i}", [128, 2], F32, ph) for i in range(2)]
            b_rd = [Buf(f"rd{i}") for i in range(2)]
            sps = [[pst(f"sp{i}_{t}", [128, 512], F32, ph) for t in range(3)] for i in range(2)]
            b_sps = [[Buf(f"sp{i}_{t}") for t in range(3)] for i in range(2)]
            ops_f = [pst(f"oa{i}", [128, 512], F32, ph) for i in range(2)]
            ops_ = [t[:, 0:130].rearrange("p (h d) -> p h d", h=2) for t in ops_f]
            b_ops = [Buf(f"oa{i}") for i in range(2)]
            it = 0
            for p in range(4):
                pi = p % 2
                DMA("sp", f"kta{pi}", [b_KT[p]], [b_kta[pi]], kta[pi][:], KT_s[p, :, :])
                for q4 in range(4):
                    DMA("sp", f"va{pi}", [b_VA], [b_va[pi]], va[pi][:, q4 * 16:(q4 + 1) * 16, :],
                        VA_s[q4 * 16:(q4 + 1) * 16, :, p * 130:(p + 1) * 130].rearrange("b p n -> p b n"))
                for j in range(NOWN):
                    i2 = it % 2
                    it += 1
                    L = 4 * j + 3
                    offs = [o for o in range(5) if L - 4 + o >= 0]
                    def slot(h, o):
                        return (h, o) if o < 4 else (2, h)
                    for h in range(2):
                        for o in offs:
                            Lk = L - 4 + o
                            bnk, col = slot(h, o)
                            dst = sps[i2][bnk][:, col * 128:(col + 1) * 128]
                            OP("pe", "matmul", [b_kta[pi], b_QT[p]], [b_sps[i2][bnk]], dst,
                               lhsT=kta[pi][64 * h:64 * h + 64, Lk * 128:(Lk + 1) * 128],
                               rhs=QT[64 * h:64 * h + 64, p, j * 128:(j + 1) * 128], start=True, stop=False, sig=False)
                            OP("pe", "matmul", [b_bm, b_cmb], [b_sps[i2][bnk]], dst,
                               lhsT=identB, rhs=bm[:, o, 2 * p + h, :], start=False, stop=True)
                    for h in range(2):
                        o_lo = offs[0]
                        n4 = len([o for o in offs if o < 4])
                        OP("act", "activation", [b_sps[i2][h]], [b_et[i2]],
                           out=et[i2][:, h * 5 + o_lo:h * 5 + o_lo + n4, :],
                           in_=sps[i2][h][:, o_lo * 128:(o_lo + n4) * 128].rearrange("p (o q) -> p o q", q=128), func=AF.Exp)
                    for h in range(2):
                        OP("act", "activation", [b_sps[i2][2]], [b_et[i2]], out=et[i2][:, h * 5 + 4, :],
                           in_=sps[i2][2][:, h * 128:(h + 1) * 128], func=AF.Exp)
                    if j == 0:
                        for h in range(2):
                            for o in offs:
                                Lk = L - 4 + o
                                if Lk < 3:
                                    OP("dve", "tensor_scalar", [b_et[i2], b_padt], [b_et[i2]], out=et[i2][:, h * 5 + o, :],
                                       in0=et[i2][:, h * 5 + o, :], scalar1=padt[:, Lk:Lk + 1], scalar2=None, op0=ALU.mult)
                    for h in range(2):
                        for n, o in enumerate(offs):
                            Lk = L - 4 + o
                            OP("pe", "matmul", [b_et[i2], b_va[pi]], [b_ops[i2]], ops_[i2][:, h, :],
                               lhsT=et[i2][:, h * 5 + o, :], rhs=va[pi][:, Lk, h * 65:(h + 1) * 65],
                               start=(n == 0), stop=(n == len(offs) - 1), sig=(n == len(offs) - 1))
                    OP("dve", "reciprocal", [b_ops[i2]], [b_rd[i2]], out=rd[i2][:], in_=ops_[i2][:, :, 64])
                    for h in range(2):
                        hh = 2 * p + h
                        OP("dve", "tensor_scalar", [b_ops[i2], b_rd[i2]], [b_OA[j]], out=OA[:, j, hh * 64:(hh + 1) * 64],
                           in0=ops_[i2][:, h, 0:64], scalar1=rd[i2][:, h:h + 1], scalar2=None, op0=ALU.mult)
            S.flush(nc, top)

        if stop_after == 15:
            with ExitStack() as ph:
                t32 = sb("t32", [128, NOWN, 512], F32, ph)
                b_t32 = Buf("t32")
                OP("dve", "tensor_copy", b_OA, [b_t32], out=t32[:], in_=OA[:])
                DMA("sp", "dbg", [b_t32], [], dbg_o["d_oa"].rearrange("(j p) n -> p j n", p=128), t32[:])
                S.flush(nc, top)
            qs.close()
            oas.close()
            return nc
        with ExitStack() as ph:
            ktb = sb("ktb", [128, 2, SEQ], BF16, ph)
            vb = sb("vb", [128, NBLK, 256], BF16, ph)
            b_ktb, b_vb = Buf("ktb"), Buf("vb")
            ee = [sb(f"ee{i}", [128, 512], F32, ph) for i in range(4)]
            ll = [sb(f"ll{i}", [128, 512], BF16, ph) for i in range(4)]
            ex = [sb(f"ex{i}", [128, 512], F32, ph) for i in range(3)]
            at = [sb(f"at{i}", [128, 512], BF16, ph) for i in range(3)]
            b_ee = [Buf(f"ee{i}") for i in range(4)]
            b_ll = [Buf(f"ll{i}") for i in range(4)]
            b_ex = [Buf(f"ex{i}") for i in range(3)]
            b_at = [Buf(f"at{i}") for i in range(3)]
            zp = [pst(f"zp{i}", [128, 512], F32, ph) for i in range(3)]
            b_zp = [Buf(f"zp{i}") for i in range(3)]
            cp = [pst(f"cp{i}", [128, 512], F32, ph) for i in range(2)]
            b_cp = [Buf(f"cp{i}") for i in range(2)]
            op_ = [pst(f"ob{i}", [128, 512], F32, ph) for i in range(2)]
            b_op = [Buf(f"ob{i}") for i in range(2)]
            import os
            SB_G = int(os.environ.get("SB_G", "2"))
            SB_J = int(os.environ.get("SB_J", str(NOWN)))
            SB_PIPE = int(os.environ.get("SB_PIPE", "1"))
            for G in range(SB_G):
                DMA("sp", "ktb", [b_KT[4 + 2 * G], b_KT[5 + 2 * G]], [b_ktb], ktb[:],
                    KT_s[4 + 2 * G:6 + 2 * G, :, :].rearrange("c p n -> p c n"))
                for q4 in range(4):
                    DMA("sp", "vb", [b_VB], [b_vb], vb[:, q4 * 16:(q4 + 1) * 16, :],
                        VB_s[q4 * 16:(q4 + 1) * 16, :, G * 256:(G + 1) * 256].rearrange("b p n -> p b n"))
                steps = []
                for j in range(SB_J):
                    L = 4 * j + 3
                    for n, Lk in enumerate(range(L, -1, -1)):
                        steps.append((j, Lk, n == 0, Lk == 0))

                def stA(n, G=G):
                    j, Lk, first, last = steps[n]
                    s3, s4 = n % 3, n % 4
                    for pl in range(2):
                        OP("pe", "matmul", [b_ktb, b_QT[4 + 2 * G + pl]], [b_zp[s3]], zp[s3][:, pl * 256:(pl + 1) * 256],
                           lhsT=ktb[:, pl, Lk * 128:(Lk + 1) * 128], rhs=QTB[:, 2 * G + pl, j, :],
                           start=True, stop=True, sig=(pl == 1))
                    OP("act", "activation", [b_zp[s3]], [b_ee[s4]], out=ee[s4][:], in_=zp[s3][:], func=AF.Exp)
                    if first:
                        OP("dve", "tensor_tensor", [b_ee[s4], b_tril8], [b_ee[s4]], out=ee[s4][:], in0=ee[s4][:],
                           in1=tril8[:, 0:512], op=ALU.mult)
                    elif Lk < 3:
                        OP("dve", "tensor_scalar", [b_ee[s4], b_padt], [b_ee[s4]], out=ee[s4][:], in0=ee[s4][:],
                           scalar1=padt[:, Lk:Lk + 1], scalar2=None, op0=ALU.mult)
                    OP("act", "activation", [b_ee[s4]], [b_ll[s4]], out=ll[s4][:], in_=ee[s4][:], func=AF.Ln, bias=1.0)

                def stU(n, G=G):
                    j, Lk, first, last = steps[n]
                    s4, s2, ci = n % 4, n % 2, j % 2
                    OP("pe", "matmul", [b_ll[s4], b_cmb], [b_cp[ci]], cp[ci][:], lhsT=triU, rhs=ll[s4][:],
                       start=first, stop=True, skip_group_check=True)
                    OP("act", "activation", [b_cp[ci]], [b_ex[s2]], out=ex[s2][:], in_=cp[ci][:], func=AF.Exp, scale=-1.0)

                def stC(n, G=G):
                    j, Lk, first, last = steps[n]
                    s4, ci = n % 4, j % 2
                    if not last:
                        OP("pe", "matmul", [b_ll[s4], b_cmb], [b_cp[ci]], cp[ci][:], lhsT=comp, rhs=ll[s4][:],
                           start=False, stop=True, skip_group_check=True)

                def stV(n, G=G):
                    j, Lk, first, last = steps[n]
                    s4, s2, ci = n % 4, n % 2, j % 2
                    OP("dve", "tensor_tensor", [b_ee[s4], b_ex[s2]], [b_at[s2]], out=at[s2][:], in0=ee[s4][:],
                       in1=ex[s2][:], op=ALU.mult)
                    for hl in range(4):
                        OP("pe", "matmul", [b_at[s2], b_vb], [b_op[ci]], op_[ci][:, hl * 64:(hl + 1) * 64],
                           lhsT=at[s2][:, hl * 128:(hl + 1) * 128], rhs=vb[:, Lk, hl * 64:(hl + 1) * 64],
                           start=(first and hl == 0), stop=last, skip_group_check=True, sig=(hl == 3))
                    if last:
                        OP("dve", "tensor_copy", [b_op[ci]], [b_OB[j]], out=OB[:, j, G * 256:(G + 1) * 256], in_=op_[ci][:, 0:256])

                NS = len(steps)
                for n in range(min(3, NS)):
                    stA(n)
                stU(0)
                for n in range(NS):
                    stC(n)
                    if n + 1 < NS:
                        stU(n + 1)
                    stV(n)
                    if n + 3 < NS:
                        stA(n + 3)
            S.flush(nc, top)

        qs.close()
        if dbg:
            with ExitStack() as ph:
                t32 = sb("t32", [128, NOWN, 512], F32, ph)
                b_t32 = Buf("t32")
                for nm, src, bsrc in (("d_oa", OA, b_OA), ("d_ob", OB, b_OB)):
                    OP("dve", "tensor_copy", bsrc, [b_t32], out=t32[:], in_=src[:])
                    DMA("sp", "dbg", [b_t32], [], dbg_o[nm].rearrange("(j p) n -> p j n", p=128), t32[:])
                S.flush(nc, top)

        if stop_after <= 2:
            oas.close()
            return nc
        b_GT = [Buf(f"GT{j}") for j in range(NOWN)]
        b_IDX = [Buf(f"IDX{j}") for j in range(NOWN)]
        with ExitStack() as ph:
            wout = sb("wout", [128, 8, D], BF16, ph)
            wq = sb("wq", [128, 8, D], BF16, ph)
            wo = sb("wo", [128, 8, D], BF16, ph)
            wr = sb("wr", [128, 8, NEXP], F32, ph)
            b_wout, b_wq, b_wo, b_wkv, b_wr = Buf("wout"), Buf("wq"), Buf("wo"), Buf("wkv"), Buf("wr")
            for k in range(8):
                DMA("pool", "w_wout", [], [b_wout], wout[:, k, :], w_out[k * 128:(k + 1) * 128, :])
                DMA("pool", "w_wq", [], [b_wq], wq[:, k, :], w_q[k * 128:(k + 1) * 128, :])
                DMA("pool", "w_wo", [], [b_wo], wo[:, k, :], w_o[k * 128:(k + 1) * 128, :])
            DMA("sp", "w_wr", [], [b_wr], wr[:], w_r.rearrange("(k p) n -> p k n", p=128))
            lng = sb("lng", [128, 2, D], F32, ph)
            lnb = sb("lnb", [128, 2, D], F32, ph)
            ggt = sb("ggt", [128, 2, 512], F32, ph)
            brt = sb("brt", [128, NEXP], F32, ph)
            b_vec = Buf("vec")
            for i in range(2):
                DMA("sp", "w_vec", [], [b_vec], lng[:, i, :], ln_g[i:i + 1, :].partition_broadcast(128))
                DMA("sp", "w_vec", [], [b_vec], lnb[:, i, :], ln_b[i:i + 1, :].partition_broadcast(128))
            DMA("sp", "w_vec", [], [b_vec], ggt[:, 0, :], gga.partition_broadcast(128))
            DMA("sp", "w_vec", [], [b_vec], ggt[:, 1, :], ggb.partition_broadcast(128))
            DMA("sp", "w_vec", [], [b_vec], brt[:], b_r.partition_broadcast(128))

            p3 = [pst(f"p3_{i}", [128, 512], F32, ph) for i in range(6)]
            b_p3 = [Buf(f"p3_{i}") for i in range(6)]
            pbt = pst("pbt", [128, 1024], BF16, ph)
            b_pbt = Buf("pbt")
            pcnt = {"i": 0}

            def bank():
                i = pcnt["i"] % 6
                pcnt["i"] += 1
                return p3[i], b_p3[i]

            KmT = sb("KmT", [128, 8, 256], BF16, ph)
            Vm = sb("Vm", [128, 2, D], BF16, ph)
            cntt = sb("cntt", [128, NEXP], F32, ph)
            kvs = ExitStack()
            wkv = sb("wkv", [128, 8, 2 * D], BF16, kvs)
            mems = sb("mems", [128, 2, D], F32, kvs)
            memT = sb("memT", [128, 8, 256], BF16, kvs)
            for k in range(8):
                DMA("pool", "w_wkv", [], [b_wkv], wkv[:, k, :], w_kv[k * 128:(k + 1) * 128, :])
            b_mems, b_memT, b_KmT, b_Vm = Buf("mems"), Buf("memT"), Buf("KmT"), Buf("Vm")
            DMA("sp", "w_mems", [], [b_mems], mems[:], memb.rearrange("(a p) n -> p a n", p=128))
            for a in range(2):
                for hf in range(2):
                    pb, bb = bank()
                    for k4 in range(4):
                        k = hf * 4 + k4
                        OP("pe", "transpose", [b_mems, b_cm], [bb], out=pb[:, k4 * 128:(k4 + 1) * 128],
                           in_=mems[:, a, k * 128:(k + 1) * 128], identity=identF, sig=(k4 == 3))
                    OP("dve", "tensor_copy", [bb], [b_memT], out=memT[:, hf * 4:hf * 4 + 4, a * 128:(a + 1) * 128],
                       in_=pb[:].rearrange("p (k n) -> p k n", k=4))
            for c in range(8):
                pb, bb = bank()
                for k in range(8):
                    OP("pe", "matmul", [b_wkv, b_memT], [bb], pb[:, 0:256], lhsT=wkv[:, k, c * 128:(c + 1) * 128],
                       rhs=memT[:, k, :], start=(k == 0), stop=(k == 7), sig=(k == 7))
                OP("act", "copy", [bb], [b_KmT], out=KmT[:, c, :], in_=pb[:, 0:256])
            for a in range(2):
                for hf in range(2):
                    pb, bb = bank()
                    for k in range(8):
                        OP("pe", "matmul", [b_wkv, b_memT], [bb], pb[:], lhsT=memT[:, k, a * 128:(a + 1) * 128],
                           rhs=wkv[:, k, D + hf * 512:D + (hf + 1) * 512], start=(k == 0), stop=(k == 7), sig=(k == 7))
                    OP("dve", "tensor_copy", [bb], [b_Vm], out=Vm[:, a, hf * 512:(hf + 1) * 512], in_=pb[:])

            S.flush(nc, top)
            kvs.close()
            def mk(name, shape, dt, n=2):
                return [sb(f"{name}{i}", shape, dt, ph) for i in range(n)], [Buf(f"{name}{i}") for i in range(n)]
            xoj, b_xoj = mk("xoj", [128, D], F32)
            cat, b_cat = mk("cat", [128, D], BF16)
            catT, b_catT = mk("catT", [128, 8, 128], BF16)
            yy, b_yy = mk("yy", [128, D], F32)
            x1, b_x1 = mk("x1", [128, D], F32)
            x1T, b_x1T = mk("x1T", [128, 8, 128], BF16)
            qT, b_qT = mk("qT", [128, 8, 128], BF16)
            eT, b_eT = mk("eT", [128, 8, 128], BF16)
            rdn, b_rdn = mk("rdn", [128, 512], F32)
            oT, b_oT = mk("oT", [128, 8, 128], BF16)
            x2, b_x2 = mk("x2", [128, D], F32)
            x2b, b_x2b = mk("x2b", [128, D], BF16)
            x2T, b_x2T = mk("x2T", [128, 8, 128], F32)
            junk, b_junk = mk("junk", [128, D], F32)
            sm, b_sm = mk("sm", [128, 64], F32)
            lg, b_lg = mk("lg", [128, 8, NEXP], F32)
            mselb, b_mselb = mk("mselb", [128, NEXP], BF16)
            idxf, b_idxf = mk("idxf", [128, 4], F32)
            b_cntt = Buf("cntt")
            OP("pool", "memset", [], [b_cntt], cntt[:], 0.0)

            def layer_norm(i2, src, b_src, dst, b_dst, li):
                s_ = sm[i2]
                bs = b_sm[i2]
                OP("dve", "memset", [], [bs], s_[:, 0:8], 0.0)
                OP("act", "activation", [b_src, bs], [b_junk[i2], bs], out=junk[i2][:], in_=src[:], func=AF.Copy,
                   accum_out=s_[:, 0:1])
                OP("act", "activation", [b_src, bs], [b_junk[i2], bs], out=junk[i2][:], in_=src[:], func=AF.Square,
                   accum_out=s_[:, 1:2])
                OP("dve", "tensor_scalar", [bs], [bs], out=s_[:, 2:4], in0=s_[:, 0:2], scalar1=1.0 / D, scalar2=None, op0=ALU.mult)
                OP("dve", "tensor_tensor", [bs], [bs], out=s_[:, 4:5], in0=s_[:, 2:3], in1=s_[:, 2:3], op=ALU.mult)
                OP("dve", "tensor_tensor", [bs], [bs], out=s_[:, 5:6], in0=s_[:, 3:4], in1=s_[:, 4:5], op=ALU.subtract)
                OP("act", "activation", [bs], [bs], out=s_[:, 6:7], in_=s_[:, 5:6], func=AF.Ln, bias=LN_EPS)
                OP("act", "activation", [bs], [bs], out=s_[:, 7:8], in_=s_[:, 6:7], func=AF.Exp, scale=-0.5)
                OP("dve", "tensor_scalar", [b_src, bs], [b_dst], out=dst[:], in0=src[:], scalar1=s_[:, 2:3], scalar2=s_[:, 7:8],
                   op0=ALU.subtract, op1=ALU.mult)
                OP("pool", "tensor_tensor", [b_dst, b_vec], [b_dst], out=dst[:], in0=dst[:], in1=lng[:, li, :], op=ALU.mult)
                OP("pool", "tensor_tensor", [b_dst, b_vec], [b_dst], out=dst[:], in0=dst[:], in1=lnb[:, li, :], op=ALU.add)

            for j in range(NOWN):
                i2 = j % 2
                s_ = sm[i2]
                bs = b_sm[i2]
                DMA("sp", f"xoj{i2}", [], [b_xoj[i2]], xoj[i2][:], xo[j * 128:(j + 1) * 128, :])
                OP("dve", "memset", [], [bs], s_[:, 16:24], 0.0)
                for gi, (O_, bO) in enumerate(((OA, b_OA[j]), (OB, b_OB[j]))):
                    OP("act", "activation", [bO, bs], [b_junk[i2], bs], out=junk[i2][:, 0:512], in_=O_[:, j, :], func=AF.Square,
                       accum_out=s_[:, 16 + gi:17 + gi])
                OP("act", "activation", [bs], [bs], out=s_[:, 18:20], in_=s_[:, 16:18], func=AF.Ln, scale=1.0 / 512, bias=RMS_EPS)
                OP("act", "activation", [bs], [bs], out=s_[:, 20:22], in_=s_[:, 18:20], func=AF.Exp, scale=-0.5)
                for gi, (O_, bO) in enumerate(((OA, b_OA[j]), (OB, b_OB[j]))):
                    OP("dve", "scalar_tensor_tensor", [bO, bs, b_vec], [b_cat[i2]], out=cat[i2][:, gi * 512:(gi + 1) * 512],
                       in0=O_[:, j, :], scalar=s_[:, 20 + gi:21 + gi], in1=ggt[:, gi, :], op0=ALU.mult, op1=ALU.mult)
                for k in range(8):
                    OP("pe", "transpose", [b_cat[i2], b_cmb], [b_pbt], out=pbt[:, k * 128:(k + 1) * 128],
                       in_=cat[i2][:, k * 128:(k + 1) * 128], identity=identB, sig=(k == 7))
                OP("dve", "tensor_copy", [b_pbt], [b_catT[i2]], out=catT[i2][:], in_=pbt[:].rearrange("p (k n) -> p k n", k=8))
                for hf in range(2):
                    pb, bb = bank()
                    for k in range(8):
                        OP("pe", "matmul", [b_catT[i2], b_wout], [bb], pb[:], lhsT=catT[i2][:, k, :],
                           rhs=wout[:, k, hf * 512:(hf + 1) * 512], start=(k == 0), stop=(k == 7), sig=(k == 7))
                    OP("dve", "scalar_tensor_tensor", [b_xoj[i2], bb], [b_yy[i2]], out=yy[i2][:, hf * 512:(hf + 1) * 512],
                       in0=xoj[i2][:, hf * 512:(hf + 1) * 512], scalar=ALPHA, in1=pb[:], op0=ALU.mult, op1=ALU.add)
                layer_norm(i2, yy[i2], b_yy[i2], x1[i2], b_x1[i2], 0)
                if dbg:
                    DMA("sp", "dbg_x1", [b_x1[i2]], [], dbg_o["d_x1"][j * 128:(j + 1) * 128, :], x1[i2][:])
                for hf in range(2):
                    pb, bb = bank()
                    for k4 in range(4):
                        k = hf * 4 + k4
                        OP("pe", "transpose", [b_x1[i2], b_cm], [bb], out=pb[:, k4 * 128:(k4 + 1) * 128],
                           in_=x1[i2][:, k * 128:(k + 1) * 128], identity=identF)
                    OP("act", "copy", [bb], [b_x1T[i2]], out=x1T[i2][:, hf * 4:hf * 4 + 4, :],
                       in_=pb[:].rearrange("p (k n) -> p k n", k=4))
                for hf in range(2):
                    pb, bb = bank()
                    for c4 in range(4):
                        c = hf * 4 + c4
                        for k in range(8):
                            OP("pe", "matmul", [b_wq, b_x1T[i2]], [bb], pb[:, c4 * 128:(c4 + 1) * 128],
                               lhsT=wq[:, k, c * 128:(c + 1) * 128], rhs=x1T[i2][:, k, :], start=(k == 0), stop=(k == 7), sig=(k == 7))
                    OP("act", "mul", [bb], [b_qT[i2]], out=qT[i2][:, hf * 4:hf * 4 + 4, :],
                       in_=pb[:].rearrange("p (k n) -> p k n", k=4), mul=1.0 / 16)
                for hf in range(2):
                    pb, bb = bank()
                    for t4 in range(4):
                        t = hf * 4 + t4
                        hd, mc = t // 2, t % 2
                        for dc in range(2):
                            OP("pe", "matmul", [b_KmT, b_qT[i2]], [bb], pb[:, t4 * 128:(t4 + 1) * 128],
                               lhsT=KmT[:, 2 * hd + dc, mc * 128:(mc + 1) * 128], rhs=qT[i2][:, 2 * hd + dc, :],
                               start=(dc == 0), stop=(dc == 1), sig=(dc == 1))
                    OP("act", "activation", [bb], [b_eT[i2]], out=eT[i2][:, hf * 4:hf * 4 + 4, :],
                       in_=pb[:].rearrange("p (k n) -> p k n", k=4), func=AF.Exp)
                pbd, bbd = bank()
                for hd in range(4):
                    for mc in range(2):
                        OP("pe", "matmul", [b_eT[i2], b_cmb], [bbd], pbd[:, hd * 128:(hd + 1) * 128], lhsT=onesB,
                           rhs=eT[i2][:, hd * 2 + mc, :], start=(mc == 0), stop=(mc == 1), sig=(mc == 1))
                OP("dve", "reciprocal", [bbd], [b_rdn[i2]], out=rdn[i2][:], in_=pbd[:])
                for hf in range(2):
                    pb, bb = bank()
                    for t4 in range(4):
                        t = hf * 4 + t4
                        hd, dc = t // 2, t % 2
                        for mc in range(2):
                            OP("pe", "matmul", [b_Vm, b_eT[i2]], [bb], pb[:, t4 * 128:(t4 + 1) * 128],
                               lhsT=Vm[:, mc, t * 128:(t + 1) * 128], rhs=eT[i2][:, hd * 2 + mc, :],
                               start=(mc == 0), stop=(mc == 1), sig=(mc == 1))
                    for t4 in range(4):
                        t = hf * 4 + t4
                        hd = t // 2
                        OP("dve", "tensor_tensor", [bb, b_rdn[i2]], [b_oT[i2]], out=oT[i2][:, t, :],
                           in0=pb[:, t4 * 128:(t4 + 1) * 128], in1=rdn[i2][:, hd * 128:(hd + 1) * 128], op=ALU.mult)
                for hf in range(2):
                    pb, bb = bank()
                    for k in range(8):
                        OP("pe", "matmul", [b_oT[i2], b_wo], [bb], pb[:], lhsT=oT[i2][:, k, :],
                           rhs=wo[:, k, hf * 512:(hf + 1) * 512], start=(k == 0), stop=(k == 7), sig=(k == 7))
                    OP("dve", "scalar_tensor_tensor", [b_x1[i2], bb], [b_yy[i2]], out=yy[i2][:, hf * 512:(hf + 1) * 512],
                       in0=x1[i2][:, hf * 512:(hf + 1) * 512], scalar=ALPHA, in1=pb[:], op0=ALU.mult, op1=ALU.add)
                layer_norm(i2, yy[i2], b_yy[i2], x2[i2], b_x2[i2], 1)
                DMA("sp", f"x2s{i2}", [b_x2[i2]], [b_X2S], X2S[j * 128:(j + 1) * 128, :], x2[i2][:])
                if dbg:
                    DMA("sp", "dbg_x2", [b_x2[i2]], [], dbg_o["d_x2"][j * 128:(j + 1) * 128, :], x2[i2][:])
                OP("act", "copy", [b_x2[i2]], [b_x2b[i2]], out=x2b[i2][:], in_=x2[i2][:])
                for hf in range(2):
                    pb, bb = bank()
                    for k4 in range(4):
                        k = hf * 4 + k4
                        OP("pe", "transpose", [b_x2[i2], b_cm], [bb], out=pb[:, k4 * 128:(k4 + 1) * 128],
                           in_=x2[i2][:, k * 128:(k + 1) * 128], identity=identF)
                    OP("dve", "tensor_copy", [bb], [b_x2T[i2]], out=x2T[i2][:, hf * 4:hf * 4 + 4, :],
                       in_=pb[:].rearrange("p (k n) -> p k n", k=4))
                pb, bb = bank()
                for k in range(8):
                    OP("pe", "matmul", [b_x2T[i2], b_wr], [bb], pb[:, 0:NEXP], lhsT=x2T[i2][:, k, :], rhs=wr[:, k, :],
                       start=(k == 0), stop=(k == 7), sig=(k == 7))
                W = lg[i2]
                bW = b_lg[i2]
                OP("dve", "tensor_tensor", [bb, b_vec], [bW], out=W[:, 0, :], in0=pb[:, 0:NEXP], in1=brt[:], op=ALU.add)
                if dbg:
                    DMA("sp", "dbg_lg", [bW], [], dbg_o["d_lg"][j * 128:(j + 1) * 128, :], W[:, 0, :])
                OP("dve", "max", [bW], [bs], out=s_[:, 32:40], in_=W[:, 0, :])
                OP("dve", "tensor_scalar", [bW, bs], [bW], out=W[:, 1, :], in0=W[:, 0, :], scalar1=s_[:, 35:36], scalar2=None,
                   op0=ALU.is_ge)
                OP("dve", "tensor_scalar", [bs], [bs], out=s_[:, 40:41], in0=s_[:, 32:33], scalar1=-1.0, scalar2=None, op0=ALU.mult)
                OP("act", "activation", [bW, bs], [bW], out=W[:, 2, :], in_=W[:, 0, :], func=AF.Exp, bias=s_[:, 40:41])
                OP("dve", "memset", [], [bs], s_[:, 41:42], 0.0)
                OP("dve", "tensor_tensor", [bW], [bW], out=W[:, 3, :], in0=W[:, 2, :], in1=W[:, 1, :], op=ALU.mult)
                OP("dve", "reduce_sum", [bW], [bs], out=s_[:, 41:42], in_=W[:, 3, :], axis=mybir.AxisListType.X)
                OP("dve", "reciprocal", [bs], [bs], out=s_[:, 42:43], in_=s_[:, 41:42])
                OP("dve", "tensor_scalar", [bW, bs], [bW], out=W[:, 3, :], in0=W[:, 3, :], scalar1=s_[:, 42:43], scalar2=None,
                   op0=ALU.mult)
                OP("dve", "tensor_copy", [bW], [b_mselb[i2]], out=mselb[i2][:], in_=W[:, 1, :])
                pb2, bb2 = bank()
                OP("pe", "matmul", [b_mselb[i2], b_cmb], [bb2], pb2[:, 0:NEXP], lhsT=comp, rhs=mselb[i2][:], start=True, stop=True)
                OP("pe", "matmul", [b_mselb[i2], b_cmb], [bb2], pb2[:, 64:64 + NEXP], lhsT=onesB, rhs=mselb[i2][:], start=True, stop=True)
                OP("dve", "tensor_tensor", [bb2, b_cntt], [bW], out=W[:, 4, :], in0=pb2[:, 0:NEXP], in1=cntt[:], op=ALU.add)
                OP("dve", "tensor_tensor", [bb2, b_cntt], [b_cntt], out=cntt[:], in0=pb2[:, 64:64 + NEXP], in1=cntt[:], op=ALU.add)
                OP("dve", "tensor_scalar", [bW], [bW], out=W[:, 5, :], in0=W[:, 4, :], scalar1=float(CAP), scalar2=BIG,
                   op0=ALU.is_ge, op1=ALU.mult)
                OP("dve", "tensor_tensor", [bW, b_eoff], [bW], out=W[:, 4, :], in0=W[:, 4, :], in1=eofft[:], op=ALU.add)
                OP("dve", "tensor_tensor", [bW], [bW], out=W[:, 4, :], in0=W[:, 4, :], in1=W[:, 5, :], op=ALU.add)
                OP("dve", "memset", [], [b_idxf[i2]], idxf[i2][:], 0.0)
                OP("dve", "memset", [], [b_GT[j]], GT[:, j, :], 0.0)
                for kk in range(4):
                    OP("dve", "tensor_scalar", [bW, bs], [bW], out=W[:, 6, :], in0=W[:, 0, :], scalar1=s_[:, 32 + kk:33 + kk],
                       scalar2=None, op0=ALU.is_equal)
                    OP("dve", "tensor_tensor", [bW], [bW], out=W[:, 7, :], in0=W[:, 6, :], in1=W[:, 4, :], op=ALU.mult)
                    OP("dve", "reduce_sum", [bW], [b_idxf[i2]], out=idxf[i2][:, kk:kk + 1], in_=W[:, 7, :], axis=mybir.AxisListType.X)
                    OP("dve", "tensor_tensor", [bW], [bW], out=W[:, 7, :], in0=W[:, 6, :], in1=W[:, 3, :], op=ALU.mult)
                    OP("dve", "reduce_sum", [bW], [b_GT[j]], out=GT[:, j, kk:kk + 1], in_=W[:, 7, :], axis=mybir.AxisListType.X)
                OP("dve", "tensor_scalar", [b_idxf[i2]], [bs], out=s_[:, 44:48], in0=idxf[i2][:], scalar1=float(NROWS), scalar2=None,
                   op0=ALU.is_lt)
                OP("dve", "tensor_tensor", [b_GT[j], bs], [b_GT[j]], out=GT[:, j, :], in0=GT[:, j, :], in1=s_[:, 44:48], op=ALU.mult)
                OP("dve", "tensor_scalar", [b_idxf[i2]], [b_idxf[i2]], out=idxf[i2][:], in0=idxf[i2][:], scalar1=float(NROWS), scalar2=None,
                   op0=ALU.min)
                OP("dve", "tensor_copy", [b_idxf[i2]], [b_IDX[j]], out=IDX[:, j, :], in_=idxf[i2][:])
                if dbg:
                    DMA("sp", "dbg_idx", [b_idxf[i2]], [], dbg_o["d_idx"][j * 128:(j + 1) * 128, :], idxf[i2][:])
                    DMA("sp", "dbg_gt", [b_GT[j]], [], dbg_o["d_gate"][j * 128:(j + 1) * 128, :], GT[:, j, :])
                for kk in range(4):
                    S.dma("pool", "scat", lambda eng, j=j, kk=kk, i2=i2: eng.indirect_dma_start(
                        out=XS[:, :], out_offset=bass.IndirectOffsetOnAxis(ap=IDX[:, j, kk:kk + 1], axis=0),
                        in_=x2b[i2][:, :], in_offset=None),
                        [b_x2b[i2], b_IDX[j]], [b_XS])
            S.flush(nc, top)

        oas.close()
        if stop_after <= 3:
            return nc
        with ExitStack() as ph:
            wgu = [sb(f"wgu{i}", [128, 8, 2 * D], BF16, ph) for i in range(2)]
            wd = [sb(f"wd{i}", [128, 8, D], BF16, ph) for i in range(2)]
            b_wgu = [[Buf(f"wgu{i}_{k}") for k in range(8)] for i in range(2)]
            b_wd = [[Buf(f"wd{i}_{k}") for k in range(8)] for i in range(2)]
            stg = [sb(f"stg{i}", [128, 2 * D], F32, ph) for i in range(3)]
            std = [sb(f"std{i}", [128, D], F32, ph) for i in range(2)]
            b_stg = [Buf(f"stg{i}") for i in range(3)]
            b_std = [Buf(f"std{i}") for i in range(2)]
            bdt = [sb(f"bdt{i}", [128, D], F32, ph) for i in range(1)] * 2
            b_bdt = [Buf("bdt0")] * 2
            xg = [sb(f"xg{i}", [128, NRB, D], BF16, ph) for i in range(1)] * 2
            b_xg = [Buf("xg0")] * 2
            xgT = [sb(f"xgT{i}", [128, 8, CAP], BF16, ph) for i in range(2)]
            b_xgT = [Buf(f"xgT{i}") for i in range(2)]
            hT = [sb(f"hT{i}", [128, 8, CAP], BF16, ph) for i in range(2)]
            b_hT = [Buf(f"hT{i}") for i in range(2)]
            gg = [sb(f"gg{i}", [128, CAP], F32, ph) for i in range(2)]
            sg = [sb(f"sg{i}", [128, CAP], F32, ph) for i in range(2)]
            uu = [sb(f"uu{i}", [128, CAP], F32, ph) for i in range(2)]
            b_gg = [Buf(f"gg{i}") for i in range(2)]
            b_sg = [Buf(f"sg{i}") for i in range(2)]
            b_uu = [Buf(f"uu{i}") for i in range(2)]
            ys = [sb(f"ys{i}", [128, D], F32, ph) for i in range(1)] * 2
            b_ys = [Buf("ys0")] * 2
            bgs = sb("bgs", [128, 4, 128], F32, ph)
            bgT = sb("bgT", [128, 512], F32, ph)
            b_bgs, b_bgT = Buf("bgs"), Buf("bgT")
            pt = [pst(f"pt{i}", [128, 1024], BF16, ph) for i in range(2)]
            b_pt = [Buf(f"pt{i}") for i in range(2)]
            pg = [pst(f"pg{i}", [128, 512], F32, ph) for i in range(4)]
            b_pg = [Buf(f"pg{i}") for i in range(4)]
            py = [pst(f"py{i}", [128, 512], F32, ph) for i in range(2)]
            b_py = [Buf(f"py{i}") for i in range(2)]
            DMA("sp", "bgs", [], [b_bgs], bgs[:], b_gu.rearrange("(a r) p -> r a p", r=128))
            for a in range(4):
                OP("pe", "transpose", [b_bgs, b_cm], [b_pg[0]], out=pg[0][:, a * 128:(a + 1) * 128], in_=bgs[:, a, :], identity=identF)
            OP("dve", "tensor_copy", [b_pg[0]], [b_bgT], out=bgT[:], in_=pg[0][:])
            bgT3 = bgT[:].rearrange("p (e c) -> p e c", c=16)
            OP("dve", "tensor_scalar", [b_bgT], [b_bgT], out=bgT3[:, :, 8:16], in0=bgT3[:, :, 8:16], scalar1=1.0, scalar2=None,
               op0=ALU.add)
            tc_ = {"t": 0, "g": 0, "y": 0}
            OP("pool", "memset", [], [b_ys[0]], ys[0][:], 0.0)
            DMA("sp", "ys0", [b_ys[0]], [b_YS], YS[NROWS:NROWS + 128, :], ys[0][:])

            def issue_gu(e, k):
                sl = k % 3
                DMA("sp", f"stg{sl}", [], [b_stg[sl]], stg[sl][:], w_gu[e, k * 128:(k + 1) * 128, :])

            def issue_d(e, k):
                sl = k % 2
                DMA("sp", f"std{sl}", [], [b_std[sl]], std[sl][:], w_d[e, k * 128:(k + 1) * 128, :])

            def cast_gu(e, k):
                sl, i = k % 3, e % 2
                if k % 2 == 0:
                    OP("act", "copy", [b_stg[sl]], [b_wgu[i][k]], out=wgu[i][:, k, :], in_=stg[sl][:])
                else:
                    OP("dve", "tensor_copy", [b_stg[sl]], [b_wgu[i][k]], out=wgu[i][:, k, :], in_=stg[sl][:])

            def cast_d(e, k):
                sl, i = k % 2, e % 2
                if k % 2 == 1:
                    OP("act", "copy", [b_std[sl]], [b_wd[i][k]], out=wd[i][:, k, :], in_=std[sl][:])
                else:
                    OP("dve", "tensor_copy", [b_std[sl]], [b_wd[i][k]], out=wd[i][:, k, :], in_=std[sl][:])

            def load_bd(e):
                i = e % 2
                DMA("sp", "bdt0", [], [b_bdt[i]], bdt[i][:], b_d[e:e + 1, :].partition_broadcast(128))

            def load_xg(e):
                i = e % 2
                DMA("sp", "xg0", [b_XS], [b_xg[i]], xg[i][:], XS[e * CAP:(e + 1) * CAP, :].rearrange("(a p) n -> p a n", p=128))

            load_bd(0)
            load_xg(0)
            for k in range(8):
                issue_gu(0, k)
                cast_gu(0, k)
            for k in range(8):
                issue_d(0, k)
                cast_d(0, k)
            for e in range(NEXP):
                i = e % 2
                nxt = e + 1 < NEXP
                if nxt:
                    for k in range(3):
                        issue_gu(e + 1, k)
                    for k in range(2):
                        issue_d(e + 1, k)
                for a in range(NRB):
                    t = tc_["t"] % 2
                    tc_["t"] += 1
                    for k in range(8):
                        OP("pe", "transpose", [b_xg[i], b_cmb], [b_pt[t]], out=pt[t][:, k * 128:(k + 1) * 128],
                           in_=xg[i][:, a, k * 128:(k + 1) * 128], identity=identB, sig=(k == 7))
                    OP("act" if a % 2 else "dve", "copy" if a % 2 else "tensor_copy", [b_pt[t]], [b_xgT[i]],
                       out=xgT[i][:, :, a * 128:(a + 1) * 128], in_=pt[t][:].rearrange("p (k n) -> p k n", k=8))
                if nxt:
                    load_xg(e + 1)
                for c in range(8):
                    g0 = tc_["g"] % 2
                    tc_["g"] += 1
                    pgg, pgu = pg[2 * g0], pg[2 * g0 + 1]
                    bgg_, bgu_ = b_pg[2 * g0], b_pg[2 * g0 + 1]
                    for k in range(8):
                        OP("pe", "matmul", [b_wgu[i][k], b_xgT[i]], [bgg_], pgg[:, 0:CAP], lhsT=wgu[i][:, k, c * 128:(c + 1) * 128],
                           rhs=xgT[i][:, k, :], start=(k == 0), stop=(k == 7), sig=(k == 7))
                    for k in range(8):
                        OP("pe", "matmul", [b_wgu[i][k], b_xgT[i]], [bgu_], pgu[:, 0:CAP], lhsT=wgu[i][:, k, D + c * 128:D + (c + 1) * 128],
                           rhs=xgT[i][:, k, :], start=(k == 0), stop=(k == 7), sig=(k == 7))
                    w2 = g0
                    OP("dve", "tensor_scalar", [bgg_, b_bgT], [b_gg[w2]], out=gg[w2][:], in0=pgg[:, 0:CAP],
                       scalar1=bgT[:, e * 16 + c:e * 16 + c + 1], scalar2=7.0, op0=ALU.add, op1=ALU.min)
                    OP("act", "activation", [b_gg[w2]], [b_sg[w2]], out=sg[w2][:], in_=gg[w2][:], func=AF.Silu, scale=1.702)
                    OP("act", "activation", [bgu_, b_bgT], [b_uu[w2]], out=uu[w2][:], in_=pgu[:, 0:CAP], func=AF.Identity,
                       bias=bgT[:, e * 16 + 8 + c:e * 16 + 8 + c + 1])
                    OP("dve", "tensor_scalar", [b_uu[w2]], [b_uu[w2]], out=uu[w2][:], in0=uu[w2][:], scalar1=-6.0, scalar2=8.0,
                       op0=ALU.max, op1=ALU.min)
                    OP("dve", "scalar_tensor_tensor", [b_uu[w2], b_sg[w2]], [b_hT[i]], out=hT[i][:, c, :], in0=uu[w2][:],
                       scalar=1.0 / 1.702, in1=sg[w2][:], op0=ALU.mult, op1=ALU.mult)
                    if nxt:
                        cast_gu(e + 1, c)
                        if c + 3 < 8:
                            issue_gu(e + 1, c + 3)
                        cast_d(e + 1, c)
                        if c + 2 < 8:
                            issue_d(e + 1, c + 2)
                for a in range(NRB):
                    yi = tc_["y"] % 2
                    tc_["y"] += 1
                    for hf in range(2):
                        for k in range(8):
                            OP("pe", "matmul", [b_hT[i], b_wd[i][k]], [b_py[hf]], py[hf][:], lhsT=hT[i][:, k, a * 128:(a + 1) * 128],
                               rhs=wd[i][:, k, hf * 512:(hf + 1) * 512], start=(k == 0), stop=(k == 7), sig=(k == 7))
                        OP("dve", "tensor_tensor", [b_py[hf], b_bdt[i]], [b_ys[yi]], out=ys[yi][:, hf * 512:(hf + 1) * 512],
                           in0=py[hf][:], in1=bdt[i][:, hf * 512:(hf + 1) * 512], op=ALU.add)
                    DMA("sp", "ys0", [b_ys[yi]], [b_YS], YS[e * CAP + a * 128:e * CAP + (a + 1) * 128, :], ys[yi][:])
                if nxt:
                    load_bd(e + 1)
            S.flush(nc, top)

        with ExitStack() as ph:
            NB5 = 4
            lng3 = sb("lng3", [128, D], F32, ph)
            lnb3 = sb("lnb3", [128, D], F32, ph)
            b_v3 = Buf("v3")
            DMA("sp", "v3", [], [b_v3], lng3[:], ln_g[2:3, :].partition_broadcast(128))
            DMA("sp", "v3", [], [b_v3], lnb3[:], ln_b[2:3, :].partition_broadcast(128))
            x2r = [sb(f"x2r{i}", [128, D], F32, ph) for i in range(NB5)]
            yk = [[sb(f"yk{i}_{k}", [128, D], F32, ph) for k in range(4)] for i in range(NB5)]
            acc = [sb(f"acc{i}", [128, D], F32, ph) for i in range(NB5)]
            res = [sb(f"res{i}", [128, D], F32, ph) for i in range(NB5)]
            jk5 = [sb(f"jk5{i}", [128, D], F32, ph) for i in range(NB5)]
            s5 = [sb(f"s5{i}", [128, 8], F32, ph) for i in range(NB5)]
            b_x2r = [Buf(f"x2r{i}") for i in range(NB5)]
            b_yk = [[Buf(f"yk{i}_{k}") for k in range(4)] for i in range(NB5)]
            b_acc = [Buf(f"acc{i}") for i in range(NB5)]
            b_res = [Buf(f"res{i}") for i in range(NB5)]
            b_jk5 = [Buf(f"jk5{i}") for i in range(NB5)]
            b_s5 = [Buf(f"s5{i}") for i in range(NB5)]
            for i in range(NB5):
                for k in range(4):
                    OP("pool", "memset", [], [b_yk[i][k]], yk[i][k][:], 0.0)
            for j in range(NOWN):
                i2 = j % NB5
                DMA("sp", f"x2r{i2}", [b_X2S], [b_x2r[i2]], x2r[i2][:], X2S[j * 128:(j + 1) * 128, :])
                for kk in range(4):
                    S.dma("pool", f"gath{i2}{kk}", lambda eng, j=j, kk=kk, i2=i2: eng.indirect_dma_start(
                        out=yk[i2][kk][:, :], out_offset=None, in_=YS[:, :],
                        in_offset=bass.IndirectOffsetOnAxis(ap=IDX[:, j, kk:kk + 1], axis=0)), [b_YS, b_IDX[j]], [b_yk[i2][kk]])
                OP("act", "mul", [b_x2r[i2]], [b_acc[i2]], out=acc[i2][:], in_=x2r[i2][:], mul=ALPHA)
                for kk in range(4):
                    OP("dve", "scalar_tensor_tensor", [b_yk[i2][kk], b_GT[j], b_acc[i2]], [b_acc[i2]], out=acc[i2][:],
                       in0=yk[i2][kk][:], scalar=GT[:, j, kk:kk + 1], in1=acc[i2][:], op0=ALU.mult, op1=ALU.add)
                s_ = s5[i2]
                bs = b_s5[i2]
                OP("dve", "memset", [], [bs], s_[:], 0.0)
                OP("act", "activation", [b_acc[i2], bs], [b_jk5[i2], bs], out=jk5[i2][:], in_=acc[i2][:], func=AF.Copy, accum_out=s_[:, 0:1])
                OP("act", "activation", [b_acc[i2], bs], [b_jk5[i2], bs], out=jk5[i2][:], in_=acc[i2][:], func=AF.Square, accum_out=s_[:, 1:2])
                OP("dve", "tensor_scalar", [bs], [bs], out=s_[:, 2:4], in0=s_[:, 0:2], scalar1=1.0 / D, scalar2=None, op0=ALU.mult)
                OP("dve", "tensor_tensor", [bs], [bs], out=s_[:, 4:5], in0=s_[:, 2:3], in1=s_[:, 2:3], op=ALU.mult)
                OP("dve", "tensor_tensor", [bs], [bs], out=s_[:, 5:6], in0=s_[:, 3:4], in1=s_[:, 4:5], op=ALU.subtract)
                OP("act", "activation", [bs], [bs], out=s_[:, 6:7], in_=s_[:, 5:6], func=AF.Ln, bias=LN_EPS)
                OP("act", "activation", [bs], [bs], out=s_[:, 7:8], in_=s_[:, 6:7], func=AF.Exp, scale=-0.5)
                OP("dve", "tensor_scalar", [b_acc[i2], bs], [b_res[i2]], out=res[i2][:], in0=acc[i2][:], scalar1=s_[:, 2:3],
                   scalar2=s_[:, 7:8], op0=ALU.subtract, op1=ALU.mult)
                OP("pool", "tensor_tensor", [b_res[i2], b_v3], [b_res[i2]], out=res[i2][:], in0=res[i2][:], in1=lng3[:], op=ALU.mult)
                OP("pool", "tensor_tensor", [b_res[i2], b_v3], [b_res[i2]], out=res[i2][:], in0=res[i2][:], in1=lnb3[:], op=ALU.add)
                DMA("sp", f"out{i2}", [b_res[i2]], [], out[j * 128:(j + 1) * 128, :], res[i2][:])
            S.flush(nc, top)

    return nc


def _const_mats():
    idx = np.arange(128)
    ident = np.eye(128, dtype=np.float32)
    triU = (idx[:, None] >= idx[None, :]).astype(np.float32)
    comp = (idx[:, None] < idx[None, :]).astype(np.float32)
    tril = (idx[:, None] < idx[None, :]).astype(np.float32)
    ones = np.ones((128, 128), np.float32)
    return np.concatenate([ident, triU, comp, tril, ones], axis=1)


def _bias_mask(rel_bias):
    kl = np.arange(640)
    q = np.arange(128)
    rel = 512 + q[None, :] - kl[:, None]
    ridx = np.clip(rel, -128, 128) + 128
    cq = 8 + q // 64
    ck = kl // 64
    vis = (ck[:, None] >= cq[None, :] - 8) & (ck[:, None] <= cq[None, :])
    bm = rel_bias[:, ridx]
    bm = np.where(vis[None], bm, np.float32(-30000.0)).astype(np.float32)
    bm = bm.reshape(8, 5, 128, 128).transpose(2, 1, 0, 3)
    return np.ascontiguousarray(bm.reshape(128, 5 * 8 * 128))


def make_in_maps(inputs, cores=range(8)):
    x = np.asarray(inputs["x"], np.float32)
    f = lambda k: np.ascontiguousarray(np.asarray(inputs[k], np.float32)[0])
    shared = {
        "w_in": f("w_in"), "w_out": f("w_out"), "w_q": f("w_q_mem"), "w_kv": f("w_kv_mem"), "w_o": f("w_o_mem"),
        "w_r": f("w_router"), "w_gu": f("w_gate_up"), "w_d": f("w_down"),
        "b_r": f("b_router").reshape(1, NEXP), "b_gu": f("b_gate_up").reshape(NEXP * 16, 128), "b_d": f("b_down"),
        "ln_g": f("ln_g"), "ln_b": f("ln_b"), "gga": f("g_group_a").reshape(1, 512), "ggb": f("g_group_b").reshape(1, 512),
        "bmT": _bias_mask(f("rel_bias")), "cmat": _const_mats(),
        "eoff": np.ascontiguousarray(np.broadcast_to((np.arange(NEXP) * CAP).astype(np.float32)[None, :], (128, NEXP))),
    }
    maps = []
    for c in cores:
        b, r = c // 4, c % 4
        sh = 3 - r
        xbs = np.zeros((SEQ, D), np.float32)
        xbs[sh * 128:] = x[b, :SEQ - sh * 128]
        xoo = np.ascontiguousarray(x[b].reshape(NBLK, 128, D)[r::4].reshape(NOWN * 128, D))
        padm = np.zeros((128, 4), np.float32)
        for Lk in range(4):
            padm[:, Lk] = 1.0 if Lk >= sh else 0.0
        m = dict(shared)
        m.update({"xb": xbs, "xo": xoo, "memb": np.ascontiguousarray(np.asarray(inputs["mem"], np.float32)[b]), "padm": padm})
        maps.append(m)
    return maps


_NC_CACHE = {}


def kernel(**inputs):
    if "nc" not in _NC_CACHE:
        _NC_CACHE["nc"] = build_nc()
    nc = _NC_CACHE["nc"]
    maps = make_in_maps(inputs)
    res = run_bass_kernel_spmd(nc, maps, core_ids=list(range(8)))
    outp = np.zeros((2, SEQ, D), np.float32)
    for c in range(8):
        b, r = c // 4, c % 4
        o = np.asarray(res.results[c]["out"]).reshape(NOWN, 128, D)
        outp[b].reshape(NBLK, 128, D)[r::4] = o
    return outp
```

```python
import numpy as np
from contextlib import ExitStack
import concourse.bass as bass
import concourse.mybir as mybir
from concourse.bass_utils import run_bass_kernel_spmd

F32 = mybir.dt.float32
BF16 = mybir.dt.bfloat16
I32 = mybir.dt.int32
AF = mybir.ActivationFunctionType
ALU = mybir.AluOpType

D = 1024
SEQ = 8192
NBLK = 64
NOWN = 16
NEXP = 32
CAP = 512
NRB = CAP // 128
NROWS = NEXP * CAP
ALPHA = 2.0 ** 0.25
LN_EPS = 1e-5
RMS_EPS = 1e-6
BIG = float(NROWS + 4096)


class Buf:
    __slots__ = ("name", "writer", "readers")

    def __init__(self, name):
        self.name = name
        self.writer = None
        self.readers = []


class Sched:
    ENGS = ("pe", "act", "dve", "pool", "sp")

    def __init__(self):
        self.epoch = 0
        self._reset()

    def _reset(self):
        self.prog = {e: [] for e in self.ENGS}
        self.cnt = {e: 0 for e in self.ENGS}
        self.dcnt = {}
        self.seen = {e: {} for e in self.ENGS}
        self.pending = {e: False for e in self.ENGS}

    def _need(self, e, tok, waits):
        if tok is None:
            return
        ep, kind, src, val = tok
        if ep != self.epoch:
            return
        if kind == "E" and src == e and e == "pe":
            return
        k = (kind, src)
        if self.seen[e].get(k, -1) >= val:
            return
        if waits.get(k, -1) < val:
            waits[k] = val

    def _emit_waits(self, e, waits):
        for (kind, src), val in waits.items():
            self.seen[e][(kind, src)] = val
            self.prog[e].append(("wait", kind, src, val))

    def _deps(self, e, reads, writes):
        waits = {}
        for b in reads:
            self._need(e, b.writer, waits)
        for b in writes:
            self._need(e, b.writer, waits)
            for t in b.readers:
                self._need(e, t, waits)
        self._emit_waits(e, waits)

    def _mark(self, tok, reads, writes):
        for b in reads:
            if b.readers and b.readers[0][0] != self.epoch:
                b.readers = []
            b.readers.append(tok)
        for b in writes:
            b.writer = tok
            b.readers = []

    def op(self, e, fn, reads=(), writes=(), sig=True):
        self._deps(e, reads, writes)
        idx = self.cnt[e]
        if sig:
            self.cnt[e] += 1
            self.prog[e].append(("op", fn, idx))
            self.pending[e] = False
        else:
            self.prog[e].append(("opn", fn, idx))
            self.pending[e] = True
        self._mark((self.epoch, "E", e, idx), reads, writes)

    def dma(self, e, key, fn, reads=(), writes=()):
        self._deps(e, reads, writes)
        v = self.dcnt.get(key, 0) + 16
        self.dcnt[key] = v
        self.prog[e].append(("dma", fn, key))
        self._mark((self.epoch, "D", key, v), reads, writes)

    def wait_all(self, e):
        waits = {}
        for e2 in self.ENGS:
            if self.cnt[e2] > 0:
                self._need(e, (self.epoch, "E", e2, self.cnt[e2] - 1), waits)
        for key, v in self.dcnt.items():
            self._need(e, (self.epoch, "D", key, v), waits)
        self._emit_waits(e, waits)

    def flush(self, nc, stack=None):
        assert not any(self.pending.values()), self.pending
        for e in self.ENGS:
            self.wait_all(e)
        ep = self.epoch
        CH = 2000
        with ExitStack() as sst:
            esem = {e: [sst.enter_context(nc.semaphore(f"s{ep}_{e}{i}")) for i in range(self.cnt[e] // CH + 1)]
                    for e in self.ENGS}
            dsem = {k: sst.enter_context(nc.semaphore(f"d{ep}_{k}")) for k in self.dcnt}
            prog = self.prog

            def run(e, eng):
                for it in prog[e]:
                    if it[0] == "wait":
                        _, kind, src, val = it
                        if kind == "E":
                            eng.wait_ge(esem[src][val // CH], val % CH + 1)
                        else:
                            eng.wait_ge(dsem[src], val)
                    elif it[0] == "op":
                        it[1](eng).then_inc(esem[e][it[2] // CH], 1)
                    elif it[0] == "opn":
                        it[1](eng)
                    else:
                        it[1](eng).then_inc(dsem[it[2]], 16)

            with nc.Block() as block:
                @block.tensor
                def _(eng):
                    run("pe", eng)

                @block.scalar
                def _(eng):
                    run("act", eng)

                @block.vector
                def _(eng):
                    run("dve", eng)

                @block.gpsimd
                def _(eng):
                    run("pool", eng)

                @block.sync
                def _(eng):
                    run("sp", eng)
            allsems = [x for l in esem.values() for x in l] + list(dsem.values())
            with nc.Block() as block:
                @block.sync
                def _(eng):
                    for x in allsems:
                        eng.sem_clear(x)
        self.epoch += 1
        self._reset()


def build_nc(dbg=False, stop_after=99):
    nc = bass.Bass("TRN2", target_bir_lowering=False)
    S = Sched()

    def din(name, shape, dt=F32):
        return nc.dram_tensor(name, list(shape), dt, kind="ExternalInput").ap()

    def dscr(name, shape, dt):
        return nc.dram_tensor(name, list(shape), dt).ap()

    xb = din("xb", [SEQ, D])
    xo = din("xo", [NOWN * 128, D])
    memb = din("memb", [256, D])
    w_in = din("w_in", [D, 3072])
    w_out = din("w_out", [D, D])
    w_q = din("w_q", [D, D])
    w_kv = din("w_kv", [D, 2 * D])
    w_o = din("w_o", [D, D])
    w_r = din("w_r", [D, NEXP])
    w_gu = din("w_gu", [NEXP, D, 2 * D]) if (stop_after >= 4 and stop_after != 15) else None
    w_d = din("w_d", [NEXP, D, D]) if (stop_after >= 4 and stop_after != 15) else None
    b_r = din("b_r", [1, NEXP])
    b_gu = din("b_gu", [NEXP * 16, 128])
    b_d = din("b_d", [NEXP, D])
    ln_g = din("ln_g", [3, D])
    ln_b = din("ln_b", [3, D])
    gga = din("gga", [1, 512])
    ggb = din("ggb", [1, 512])
    bmT = din("bmT", [128, 5 * 8 * 128])
    cmat = din("cmat", [128, 5 * 128])
    padm = din("padm", [128, 4])
    eoff = din("eoff", [128, NEXP])
    out = nc.dram_tensor("out", [NOWN * 128, D], F32, kind="ExternalOutput").ap()
    dbg_o = {}
    if dbg:
        for nm, shp in (("d_oa", [NOWN * 128, 512]), ("d_ob", [NOWN * 128, 512]), ("d_x1", [NOWN * 128, D]),
                        ("d_x2", [NOWN * 128, D]), ("d_lg", [NOWN * 128, NEXP]), ("d_idx", [NOWN * 128, 4]),
                        ("d_gate", [NOWN * 128, 4])):
            dbg_o[nm] = nc.dram_tensor(nm, shp, F32, kind="ExternalOutput").ap()

    KT_s = dscr("KT_s", [8, 128, SEQ], BF16)
    VA_s = dscr("VA_s", [NBLK, 128, 8 * 65], BF16)
    VB_s = dscr("VB_s", [NBLK, 128, 512], BF16)
    X2S = dscr("X2S", [NOWN * 128, D], F32)
    XS = dscr("XS", [NROWS + 128, D], BF16)
    YS = dscr("YS", [NROWS + 128, D], F32)
    b_KT = [Buf(f"KT{c}") for c in range(8)]
    b_VA, b_VB, b_X2S, b_XS, b_YS = Buf("VA"), Buf("VB"), Buf("X2S"), Buf("XS"), Buf("YS")

    with ExitStack() as top:
        def sb(name, shape, dt, stack=top):
            return stack.enter_context(nc.sbuf_tensor(name, list(shape), dt))

        def pst(name, shape, dt, stack):
            return stack.enter_context(nc.psum_tensor(name, list(shape), dt))

        def OP(e, name, reads, writes, *a, sig=True, **kw):
            S.op(e, lambda eng: getattr(eng, name)(*a, **kw), reads, writes, sig=sig)

        def DMA(e, key, reads, writes, o, i, **kw):
            S.dma(e, key, lambda eng: eng.dma_start(out=o, in_=i, **kw), reads, writes)

        cm = sb("cm", [128, 640], F32)
        cmb = sb("cmb", [128, 640], BF16)
        padt = sb("padt", [128, 4], F32)
        eofft = sb("eofft", [128, NEXP], F32)
        tril8 = sb("tril8", [128, 1024], F32)
        b_cm, b_cmb, b_padt, b_eoff, b_tril8 = Buf("cm"), Buf("cmb"), Buf("padt"), Buf("eoff"), Buf("tril8")
        DMA("sp", "c_cm", [], [b_cm], cm[:], cmat)
        DMA("sp", "c_padt", [], [b_padt], padt[:], padm)
        DMA("sp", "c_eoff", [], [b_eoff], eofft[:], eoff)
        OP("dve", "tensor_copy", [b_cm], [b_cmb], cmb[:], cm[:])
        for h in range(8):
            OP("pool", "tensor_copy", [b_cm], [b_tril8], tril8[:, h * 128:(h + 1) * 128], cm[:, 384:512])
        identF = cm[:, 0:128]
        identB = cmb[:, 0:128]
        triU = cmb[:, 128:256]
        comp = cmb[:, 256:384]
        onesB = cmb[:, 512:640]

        GT = sb("GT", [128, NOWN, 4], F32)
        IDX = sb("IDX", [128, NOWN, 4], I32)
        oas = ExitStack()
        OA = sb("OA", [128, NOWN, 512], BF16, oas)
        OB = sb("OB", [128, NOWN, 512], BF16, oas)
        qs = ExitStack()
        QT = sb("QT", [128, 4, NOWN * 128], BF16, qs)
        QTB = sb("QTB", [128, 4, NOWN, 256], BF16, qs)
        b_QT = [Buf(f"QT{c}") for c in range(8)]
        b_OA = [Buf(f"OA{j}") for j in range(NOWN)]
        b_OB = [Buf(f"OB{j}") for j in range(NOWN)]

        with ExitStack() as ph:
            win = sb("win", [128, 8, 3072], BF16, ph)
            b_win = Buf("win")
            for k in range(8):
                DMA("pool", "win", [], [b_win], win[:, k, :], w_in[k * 128:(k + 1) * 128, :])
            xs = [sb(f"xs{i}", [128, 4, D], F32, ph) for i in range(2)]
            b_xs = [Buf(f"xs{i}") for i in range(2)]
            xT = [sb(f"xT{i}", [128, 8, 512], BF16, ph) for i in range(2)]
            b_xT = [Buf(f"xT{i}") for i in range(2)]
            kst = [sb(f"kst{i}", [128, 8, 512], BF16, ph) for i in range(1)] * 2
            b_kst = [Buf("kst0")] * 2
            vsa = [sb(f"vsa{i}", [128, 4, 8, 65], BF16, ph) for i in range(1)] * 2
            vsb = [sb(f"vsb{i}", [128, 4, 512], BF16, ph) for i in range(1)] * 2
            b_vsa = [Buf("vsa0")] * 2
            b_vsb = [Buf("vsb0")] * 2
            OP("pool", "memset", [], [b_vsa[0]], vsa[0][:], 1.0)
            OP("pool", "memset", [], b_QT[4:8], QTB[:], 0.0)
            tp = [pst(f"tp{i}", [128, 1024], F32, ph) for i in range(2)]
            b_tp = [Buf(f"tp{i}") for i in range(2)]
            mm = [pst(f"mm{i}", [128, 512], F32, ph) for i in range(4)]
            b_mm = [Buf(f"mm{i}") for i in range(4)]
            cnt = {"tp": 0, "mm": 0, "ev": 0}

            def transpose_block(src_ap_fn, dstT, b_src, b_dst, col0):
                i = cnt["tp"] % 2
                cnt["tp"] += 1
                for k in range(8):
                    OP("pe", "transpose", [b_src, b_cm], [b_tp[i]], out=tp[i][:, k * 128:(k + 1) * 128],
                       in_=src_ap_fn(k), identity=identF, sig=(k == 7))
                eng = "act" if cnt["tp"] % 2 else "dve"
                if eng == "act":
                    OP("act", "copy", [b_tp[i]], [b_dst], out=dstT[:, :, col0:col0 + 128],
                       in_=tp[i][:].rearrange("p (k n) -> p k n", k=8))
                else:
                    OP("dve", "tensor_copy", [b_tp[i]], [b_dst], out=dstT[:, :, col0:col0 + 128],
                       in_=tp[i][:].rearrange("p (k n) -> p k n", k=8))

            def evac(dst_ap, src_ap, b_src, b_dst, scale=None):
                cnt["ev"] += 1
                if scale is not None:
                    OP("act", "mul", [b_src], [b_dst], out=dst_ap, in_=src_ap, mul=scale)
                elif cnt["ev"] % 2:
                    OP("act", "copy", [b_src], [b_dst], out=dst_ap, in_=src_ap)
                else:
                    OP("dve", "tensor_copy", [b_src], [b_dst], out=dst_ap, in_=src_ap)

            KCOLS = [512 + c * 128 for c in range(4)] + [2048 + c * 128 for c in range(4)]
            QCOLS = [c * 128 for c in range(4)] + [1536 + c * 128 for c in range(4)]
            for g in range(NBLK // 4):
                i2 = g % 2
                DMA("sp", f"xs{i2}", [], [b_xs[i2]], xs[i2][:],
                    xb[g * 512:(g + 1) * 512, :].rearrange("(a p) n -> p a n", p=128))
                for a in range(4):
                    transpose_block(lambda k, a=a, i2=i2: xs[i2][:, a, k * 128:(k + 1) * 128], xT[i2], b_xs[i2], b_xT[i2], a * 128)
                for c in range(8):
                    m = cnt["mm"] % 4
                    cnt["mm"] += 1
                    for k in range(8):
                        OP("pe", "matmul", [b_win, b_xT[i2]], [b_mm[m]], mm[m][:], lhsT=win[:, k, KCOLS[c]:KCOLS[c] + 128],
                           rhs=xT[i2][:, k, :], start=(k == 0), stop=(k == 7), sig=(k == 7))
                    evac(kst[i2][:, c, :], mm[m][:], b_mm[m], b_kst[i2])
                DMA("sp", "kst0", [b_kst[i2]], b_KT, KT_s[:, :, g * 512:(g + 1) * 512].rearrange("c p n -> p c n"), kst[i2][:])
                for a in range(4):
                    for vg in range(2):
                        m = cnt["mm"] % 4
                        cnt["mm"] += 1
                        c0 = 1024 if vg == 0 else 2560
                        for k in range(8):
                            OP("pe", "matmul", [b_win, b_xT[i2]], [b_mm[m]], mm[m][:], lhsT=xT[i2][:, k, a * 128:(a + 1) * 128],
                               rhs=win[:, k, c0:c0 + 512], start=(k == 0), stop=(k == 7), sig=(k == 7))
                        if vg == 0:
                            evac(vsa[i2][:, a, :, 0:64], mm[m][:].rearrange("p (h d) -> p h d", h=8), b_mm[m], b_vsa[i2])
                        else:
                            evac(vsb[i2][:, a, :], mm[m][:], b_mm[m], b_vsb[i2])
                DMA("sp", "vsa0", [b_vsa[i2]], [b_VA], VA_s[g * 4:(g + 1) * 4, :, :].rearrange("b p n -> p b n"),
                    vsa[i2][:].rearrange("p a h d -> p a (h d)"))
                DMA("sp", "vsb0", [b_vsb[i2]], [b_VB], VB_s[g * 4:(g + 1) * 4, :, :].rearrange("b p n -> p b n"), vsb[i2][:])
            for g in range(NOWN // 4):
                i2 = g % 2
                DMA("sp", f"xs{i2}", [], [b_xs[i2]], xs[i2][:],
                    xo[g * 512:(g + 1) * 512, :].rearrange("(a p) n -> p a n", p=128))
                for a in range(4):
                    transpose_block(lambda k, a=a, i2=i2: xs[i2][:, a, k * 128:(k + 1) * 128], xT[i2], b_xs[i2], b_xT[i2], a * 128)
                for c in range(8):
                    m = cnt["mm"] % 4
                    cnt["mm"] += 1
                    for k in range(8):
                        OP("pe", "matmul", [b_win, b_xT[i2]], [b_mm[m]], mm[m][:], lhsT=win[:, k, QCOLS[c]:QCOLS[c] + 128],
                           rhs=xT[i2][:, k, :], start=(k == 0), stop=(k == 7), sig=(k == 7))
                    if c < 4:
                        evac(QT[:, c, g * 512:(g + 1) * 512], mm[m][:], b_mm[m], b_QT[c], scale=0.125)
                    else:
                        OP("act", "mul", [b_mm[m]], [b_QT[c]], out=QTB[0:64, c - 4, 4 * g:4 * g + 4, 0:128],
                           in_=mm[m][0:64, :].rearrange("p (a q) -> p a q", a=4), mul=0.125)
                        OP("act", "mul", [b_mm[m]], [b_QT[c]], out=QTB[64:128, c - 4, 4 * g:4 * g + 4, 128:256],
                           in_=mm[m][64:128, :].rearrange("p (a q) -> p a q", a=4), mul=0.125)
            S.flush(nc, top)

        if stop_after <= 1:
            with ExitStack() as ph:
                t32 = sb("t32q", [128, 4, NOWN * 128], F32, ph)
                b_t32 = Buf("t32q")
                OP("dve", "tensor_copy", b_QT, [b_t32], out=t32[:], in_=QT[:])
                DMA("sp", "dbgq", [b_t32], [], dbg_o["d_ob"].rearrange("(c p) n -> p c n", p=128)[:, 0:4, 0:512], t32[:, :, 0:512])
                S.flush(nc, top)
            qs.close()
            oas.close()
            return nc
        with ExitStack() as ph:
            bm = sb("bm", [128, 5, 8, 128], BF16, ph)
            b_bm = Buf("bm")
            zt = sb("zt", [128, NRB * D], BF16, ph)
            b_zt = Buf("zt")
            OP("pool", "memset", [], [b_zt], zt[:], 0.0)
            for e in range(NEXP):
                DMA("sp", "zx", [b_zt], [b_XS], XS[e * CAP:(e + 1) * CAP, :].rearrange("(a p) n -> p a n", p=128),
                    zt[:].rearrange("p (a n) -> p a n", a=NRB))
            DMA("pool", "bm", [], [b_bm], bm[:].rearrange("p o h q -> p (o h q)"), bmT)
            kta = [sb(f"kta{i}", [128, SEQ], BF16, ph) for i in range(2)]
            va = [sb(f"va{i}", [128, NBLK, 130], BF16, ph) for i in range(2)]
            b_kta = [Buf(f"kta{i}") for i in range(2)]
            b_va = [Buf(f"va{i}") for i in range(2)]
            et = [sb(f"et{i}", [128, 10, 128], BF16, ph) for i in range(2)]
            b_et = [Buf(f"et{i}") for i in range(2)]
            rd = [sb(f"rd{i}", [128, 2], F32, ph) for i in range(2)]
            b_rd = [Buf(f"rd{i}") for i in range(2)]
            sps = [[pst(f"sp{i}_{t}", [128, 512], F32, ph) for t in range(3)] for i in range(2)]
            b_sps = [[Buf(f"sp{i}_{t}") for t in range(3)] for i in range(2)]
            ops_f = [pst(f"oa{i}", [128, 512], F32, ph) for i in range(2)]
            ops_ = [t[:, 0:130].rearrange("p (h d) -> p h d", h=2) for t in ops_f]
            b_ops = [Buf(f"oa{i}") for i in range(2)]
            it = 0
            for p in range(4):
                pi = p % 2
                DMA("sp", f"kta{pi}", [b_KT[p]], [b_kta[pi]], kta[pi][:], KT_s[p, :, :])
                for q4 in range(4):
                    DMA("sp", f"va{pi}", [b_VA], [b_va[pi]], va[pi][:, q4 * 16:(q4 + 1) * 16, :],
                        VA_s[q4 * 16:(q4 + 1) * 16, :, p * 130:(p + 1) * 130].rearrange("b p n -> p b n"))
                for j in range(NOWN):
                    i2 = it % 2
                    it += 1
                    L = 4 * j + 3
                    offs = [o for o in range(5) if L - 4 + o >= 0]
                    def slot(h, o):
                        return (h, o) if o < 4 else (2, h)
                    o_lo = offs[0]
                    n4 = len([o for o in offs if o < 4])
                    for h in range(2):
                        OP("pe", "matmul", [b_bm, b_cmb], [b_sps[i2][h]],
                           sps[i2][h][:, o_lo * 128:(o_lo + n4) * 128].rearrange("p (o q) -> p o q", q=128),
                           lhsT=identB, rhs=bm[:, o_lo:o_lo + n4, 2 * p + h, :], start=True, stop=False, sig=False,
                           skip_group_check=True)
                        for o in offs:
                            if o >= 4:
                                continue
                            Lk = L - 4 + o
                            OP("pe", "matmul", [b_kta[pi], b_QT[p]], [b_sps[i2][h]], sps[i2][h][:, o * 128:(o + 1) * 128],
                               lhsT=kta[pi][64 * h:64 * h + 64, Lk * 128:(Lk + 1) * 128],
                               rhs=QT[64 * h:64 * h + 64, p, j * 128:(j + 1) * 128], start=False, stop=True,
                               sig=(o == 3), skip_group_check=True)
                        Lk = L
                        OP("pe", "matmul", [b_bm, b_cmb], [b_sps[i2][2]], sps[i2][2][:, h * 128:(h + 1) * 128],
                           lhsT=identB, rhs=bm[:, 4, 2 * p + h, :], start=True, stop=False, sig=False, skip_group_check=True)
                        OP("pe", "matmul", [b_kta[pi], b_QT[p]], [b_sps[i2][2]], sps[i2][2][:, h * 128:(h + 1) * 128],
                           lhsT=kta[pi][64 * h:64 * h + 64, Lk * 128:(Lk + 1) * 128],
                           rhs=QT[64 * h:64 * h + 64, p, j * 128:(j + 1) * 128], start=False, stop=True,
                           skip_group_check=True)
                    for h in range(2):
                        o_lo = offs[0]
                        n4 = len([o for o in offs if o < 4])
                        OP("act", "activation", [b_sps[i2][h]], [b_et[i2]],
                           out=et[i2][:, h * 5 + o_lo:h * 5 + o_lo + n4, :],
                           in_=sps[i2][h][:, o_lo * 128:(o_lo + n4) * 128].rearrange("p (o q) -> p o q", q=128), func=AF.Exp)
                    for h in range(2):
                        OP("act", "activation", [b_sps[i2][2]], [b_et[i2]], out=et[i2][:, h * 5 + 4, :],
                           in_=sps[i2][2][:, h * 128:(h + 1) * 128], func=AF.Exp)
                    if j == 0:
                        for h in range(2):
                            for o in offs:
                                Lk = L - 4 + o
                                if Lk < 3:
                                    OP("dve", "tensor_scalar", [b_et[i2], b_padt], [b_et[i2]], out=et[i2][:, h * 5 + o, :],
                                       in0=et[i2][:, h * 5 + o, :], scalar1=padt[:, Lk:Lk + 1], scalar2=None, op0=ALU.mult)
                    for h in range(2):
                        for n, o in enumerate(offs):
                            Lk = L - 4 + o
                            OP("pe", "matmul", [b_et[i2], b_va[pi]], [b_ops[i2]], ops_[i2][:, h, :],
                               lhsT=et[i2][:, h * 5 + o, :], rhs=va[pi][:, Lk, h * 65:(h + 1) * 65],
                               start=(n == 0), stop=(n == len(offs) - 1), sig=(n == len(offs) - 1))
                    OP("dve", "reciprocal", [b_ops[i2]], [b_rd[i2]], out=rd[i2][:], in_=ops_[i2][:, :, 64])
                    for h in range(2):
                        hh = 2 * p + h
                        OP("dve", "tensor_scalar", [b_ops[i2], b_rd[i2]], [b_OA[j]], out=OA[:, j, hh * 64:(hh + 1) * 64],
                           in0=ops_[i2][:, h, 0:64], scalar1=rd[i2][:, h:h + 1], scalar2=None, op0=ALU.mult)
            S.flush(nc, top)

        if stop_after == 15:
            with ExitStack() as ph:
                t32 = sb("t32", [128, NOWN, 512], F32, ph)
                b_t32 = Buf("t32")
                OP("dve", "tensor_copy", b_OA, [b_t32], out=t32[:], in_=OA[:])
                DMA("sp", "dbg", [b_t32], [], dbg_o["d_oa"].rearrange("(j p) n -> p j n", p=128), t32[:])
                S.flush(nc, top)
            qs.close()
            oas.close()
            return nc
        with ExitStack() as ph:
            ktb = sb("ktb", [128, 2, SEQ], BF16, ph)
            vb = sb("vb", [128, NBLK, 256], BF16, ph)
            b_ktb, b_vb = Buf("ktb"), Buf("vb")
            ee = [sb(f"ee{i}", [128, 512], F32, ph) for i in range(4)]
            ll = [sb(f"ll{i}", [128, 512], BF16, ph) for i in range(4)]
            ex = [sb(f"ex{i}", [128, 512], F32, ph) for i in range(3)]
            at = [sb(f"at{i}", [128, 512], BF16, ph) for i in range(3)]
            b_ee = [Buf(f"ee{i}") for i in range(4)]
            b_ll = [Buf(f"ll{i}") for i in range(4)]
            b_ex = [Buf(f"ex{i}") for i in range(3)]
            b_at = [Buf(f"at{i}") for i in range(3)]
            zp = [pst(f"zp{i}", [128, 512], F32, ph) for i in range(3)]
            b_zp = [Buf(f"zp{i}") for i in range(3)]
            cp = [pst(f"cp{i}", [128, 512], F32, ph) for i in range(2)]
            b_cp = [Buf(f"cp{i}") for i in range(2)]
            op_ = [pst(f"ob{i}", [128, 512], F32, ph) for i in range(2)]
            b_op = [Buf(f"ob{i}") for i in range(2)]
            import os
            SB_G = int(os.environ.get("SB_G", "2"))
            SB_J = int(os.environ.get("SB_J", str(NOWN)))
            SB_PIPE = int(os.environ.get("SB_PIPE", "1"))
            for G in range(SB_G):
                DMA("sp", "ktb", [b_KT[4 + 2 * G], b_KT[5 + 2 * G]], [b_ktb], ktb[:],
                    KT_s[4 + 2 * G:6 + 2 * G, :, :].rearrange("c p n -> p c n"))
                for q4 in range(4):
                    DMA("sp", "vb", [b_VB], [b_vb], vb[:, q4 * 16:(q4 + 1) * 16, :],
                        VB_s[q4 * 16:(q4 + 1) * 16, :, G * 256:(G + 1) * 256].rearrange("b p n -> p b n"))
                steps = []
                for j in range(SB_J):
                    L = 4 * j + 3
                    for n, Lk in enumerate(range(L, -1, -1)):
                        steps.append((j, Lk, n == 0, Lk == 0))

                def stA(n, G=G):
                    j, Lk, first, last = steps[n]
                    s3, s4 = n % 3, n % 4
                    for pl in range(2):
                        OP("pe", "matmul", [b_ktb, b_QT[4 + 2 * G + pl]], [b_zp[s3]], zp[s3][:, pl * 256:(pl + 1) * 256],
                           lhsT=ktb[:, pl, Lk * 128:(Lk + 1) * 128], rhs=QTB[:, 2 * G + pl, j, :],
                           start=True, stop=True, sig=(pl == 1))
                    OP("act", "activation", [b_zp[s3]], [b_ee[s4]], out=ee[s4][:], in_=zp[s3][:], func=AF.Exp)
                    if first:
                        OP("dve", "tensor_tensor", [b_ee[s4], b_tril8], [b_ee[s4]], out=ee[s4][:], in0=ee[s4][:],
                           in1=tril8[:, 0:512], op=ALU.mult)
                    elif Lk < 3:
                        OP("dve", "tensor_scalar", [b_ee[s4], b_padt], [b_ee[s4]], out=ee[s4][:], in0=ee[s4][:],
                           scalar1=padt[:, Lk:Lk + 1], scalar2=None, op0=ALU.mult)
                    OP("act", "activation", [b_ee[s4]], [b_ll[s4]], out=ll[s4][:], in_=ee[s4][:], func=AF.Ln, bias=1.0)

                def stU(n, G=G):
                    j, Lk, first, last = steps[n]
                    s4, s2, ci = n % 4, n % 2, j % 2
                    OP("pe", "matmul", [b_ll[s4], b_cmb], [b_cp[ci]], cp[ci][:], lhsT=triU, rhs=ll[s4][:],
                       start=first, stop=True, skip_group_check=True)
                    OP("act", "activation", [b_cp[ci]], [b_ex[s2]], out=ex[s2][:], in_=cp[ci][:], func=AF.Exp, scale=-1.0)

                def stC(n, G=G):
                    j, Lk, first, last = steps[n]
                    s4, ci = n % 4, j % 2
                    if not last:
                        OP("pe", "matmul", [b_ll[s4], b_cmb], [b_cp[ci]], cp[ci][:], lhsT=comp, rhs=ll[s4][:],
                           start=False, stop=True, skip_group_check=True)

                def stV(n, G=G):
                    j, Lk, first, last = steps[n]
                    s4, s2, ci = n % 4, n % 2, j % 2
                    OP("dve", "tensor_tensor", [b_ee[s4], b_ex[s2]], [b_at[s2]], out=at[s2][:], in0=ee[s4][:],
                       in1=ex[s2][:], op=ALU.mult)
                    for hl in range(4):
                        OP("pe", "matmul", [b_at[s2], b_vb], [b_op[ci]], op_[ci][:, hl * 64:(hl + 1) * 64],
                           lhsT=at[s2][:, hl * 128:(hl + 1) * 128], rhs=vb[:, Lk, hl * 64:(hl + 1) * 64],
                           start=(first and hl == 0), stop=last, skip_group_check=True, sig=(hl == 3))
                    if last:
                        OP("dve", "tensor_copy", [b_op[ci]], [b_OB[j]], out=OB[:, j, G * 256:(G + 1) * 256], in_=op_[ci][:, 0:256])

                NS = len(steps)
                for n in range(min(3, NS)):
                    stA(n)
                stU(0)
                for n in range(NS):
                    stC(n)
                    if n + 1 < NS:
                        stU(n + 1)
                    stV(n)
                    if n + 3 < NS:
                        stA(n + 3)
            S.flush(nc, top)

        qs.close()
        if dbg:
            with ExitStack() as ph:
                t32 = sb("t32", [128, NOWN, 512], F32, ph)
                b_t32 = Buf("t32")
                for nm, src, bsrc in (("d_oa", OA, b_OA), ("d_ob", OB, b_OB)):
                    OP("dve", "tensor_copy", bsrc, [b_t32], out=t32[:], in_=src[:])
                    DMA("sp", "dbg", [b_t32], [], dbg_o[nm].rearrange("(j p) n -> p j n", p=128), t32[:])
                S.flush(nc, top)

        if stop_after <= 2:
            oas.close()
            return nc
        b_GT = [Buf(f"GT{j}") for j in range(NOWN)]
        b_IDX = [Buf(f"IDX{j}") for j in range(NOWN)]
        with ExitStack() as ph:
            wout = sb("wout", [128, 8, D], BF16, ph)
            wq = sb("wq", [128, 8, D], BF16, ph)
            wo = sb("wo", [128, 8, D], BF16, ph)
            wr = sb("wr", [128, 8, NEXP], F32, ph)
            b_wout, b_wq, b_wo, b_wkv, b_wr = Buf("wout"), Buf("wq"), Buf("wo"), Buf("wkv"), Buf("wr")
            for k in range(8):
                DMA("pool", "w_wout", [], [b_wout], wout[:, k, :], w_out[k * 128:(k + 1) * 128, :])
                DMA("pool", "w_wq", [], [b_wq], wq[:, k, :], w_q[k * 128:(k + 1) * 128, :])
                DMA("pool", "w_wo", [], [b_wo], wo[:, k, :], w_o[k * 128:(k + 1) * 128, :])
            DMA("sp", "w_wr", [], [b_wr], wr[:], w_r.rearrange("(k p) n -> p k n", p=128))
            lng = sb("lng", [128, 2, D], F32, ph)
            lnb = sb("lnb", [128, 2, D], F32, ph)
            ggt = sb("ggt", [128, 2, 512], F32, ph)
            brt = sb("brt", [128, NEXP], F32, ph)
            b_vec = Buf("vec")
            for i in range(2):
                DMA("sp", "w_vec", [], [b_vec], lng[:, i, :], ln_g[i:i + 1, :].partition_broadcast(128))
                DMA("sp", "w_vec", [], [b_vec], lnb[:, i, :], ln_b[i:i + 1, :].partition_broadcast(128))
            DMA("sp", "w_vec", [], [b_vec], ggt[:, 0, :], gga.partition_broadcast(128))
            DMA("sp", "w_vec", [], [b_vec], ggt[:, 1, :], ggb.partition_broadcast(128))
            DMA("sp", "w_vec", [], [b_vec], brt[:], b_r.partition_broadcast(128))

            p3 = [pst(f"p3_{i}", [128, 512], F32, ph) for i in range(6)]
            b_p3 = [Buf(f"p3_{i}") for i in range(6)]
            pbt = pst("pbt", [128, 1024], BF16, ph)
            b_pbt = Buf("pbt")
            pcnt = {"i": 0}

            def bank():
                i = pcnt["i"] % 6
                pcnt["i"] += 1
                return p3[i], b_p3[i]

            KmT = sb("KmT", [128, 8, 256], BF16, ph)
            Vm = sb("Vm", [128, 2, D], BF16, ph)
            cntt = sb("cntt", [128, NEXP], F32, ph)
            kvs = ExitStack()
            wkv = sb("wkv", [128, 8, 2 * D], BF16, kvs)
            mems = sb("mems", [128, 2, D], F32, kvs)
            memT = sb("memT", [128, 8, 256], BF16, kvs)
            for k in range(8):
                DMA("pool", "w_wkv", [], [b_wkv], wkv[:, k, :], w_kv[k * 128:(k + 1) * 128, :])
            b_mems, b_memT, b_KmT, b_Vm = Buf("mems"), Buf("memT"), Buf("KmT"), Buf("Vm")
            DMA("sp", "w_mems", [], [b_mems], mems[:], memb.rearrange("(a p) n -> p a n", p=128))
            for a in range(2):
                for hf in range(2):
                    pb, bb = bank()
                    for k4 in range(4):
                        k = hf * 4 + k4
                        OP("pe", "transpose", [b_mems, b_cm], [bb], out=pb[:, k4 * 128:(k4 + 1) * 128],
                           in_=mems[:, a, k * 128:(k + 1) * 128], identity=identF, sig=(k4 == 3))
                    OP("dve", "tensor_copy", [bb], [b_memT], out=memT[:, hf * 4:hf * 4 + 4, a * 128:(a + 1) * 128],
                       in_=pb[:].rearrange("p (k n) -> p k n", k=4))
            for c in range(8):
                pb, bb = bank()
                for k in range(8):
                    OP("pe", "matmul", [b_wkv, b_memT], [bb], pb[:, 0:256], lhsT=wkv[:, k, c * 128:(c + 1) * 128],
                       rhs=memT[:, k, :], start=(k == 0), stop=(k == 7), sig=(k == 7))
                OP("act", "copy", [bb], [b_KmT], out=KmT[:, c, :], in_=pb[:, 0:256])
            for a in range(2):
                for hf in range(2):
                    pb, bb = bank()
                    for k in range(8):
                        OP("pe", "matmul", [b_wkv, b_memT], [bb], pb[:], lhsT=memT[:, k, a * 128:(a + 1) * 128],
                           rhs=wkv[:, k, D + hf * 512:D + (hf + 1) * 512], start=(k == 0), stop=(k == 7), sig=(k == 7))
                    OP("dve", "tensor_copy", [bb], [b_Vm], out=Vm[:, a, hf * 512:(hf + 1) * 512], in_=pb[:])

            S.flush(nc, top)
            kvs.close()
            def mk(name, shape, dt, n=2):
                return [sb(f"{name}{i}", shape, dt, ph) for i in range(n)], [Buf(f"{name}{i}") for i in range(n)]
            xoj, b_xoj = mk("xoj", [128, D], F32)
            cat, b_cat = mk("cat", [128, D], BF16)
            catT, b_catT = mk("catT", [128, 8, 128], BF16)
            yy, b_yy = mk("yy", [128, D], F32)
            x1, b_x1 = mk("x1", [128, D], F32)
            x1T, b_x1T = mk("x1T", [128, 8, 128], BF16)
            qT, b_qT = mk("qT", [128, 8, 128], BF16)
            eT, b_eT = mk("eT", [128, 8, 128], BF16)
            rdn, b_rdn = mk("rdn", [128, 512], F32)
            oT, b_oT = mk("oT", [128, 8, 128], BF16)
            x2, b_x2 = mk("x2", [128, D], F32)
            x2b, b_x2b = mk("x2b", [128, D], BF16)
            x2T, b_x2T = mk("x2T", [128, 8, 128], F32)
            junk, b_junk = mk("junk", [128, D], F32)
            sm, b_sm = mk("sm", [128, 64], F32)
            lg, b_lg = mk("lg", [128, 8, NEXP], F32)
            mselb, b_mselb = mk("mselb", [128, NEXP], BF16)
            idxf, b_idxf = mk("idxf", [128, 4], F32)
            b_cntt = Buf("cntt")
            OP("pool", "memset", [], [b_cntt], cntt[:], 0.0)

            def layer_norm(i2, src, b_src, dst, b_dst, li):
                s_ = sm[i2]
                bs = b_sm[i2]
                OP("dve", "memset", [], [bs], s_[:, 0:8], 0.0)
                OP("act", "activation", [b_src, bs], [b_junk[i2], bs], out=junk[i2][:], in_=src[:], func=AF.Copy,
                   accum_out=s_[:, 0:1])
                OP("act", "activation", [b_src, bs], [b_junk[i2], bs], out=junk[i2][:], in_=src[:], func=AF.Square,
                   accum_out=s_[:, 1:2])
                OP("dve", "tensor_scalar", [bs], [bs], out=s_[:, 2:4], in0=s_[:, 0:2], scalar1=1.0 / D, scalar2=None, op0=ALU.mult)
                OP("dve", "tensor_tensor", [bs], [bs], out=s_[:, 4:5], in0=s_[:, 2:3], in1=s_[:, 2:3], op=ALU.mult)
                OP("dve", "tensor_tensor", [bs], [bs], out=s_[:, 5:6], in0=s_[:, 3:4], in1=s_[:, 4:5], op=ALU.subtract)
                OP("act", "activation", [bs], [bs], out=s_[:, 6:7], in_=s_[:, 5:6], func=AF.Ln, bias=LN_EPS)
                OP("act", "activation", [bs], [bs], out=s_[:, 7:8], in_=s_[:, 6:7], func=AF.Exp, scale=-0.5)
                OP("dve", "tensor_scalar", [b_src, bs], [b_dst], out=dst[:], in0=src[:], scalar1=s_[:, 2:3], scalar2=s_[:, 7:8],
                   op0=ALU.subtract, op1=ALU.mult)
                OP("pool", "tensor_tensor", [b_dst, b_vec], [b_dst], out=dst[:], in0=dst[:], in1=lng[:, li, :], op=ALU.mult)
                OP("pool", "tensor_tensor", [b_dst, b_vec], [b_dst], out=dst[:], in0=dst[:], in1=lnb[:, li, :], op=ALU.add)

            for j in range(NOWN):
                i2 = j % 2
                s_ = sm[i2]
                bs = b_sm[i2]
                DMA("sp", f"xoj{i2}", [], [b_xoj[i2]], xoj[i2][:], xo[j * 128:(j + 1) * 128, :])
                OP("dve", "memset", [], [bs], s_[:, 16:24], 0.0)
                for gi, (O_, bO) in enumerate(((OA, b_OA[j]), (OB, b_OB[j]))):
                    OP("act", "activation", [bO, bs], [b_junk[i2], bs], out=junk[i2][:, 0:512], in_=O_[:, j, :], func=AF.Square,
                       accum_out=s_[:, 16 + gi:17 + gi])
                OP("act", "activation", [bs], [bs], out=s_[:, 18:20], in_=s_[:, 16:18], func=AF.Ln, scale=1.0 / 512, bias=RMS_EPS)
                OP("act", "activation", [bs], [bs], out=s_[:, 20:22], in_=s_[:, 18:20], func=AF.Exp, scale=-0.5)
                for gi, (O_, bO) in enumerate(((OA, b_OA[j]), (OB, b_OB[j]))):
                    OP("dve", "scalar_tensor_tensor", [bO, bs, b_vec], [b_cat[i2]], out=cat[i2][:, gi * 512:(gi + 1) * 512],
                       in0=O_[:, j, :], scalar=s_[:, 20 + gi:21 + gi], in1=ggt[:, gi, :], op0=ALU.mult, op1=ALU.mult)
                for k in range(8):
                    OP("pe", "transpose", [b_cat[i2], b_cmb], [b_pbt], out=pbt[:, k * 128:(k + 1) * 128],
                       in_=cat[i2][:, k * 128:(k + 1) * 128], identity=identB, sig=(k == 7))
                OP("dve", "tensor_copy", [b_pbt], [b_catT[i2]], out=catT[i2][:], in_=pbt[:].rearrange("p (k n) -> p k n", k=8))
                for hf in range(2):
                    pb, bb = bank()
                    for k in range(8):
                        OP("pe", "matmul", [b_catT[i2], b_wout], [bb], pb[:], lhsT=catT[i2][:, k, :],
                           rhs=wout[:, k, hf * 512:(hf + 1) * 512], start=(k == 0), stop=(k == 7), sig=(k == 7))
                    OP("dve", "scalar_tensor_tensor", [b_xoj[i2], bb], [b_yy[i2]], out=yy[i2][:, hf * 512:(hf + 1) * 512],
                       in0=xoj[i2][:, hf * 512:(hf + 1) * 512], scalar=ALPHA, in1=pb[:], op0=ALU.mult, op1=ALU.add)
                layer_norm(i2, yy[i2], b_yy[i2], x1[i2], b_x1[i2], 0)
                if dbg:
                    DMA("sp", "dbg_x1", [b_x1[i2]], [], dbg_o["d_x1"][j * 128:(j + 1) * 128, :], x1[i2][:])
                for hf in range(2):
                    pb, bb = bank()
                    for k4 in range(4):
                        k = hf * 4 + k4
                        OP("pe", "transpose", [b_x1[i2], b_cm], [bb], out=pb[:, k4 * 128:(k4 + 1) * 128],
                           in_=x1[i2][:, k * 128:(k + 1) * 128], identity=identF)
                    OP("act", "copy", [bb], [b_x1T[i2]], out=x1T[i2][:, hf * 4:hf * 4 + 4, :],
                       in_=pb[:].rearrange("p (k n) -> p k n", k=4))
                for hf in range(2):
                    pb, bb = bank()
                    for c4 in range(4):
                        c = hf * 4 + c4
                        for k in range(8):
                            OP("pe", "matmul", [b_wq, b_x1T[i2]], [bb], pb[:, c4 * 128:(c4 + 1) * 128],
                               lhsT=wq[:, k, c * 128:(c + 1) * 128], rhs=x1T[i2][:, k, :], start=(k == 0), stop=(k == 7), sig=(k == 7))
                    OP("act", "mul", [bb], [b_qT[i2]], out=qT[i2][:, hf * 4:hf * 4 + 4, :],
                       in_=pb[:].rearrange("p (k n) -> p k n", k=4), mul=1.0 / 16)
                for hf in range(2):
                    pb, bb = bank()
                    for t4 in range(4):
                        t = hf * 4 + t4
                        hd, mc = t // 2, t % 2
                        for dc in range(2):
                            OP("pe", "matmul", [b_KmT, b_qT[i2]], [bb], pb[:, t4 * 128:(t4 + 1) * 128],
                               lhsT=KmT[:, 2 * hd + dc, mc * 128:(mc + 1) * 128], rhs=qT[i2][:, 2 * hd + dc, :],
                               start=(dc == 0), stop=(dc == 1), sig=(dc == 1))
                    OP("act", "activation", [bb], [b_eT[i2]], out=eT[i2][:, hf * 4:hf * 4 + 4, :],
                       in_=pb[:].rearrange("p (k n) -> p k n", k=4), func=AF.Exp)
                pbd, bbd = bank()
                for hd in range(4):
                    for mc in range(2):
                        OP("pe", "matmul", [b_eT[i2], b_cmb], [bbd], pbd[:, hd * 128:(hd + 1) * 128], lhsT=onesB,
                           rhs=eT[i2][:, hd * 2 + mc, :], start=(mc == 0), stop=(mc == 1), sig=(mc == 1))
                OP("dve", "reciprocal", [bbd], [b_rdn[i2]], out=rdn[i2][:], in_=pbd[:])
                for hf in range(2):
                    pb, bb = bank()
                    for t4 in range(4):
                        t = hf * 4 + t4
                        hd, dc = t // 2, t % 2
                        for mc in range(2):
                            OP("pe", "matmul", [b_Vm, b_eT[i2]], [bb], pb[:, t4 * 128:(t4 + 1) * 128],
                               lhsT=Vm[:, mc, t * 128:(t + 1) * 128], rhs=eT[i2][:, hd * 2 + mc, :],
                               start=(mc == 0), stop=(mc == 1), sig=(mc == 1))
                    for t4 in range(4):
                        t = hf * 4 + t4
                        hd = t // 2
                        OP("dve", "tensor_tensor", [bb, b_rdn[i2]], [b_oT[i2]], out=oT[i2][:, t, :],
                           in0=pb[:, t4 * 128:(t4 + 1) * 128], in1=rdn[i2][:, hd * 128:(hd + 1) * 128], op=ALU.mult)
                for hf in range(2):
                    pb, bb = bank()
                    for k in range(8):
                        OP("pe", "matmul", [b_oT[i2], b_wo], [bb], pb[:], lhsT=oT[i2][:, k, :],
                           rhs=wo[:, k, hf * 512:(hf + 1) * 512], start=(k == 0), stop=(k == 7), sig=(k == 7))
                    OP("dve", "scalar_tensor_tensor", [b_x1[i2], bb], [b_yy[i2]], out=yy[i2][:, hf * 512:(hf + 1) * 512],
                       in0=x1[i2][:, hf * 512:(hf + 1) * 512], scalar=ALPHA, in1=pb[:], op0=ALU.mult, op1=ALU.add)
                layer_norm(i2, yy[i2], b_yy[i2], x2[i2], b_x2[i2], 1)
                DMA("sp", f"x2s{i2}", [b_x2[i2]], [b_X2S], X2S[j * 128:(j + 1) * 128, :], x2[i2][:])
                if dbg:
                    DMA("sp", "dbg_x2", [b_x2[i2]], [], dbg_o["d_x2"][j * 128:(j + 1) * 128, :], x2[i2][:])
                OP("act", "copy", [b_x2[i2]], [b_x2b[i2]], out=x2b[i2][:], in_=x2[i2][:])
                for hf in range(2):
                    pb, bb = bank()
                    for k4 in range(4):
                        k = hf * 4 + k4
                        OP("pe", "transpose", [b_x2[i2], b_cm], [bb], out=pb[:, k4 * 128:(k4 + 1) * 128],
                           in_=x2[i2][:, k * 128:(k + 1) * 128], identity=identF)
                    OP("dve", "tensor_copy", [bb], [b_x2T[i2]], out=x2T[i2][:, hf * 4:hf * 4 + 4, :],
                       in_=pb[:].rearrange("p (k n) -> p k n", k=4))
                pb, bb = bank()
                for k in range(8):
                    OP("pe", "matmul", [b_x2T[i2], b_wr], [bb], pb[:, 0:NEXP], lhsT=x2T[i2][:, k, :], rhs=wr[:, k, :],
                       start=(k == 0), stop=(k == 7), sig=(k == 7))
                W = lg[i2]
                bW = b_lg[i2]
                OP("dve", "tensor_tensor", [bb, b_vec], [bW], out=W[:, 0, :], in0=pb[:, 0:NEXP], in1=brt[:], op=ALU.add)
                if dbg:
                    DMA("sp", "dbg_lg", [bW], [], dbg_o["d_lg"][j * 128:(j + 1) * 128, :], W[:, 0, :])
                OP("dve", "max", [bW], [bs], out=s_[:, 32:40], in_=W[:, 0, :])
                OP("dve", "tensor_scalar", [bW, bs], [bW], out=W[:, 1, :], in0=W[:, 0, :], scalar1=s_[:, 35:36], scalar2=None,
                   op0=ALU.is_ge)
                OP("dve", "tensor_scalar", [bs], [bs], out=s_[:, 40:41], in0=s_[:, 32:33], scalar1=-1.0, scalar2=None, op0=ALU.mult)
                OP("act", "activation", [bW, bs], [bW], out=W[:, 2, :], in_=W[:, 0, :], func=AF.Exp, bias=s_[:, 40:41])
                OP("dve", "memset", [], [bs], s_[:, 41:42], 0.0)
                OP("dve", "tensor_tensor", [bW], [bW], out=W[:, 3, :], in0=W[:, 2, :], in1=W[:, 1, :], op=ALU.mult)
                OP("dve", "reduce_sum", [bW], [bs], out=s_[:, 41:42], in_=W[:, 3, :], axis=mybir.AxisListType.X)
                OP("dve", "reciprocal", [bs], [bs], out=s_[:, 42:43], in_=s_[:, 41:42])
                OP("dve", "tensor_scalar", [bW, bs], [bW], out=W[:, 3, :], in0=W[:, 3, :], scalar1=s_[:, 42:43], scalar2=None,
                   op0=ALU.mult)
                OP("dve", "tensor_copy", [bW], [b_mselb[i2]], out=mselb[i2][:], in_=W[:, 1, :])
                pb2, bb2 = bank()
                OP("pe", "matmul", [b_mselb[i2], b_cmb], [bb2], pb2[:, 0:NEXP], lhsT=comp, rhs=mselb[i2][:], start=True, stop=True)
                OP("pe", "matmul", [b_mselb[i2], b_cmb], [bb2], pb2[:, 64:64 + NEXP], lhsT=onesB, rhs=mselb[i2][:], start=True, stop=True)
                OP("dve", "tensor_tensor", [bb2, b_cntt], [bW], out=W[:, 4, :], in0=pb2[:, 0:NEXP], in1=cntt[:], op=ALU.add)
                OP("dve", "tensor_tensor", [bb2, b_cntt], [b_cntt], out=cntt[:], in0=pb2[:, 64:64 + NEXP], in1=cntt[:], op=ALU.add)
                OP("dve", "tensor_scalar", [bW], [bW], out=W[:, 5, :], in0=W[:, 4, :], scalar1=float(CAP), scalar2=BIG,
                   op0=ALU.is_ge, op1=ALU.mult)
                OP("dve", "tensor_tensor", [bW, b_eoff], [bW], out=W[:, 4, :], in0=W[:, 4, :], in1=eofft[:], op=ALU.add)
                OP("dve", "tensor_tensor", [bW], [bW], out=W[:, 4, :], in0=W[:, 4, :], in1=W[:, 5, :], op=ALU.add)
                OP("dve", "memset", [], [b_idxf[i2]], idxf[i2][:], 0.0)
                OP("dve", "memset", [], [b_GT[j]], GT[:, j, :], 0.0)
                for kk in range(4):
                    OP("dve", "tensor_scalar", [bW, bs], [bW], out=W[:, 6, :], in0=W[:, 0, :], scalar1=s_[:, 32 + kk:33 + kk],
                       scalar2=None, op0=ALU.is_equal)
                    OP("dve", "tensor_tensor", [bW], [bW], out=W[:, 7, :], in0=W[:, 6, :], in1=W[:, 4, :], op=ALU.mult)
                    OP("dve", "reduce_sum", [bW], [b_idxf[i2]], out=idxf[i2][:, kk:kk + 1], in_=W[:, 7, :], axis=mybir.AxisListType.X)
                    OP("dve", "tensor_tensor", [bW], [bW], out=W[:, 7, :], in0=W[:, 6, :], in1=W[:, 3, :], op=ALU.mult)
                    OP("dve", "reduce_sum", [bW], [b_GT[j]], out=GT[:, j, kk:kk + 1], in_=W[:, 7, :], axis=mybir.AxisListType.X)
                OP("dve", "tensor_scalar", [b_idxf[i2]], [bs], out=s_[:, 44:48], in0=idxf[i2][:], scalar1=float(NROWS), scalar2=None,
                   op0=ALU.is_lt)
                OP("dve", "tensor_tensor", [b_GT[j], bs], [b_GT[j]], out=GT[:, j, :], in0=GT[:, j, :], in1=s_[:, 44:48], op=ALU.mult)
                OP("dve", "tensor_scalar", [b_idxf[i2]], [b_idxf[i2]], out=idxf[i2][:], in0=idxf[i2][:], scalar1=float(NROWS), scalar2=None,
                   op0=ALU.min)
                OP("dve", "tensor_copy", [b_idxf[i2]], [b_IDX[j]], out=IDX[:, j, :], in_=idxf[i2][:])
                if dbg:
                    DMA("sp", "dbg_idx", [b_idxf[i2]], [], dbg_o["d_idx"][j * 128:(j + 1) * 128, :], idxf[i2][:])
                    DMA("sp", "dbg_gt", [b_GT[j]], [], dbg_o["d_gate"][j * 128:(j + 1) * 128, :], GT[:, j, :])
                for kk in range(4):
                    S.dma("pool", "scat", lambda eng, j=j, kk=kk, i2=i2: eng.indirect_dma_start(
                        out=XS[:, :], out_offset=bass.IndirectOffsetOnAxis(ap=IDX[:, j, kk:kk + 1], axis=0),
                        in_=x2b[i2][:, :], in_offset=None),
                        [b_x2b[i2], b_IDX[j]], [b_XS])
            S.flush(nc, top)

        oas.close()
        if stop_after <= 3:
            return nc
        with ExitStack() as ph:
            wgu = [sb(f"wgu{i}", [128, 8, 2 * D], BF16, ph) for i in range(2)]
            wd = [sb(f"wd{i}", [128, 8, D], BF16, ph) for i in range(2)]
            b_wgu = [[Buf(f"wgu{i}_{k}") for k in range(8)] for i in range(2)]
            b_wd = [[Buf(f"wd{i}_{k}") for k in range(8)] for i in range(2)]
            stg = [sb(f"stg{i}", [128, 2 * D], F32, ph) for i in range(3)]
            std = [sb(f"std{i}", [128, D], F32, ph) for i in range(2)]
            b_stg = [Buf(f"stg{i}") for i in range(3)]
            b_std = [Buf(f"std{i}") for i in range(2)]
            bdt = [sb(f"bdt{i}", [128, D], F32, ph) for i in range(1)] * 2
            b_bdt = [Buf("bdt0")] * 2
            xg = [sb(f"xg{i}", [128, NRB, D], BF16, ph) for i in range(1)] * 2
            b_xg = [Buf("xg0")] * 2
            xgT = [sb(f"xgT{i}", [128, 8, CAP], BF16, ph) for i in range(2)]
            b_xgT = [Buf(f"xgT{i}") for i in range(2)]
            hT = [sb(f"hT{i}", [128, 8, CAP], BF16, ph) for i in range(2)]
            b_hT = [Buf(f"hT{i}") for i in range(2)]
            gg = [sb(f"gg{i}", [128, CAP], F32, ph) for i in range(2)]
            sg = [sb(f"sg{i}", [128, CAP], F32, ph) for i in range(2)]
            uu = [sb(f"uu{i}", [128, CAP], F32, ph) for i in range(2)]
            b_gg = [Buf(f"gg{i}") for i in range(2)]
            b_sg = [Buf(f"sg{i}") for i in range(2)]
            b_uu = [Buf(f"uu{i}") for i in range(2)]
            ys = [sb(f"ys{i}", [128, D], F32, ph) for i in range(1)] * 2
            b_ys = [Buf("ys0")] * 2
            bgs = sb("bgs", [128, 4, 128], F32, ph)
            bgT = sb("bgT", [128, 512], F32, ph)
            b_bgs, b_bgT = Buf("bgs"), Buf("bgT")
            pt = [pst(f"pt{i}", [128, 1024], BF16, ph) for i in range(2)]
            b_pt = [Buf(f"pt{i}") for i in range(2)]
            pg = [pst(f"pg{i}", [128, 512], F32, ph) for i in range(4)]
            b_pg = [Buf(f"pg{i}") for i in range(4)]
            py = [pst(f"py{i}", [128, 512], F32, ph) for i in range(2)]
            b_py = [Buf(f"py{i}") for i in range(2)]
            DMA("sp", "bgs", [], [b_bgs], bgs[:], b_gu.rearrange("(a r) p -> r a p", r=128))
            for a in range(4):
                OP("pe", "transpose", [b_bgs, b_cm], [b_pg[0]], out=pg[0][:, a * 128:(a + 1) * 128], in_=bgs[:, a, :], identity=identF)
            OP("dve", "tensor_copy", [b_pg[0]], [b_bgT], out=bgT[:], in_=pg[0][:])
            bgT3 = bgT[:].rearrange("p (e c) -> p e c", c=16)
            OP("dve", "tensor_scalar", [b_bgT], [b_bgT], out=bgT3[:, :, 8:16], in0=bgT3[:, :, 8:16], scalar1=1.0, scalar2=None,
               op0=ALU.add)
            tc_ = {"t": 0, "g": 0, "y": 0}
            OP("pool", "memset", [], [b_ys[0]], ys[0][:], 0.0)
            DMA("sp", "ys0", [b_ys[0]], [b_YS], YS[NROWS:NROWS + 128, :], ys[0][:])

            def issue_gu(e, k):
                sl = k % 3
                DMA("sp", f"stg{sl}", [], [b_stg[sl]], stg[sl][:], w_gu[e, k * 128:(k + 1) * 128, :])

            def issue_d(e, k):
                sl = k % 2
                DMA("sp", f"std{sl}", [], [b_std[sl]], std[sl][:], w_d[e, k * 128:(k + 1) * 128, :])

            def cast_gu(e, k):
                sl, i = k % 3, e % 2
                if k % 2 == 0:
                    OP("act", "copy", [b_stg[sl]], [b_wgu[i][k]], out=wgu[i][:, k, :], in_=stg[sl][:])
                else:
                    OP("dve", "tensor_copy", [b_stg[sl]], [b_wgu[i][k]], out=wgu[i][:, k, :], in_=stg[sl][:])

            def cast_d(e, k):
                sl, i = k % 2, e % 2
                if k % 2 == 1:
                    OP("act", "copy", [b_std[sl]], [b_wd[i][k]], out=wd[i][:, k, :], in_=std[sl][:])
                else:
                    OP("dve", "tensor_copy", [b_std[sl]], [b_wd[i][k]], out=wd[i][:, k, :], in_=std[sl][:])

            def load_bd(e):
                i = e % 2
                DMA("sp", "bdt0", [], [b_bdt[i]], bdt[i][:], b_d[e:e + 1, :].partition_broadcast(128))

            def load_xg(e):
                i = e % 2
                DMA("sp", "xg0", [b_XS], [b_xg[i]], xg[i][:], XS[e * CAP:(e + 1) * CAP, :].rearrange("(a p) n -> p a n", p=128))

            load_bd(0)
            load_xg(0)
            for k in range(8):
                issue_gu(0, k)
                cast_gu(0, k)
            for k in range(8):
                issue_d(0, k)
                cast_d(0, k)
            for e in range(NEXP):
                i = e % 2
                nxt = e + 1 < NEXP
                if nxt:
                    for k in range(3):
                        issue_gu(e + 1, k)
                    for k in range(2):
                        issue_d(e + 1, k)
                for a in range(NRB):
                    t = tc_["t"] % 2
                    tc_["t"] += 1
                    for k in range(8):
                        OP("pe", "transpose", [b_xg[i], b_cmb], [b_pt[t]], out=pt[t][:, k * 128:(k + 1) * 128],
                           in_=xg[i][:, a, k * 128:(k + 1) * 128], identity=identB, sig=(k == 7))
                    OP("act" if a % 2 else "dve", "copy" if a % 2 else "tensor_copy", [b_pt[t]], [b_xgT[i]],
                       out=xgT[i][:, :, a * 128:(a + 1) * 128], in_=pt[t][:].rearrange("p (k n) -> p k n", k=8))
                if nxt:
                    load_xg(e + 1)
                for c in range(8):
                    g0 = tc_["g"] % 2
                    tc_["g"] += 1
                    pgg, pgu = pg[2 * g0], pg[2 * g0 + 1]
                    bgg_, bgu_ = b_pg[2 * g0], b_pg[2 * g0 + 1]
                    for k in range(8):
                        OP("pe", "matmul", [b_wgu[i][k], b_xgT[i]], [bgg_], pgg[:, 0:CAP], lhsT=wgu[i][:, k, c * 128:(c + 1) * 128],
                           rhs=xgT[i][:, k, :], start=(k == 0), stop=(k == 7), sig=(k == 7))
                    for k in range(8):
                        OP("pe", "matmul", [b_wgu[i][k], b_xgT[i]], [bgu_], pgu[:, 0:CAP], lhsT=wgu[i][:, k, D + c * 128:D + (c + 1) * 128],
                           rhs=xgT[i][:, k, :], start=(k == 0), stop=(k == 7), sig=(k == 7))
                    w2 = g0
                    OP("dve", "tensor_scalar", [bgg_, b_bgT], [b_gg[w2]], out=gg[w2][:], in0=pgg[:, 0:CAP],
                       scalar1=bgT[:, e * 16 + c:e * 16 + c + 1], scalar2=7.0, op0=ALU.add, op1=ALU.min)
                    OP("act", "activation", [b_gg[w2]], [b_sg[w2]], out=sg[w2][:], in_=gg[w2][:], func=AF.Silu, scale=1.702)
                    OP("act", "activation", [bgu_, b_bgT], [b_uu[w2]], out=uu[w2][:], in_=pgu[:, 0:CAP], func=AF.Identity,
                       bias=bgT[:, e * 16 + 8 + c:e * 16 + 8 + c + 1])
                    OP("dve", "tensor_scalar", [b_uu[w2]], [b_uu[w2]], out=uu[w2][:], in0=uu[w2][:], scalar1=-6.0, scalar2=8.0,
                       op0=ALU.max, op1=ALU.min)
                    OP("dve", "scalar_tensor_tensor", [b_uu[w2], b_sg[w2]], [b_hT[i]], out=hT[i][:, c, :], in0=uu[w2][:],
                       scalar=1.0 / 1.702, in1=sg[w2][:], op0=ALU.mult, op1=ALU.mult)
                    if nxt:
                        cast_gu(e + 1, c)
                        if c + 3 < 8:
                            issue_gu(e + 1, c + 3)
                        cast_d(e + 1, c)
                        if c + 2 < 8:
                            issue_d(e + 1, c + 2)
                for a in range(NRB):
                    yi = tc_["y"] % 2
                    tc_["y"] += 1
                    for hf in range(2):
                        for k in range(8):
                            OP("pe", "matmul", [b_hT[i], b_wd[i][k]], [b_py[hf]], py[hf][:], lhsT=hT[i][:, k, a * 128:(a + 1) * 128],
                               rhs=wd[i][:, k, hf * 512:(hf + 1) * 512], start=(k == 0), stop=(k == 7), sig=(k == 7))
                        OP("dve", "tensor_tensor", [b_py[hf], b_bdt[i]], [b_ys[yi]], out=ys[yi][:, hf * 512:(hf + 1) * 512],
                           in0=py[hf][:], in1=bdt[i][:, hf * 512:(hf + 1) * 512], op=ALU.add)
                    DMA("sp", "ys0", [b_ys[yi]], [b_YS], YS[e * CAP + a * 128:e * CAP + (a + 1) * 128, :], ys[yi][:])
                if nxt:
                    load_bd(e + 1)
            S.flush(nc, top)

        with ExitStack() as ph:
            lng3 = sb("lng3", [128, D], F32, ph)
            lnb3 = sb("lnb3", [128, D], F32, ph)
            b_v3 = Buf("v3")
            DMA("sp", "v3", [], [b_v3], lng3[:], ln_g[2:3, :].partition_broadcast(128))
            DMA("sp", "v3", [], [b_v3], lnb3[:], ln_b[2:3, :].partition_broadcast(128))
            x2r = [sb(f"x2r{i}", [128, D], F32, ph) for i in range(2)]
            yk = [[sb(f"yk{i}_{k}", [128, D], F32, ph) for k in range(4)] for i in range(2)]
            acc = [sb(f"acc{i}", [128, D], F32, ph) for i in range(2)]
            res = [sb(f"res{i}", [128, D], F32, ph) for i in range(2)]
            jk5 = [sb(f"jk5{i}", [128, D], F32, ph) for i in range(2)]
            s5 = [sb(f"s5{i}", [128, 8], F32, ph) for i in range(2)]
            b_x2r = [Buf(f"x2r{i}") for i in range(2)]
            b_yk = [[Buf(f"yk{i}_{k}") for k in range(4)] for i in range(2)]
            b_acc = [Buf(f"acc{i}") for i in range(2)]
            b_res = [Buf(f"res{i}") for i in range(2)]
            b_jk5 = [Buf(f"jk5{i}") for i in range(2)]
            b_s5 = [Buf(f"s5{i}") for i in range(2)]
            for i in range(2):
                for k in range(4):
                    OP("pool", "memset", [], [b_yk[i][k]], yk[i][k][:], 0.0)
            for j in range(NOWN):
                i2 = j % 2
                DMA("sp", f"x2r{i2}", [b_X2S], [b_x2r[i2]], x2r[i2][:], X2S[j * 128:(j + 1) * 128, :])
                for kk in range(4):
                    S.dma("pool", f"gath{i2}{kk}", lambda eng, j=j, kk=kk, i2=i2: eng.indirect_dma_start(
                        out=yk[i2][kk][:, :], out_offset=None, in_=YS[:, :],
                        in_offset=bass.IndirectOffsetOnAxis(ap=IDX[:, j, kk:kk + 1], axis=0)), [b_YS, b_IDX[j]], [b_yk[i2][kk]])
                OP("act", "mul", [b_x2r[i2]], [b_acc[i2]], out=acc[i2][:], in_=x2r[i2][:], mul=ALPHA)
                for kk in range(4):
                    OP("dve", "scalar_tensor_tensor", [b_yk[i2][kk], b_GT[j], b_acc[i2]], [b_acc[i2]], out=acc[i2][:],
                       in0=yk[i2][kk][:], scalar=GT[:, j, kk:kk + 1], in1=acc[i2][:], op0=ALU.mult, op1=ALU.add)
                s_ = s5[i2]
                bs = b_s5[i2]
                OP("dve", "memset", [], [bs], s_[:], 0.0)
                OP("act", "activation", [b_acc[i2], bs], [b_jk5[i2], bs], out=jk5[i2][:], in_=acc[i2][:], func=AF.Copy, accum_out=s_[:, 0:1])
                OP("act", "activation", [b_acc[i2], bs], [b_jk5[i2], bs], out=jk5[i2][:], in_=acc[i2][:], func=AF.Square, accum_out=s_[:, 1:2])
                OP("dve", "tensor_scalar", [bs], [bs], out=s_[:, 2:4], in0=s_[:, 0:2], scalar1=1.0 / D, scalar2=None, op0=ALU.mult)
                OP("dve", "tensor_tensor", [bs], [bs], out=s_[:, 4:5], in0=s_[:, 2:3], in1=s_[:, 2:3], op=ALU.mult)
                OP("dve", "tensor_tensor", [bs], [bs], out=s_[:, 5:6], in0=s_[:, 3:4], in1=s_[:, 4:5], op=ALU.subtract)
                OP("act", "activation", [bs], [bs], out=s_[:, 6:7], in_=s_[:, 5:6], func=AF.Ln, bias=LN_EPS)
                OP("act", "activation", [bs], [bs], out=s_[:, 7:8], in_=s_[:, 6:7], func=AF.Exp, scale=-0.5)
                OP("dve", "tensor_scalar", [b_acc[i2], bs], [b_res[i2]], out=res[i2][:], in0=acc[i2][:], scalar1=s_[:, 2:3],
                   scalar2=s_[:, 7:8], op0=ALU.subtract, op1=ALU.mult)
                OP("pool", "tensor_tensor", [b_res[i2], b_v3], [b_res[i2]], out=res[i2][:], in0=res[i2][:], in1=lng3[:], op=ALU.mult)
                OP("pool", "tensor_tensor", [b_res[i2], b_v3], [b_res[i2]], out=res[i2][:], in0=res[i2][:], in1=lnb3[:], op=ALU.add)
                DMA("sp", f"out{i2}", [b_res[i2]], [], out[j * 128:(j + 1) * 128, :], res[i2][:])
            S.flush(nc, top)

    return nc


def _const_mats():
    idx = np.arange(128)
    ident = np.eye(128, dtype=np.float32)
    triU = (idx[:, None] >= idx[None, :]).astype(np.float32)
    comp = (idx[:, None] < idx[None, :]).astype(np.float32)
    tril = (idx[:, None] < idx[None, :]).astype(np.float32)
    ones = np.ones((128, 128), np.float32)
    return np.concatenate([ident, triU, comp, tril, ones], axis=1)


def _bias_mask(rel_bias):
    kl = np.arange(640)
    q = np.arange(128)
    rel = 512 + q[None, :] - kl[:, None]
    ridx = np.clip(rel, -128, 128) + 128
    cq = 8 + q // 64
    ck = kl // 64
    vis = (ck[:, None] >= cq[None, :] - 8) & (ck[:, None] <= cq[None, :])
    bm = rel_bias[:, ridx]
    bm = np.where(vis[None], bm, np.float32(-30000.0)).astype(np.float32)
    bm = bm.reshape(8, 5, 128, 128).transpose(2, 1, 0, 3)
    return np.ascontiguousarray(bm.reshape(128, 5 * 8 * 128))


def make_in_maps(inputs, cores=range(8)):
    x = np.asarray(inputs["x"], np.float32)
    f = lambda k: np.ascontiguousarray(np.asarray(inputs[k], np.float32)[0])
    shared = {
        "w_in": f("w_in"), "w_out": f("w_out"), "w_q": f("w_q_mem"), "w_kv": f("w_kv_mem"), "w_o": f("w_o_mem"),
        "w_r": f("w_router"), "w_gu": f("w_gate_up"), "w_d": f("w_down"),
        "b_r": f("b_router").reshape(1, NEXP), "b_gu": f("b_gate_up").reshape(NEXP * 16, 128), "b_d": f("b_down"),
        "ln_g": f("ln_g"), "ln_b": f("ln_b"), "gga": f("g_group_a").reshape(1, 512), "ggb": f("g_group_b").reshape(1, 512),
        "bmT": _bias_mask(f("rel_bias")), "cmat": _const_mats(),
        "eoff": np.ascontiguousarray(np.broadcast_to((np.arange(NEXP) * CAP).astype(np.float32)[None, :], (128, NEXP))),
    }
    maps = []
    for c in cores:
        b, r = c // 4, c % 4
        sh = 3 - r
        xbs = np.zeros((SEQ, D), np.float32)
        xbs[sh * 128:] = x[b, :SEQ - sh * 128]
        xoo = np.ascontiguousarray(x[b].reshape(NBLK, 128, D)[r::4].reshape(NOWN * 128, D))
        padm = np.zeros((128, 4), np.float32)
        for Lk in range(4):
            padm[:, Lk] = 1.0 if Lk >= sh else 0.0
        m = dict(shared)
        m.update({"xb": xbs, "xo": xoo, "memb": np.ascontiguousarray(np.asarray(inputs["mem"], np.float32)[b]), "padm": padm})
        maps.append(m)
    return maps


_NC_CACHE = {}


def kernel(**inputs):
    if "nc" not in _NC_CACHE:
        _NC_CACHE["nc"] = build_nc()
    nc = _NC_CACHE["nc"]
    maps = make_in_maps(inputs)
    res = run_bass_kernel_spmd(nc, maps, core_ids=list(range(8)))
    outp = np.zeros((2, SEQ, D), np.float32)
    for c in range(8):
        b, r = c // 4, c % 4
        o = np.asarray(res.results[c]["out"]).reshape(NOWN, 128, D)
        outp[b].reshape(NBLK, 128, D)[r::4] = o
    return outp
```
